# Optimizing a Trainium2 kernel written in Bass

```python
import math
import jax
import jax.numpy as jnp
from jax import lax
import numpy as np

D_MODEL = 1024
BATCH = 4
SEQ = 4096
DEPTH = 4

GRID_W = 64
CTX_LEN = 256
EPS = 1e-6
ROPE_BASE = 10000.0
QBLK = 128

A_HEADS = 4
A_KV_HEADS = 2
A_HEAD_DIM = 64
A_WINDOW = 128
B_D_INNER = 512
B_HEADDIM = 64
B_HEADS = B_D_INNER // B_HEADDIM
B_GROUPS = 2
B_STATE = 64
B_CONV = 5
B_CHUNK = 128
C_HEADS = 4
C_QK_DIM = 32
C_V_DIM = 64

A_WIDTH = A_HEADS * A_HEAD_DIM
C_WIDTH = C_HEADS * C_V_DIM
MIX_WIDTH = A_WIDTH + B_D_INNER + C_WIDTH

A_Q = A_HEADS * A_HEAD_DIM
A_KV = A_KV_HEADS * A_HEAD_DIM
B_XBC = B_D_INNER + 2 * B_GROUPS * B_STATE
B_DT = 2 * B_HEADS
C_QK = C_HEADS * 2 * C_QK_DIM
OFF_AK = A_Q
OFF_AV = OFF_AK + A_KV
OFF_BZ = OFF_AV + A_KV
OFF_BX = OFF_BZ + B_D_INNER
OFF_BDT = OFF_BX + B_XBC
OFF_CQ = OFF_BDT + B_DT
OFF_CK = OFF_CQ + C_QK
OFF_CV = OFF_CK + C_QK
IN_COLS = OFF_CV + C_WIDTH
IN_SPLITS = (OFF_AK, OFF_AV, OFF_BZ, OFF_BX, OFF_BDT, OFF_CQ, OFF_CK, OFF_CV)

N_GROUPS = 4
EXPERTS_PER_GROUP = 8
N_EXPERTS = N_GROUPS * EXPERTS_PER_GROUP
TOP_K = 2
D_EXPERT = 512
MOE_BLK = 256

kernel_name = 'hybrid_flow_backbone'

F32 = jnp.float32


def rmsnorm(x, w):
    xf = x.astype(F32)
    y = xf * lax.rsqrt(jnp.mean(xf * xf, axis=-1, keepdims=True) + EPS)
    return (y * w.astype(F32)).astype(x.dtype)


def modulate(h, shift, scale):
    return h * (1 + scale) + shift


def axial_rope(x, row_pos, col_pos):
    dim = x.shape[-1]
    half = dim // 2
    nf = half // 2
    freqs = ROPE_BASE ** (-jnp.arange(nf, dtype=F32) / nf)

    def rot(u, pos):
        ang = pos.astype(F32)[:, None] * freqs
        cos = jnp.cos(ang)[None, :, None, :].astype(u.dtype)
        sin = jnp.sin(ang)[None, :, None, :].astype(u.dtype)
        u1, u2 = u[..., :nf], u[..., nf:]
        return jnp.concatenate([u1 * cos - u2 * sin, u1 * sin + u2 * cos], axis=-1)

    return jnp.concatenate([rot(x[..., :half], row_pos), rot(x[..., half:], col_pos)], axis=-1)


def window_attention_sink(q, k, v, k_ctx, v_ctx, sink):
    Bsz, L, H, d = q.shape
    G = H // A_KV_HEADS
    nb = L // QBLK
    Lc = k_ctx.shape[1]
    qb = q.reshape(Bsz, nb, QBLK, A_KV_HEADS, G, d)

    def band(u):
        up = jnp.pad(u, ((0, 0), (QBLK, QBLK), (0, 0), (0, 0))).reshape(Bsz, nb + 2, QBLK, A_KV_HEADS, d)
        return jnp.concatenate([up[:, :-2], up[:, 1:-1], up[:, 2:]], axis=2)

    kb, vb = band(k), band(v)
    scale = d ** -0.5
    s_loc = jnp.einsum('bnqhgd,bnkhd->bnhgqk', qb, kb).astype(F32) * scale
    s_ctx = jnp.einsum('bnqhgd,bkhd->bnhgqk', qb, k_ctx).astype(F32) * scale
    blk = jnp.arange(nb)[:, None, None]
    qpos = blk * QBLK + jnp.arange(QBLK)[None, :, None]
    kpos = (blk - 1) * QBLK + jnp.arange(3 * QBLK)[None, None, :]
    mask = (jnp.abs(kpos - qpos) <= A_WINDOW) & (kpos >= 0) & (kpos < L)
    s_loc = jnp.where(mask[None, :, None, None], s_loc, -jnp.inf)
    s_sink = jnp.broadcast_to(sink.astype(F32).reshape(1, 1, A_KV_HEADS, G, 1, 1), s_loc.shape[:-1] + (1,))
    p = jax.nn.softmax(jnp.concatenate([s_loc, s_ctx, s_sink], axis=-1), axis=-1).astype(v.dtype)
    nk = 3 * QBLK
    out = (jnp.einsum('bnhgqk,bnkhd->bnqhgd', p[..., :nk], vb)
           + jnp.einsum('bnhgqk,bkhd->bnqhgd', p[..., nk:nk + Lc], v_ctx))
    return out.reshape(Bsz, L, H * d)


def ctx_attention_sink(q, k, v, sink):
    Bsz, Lq, H, d = q.shape
    G = H // A_KV_HEADS
    qg = q.reshape(Bsz, Lq, A_KV_HEADS, G, d)
    s = jnp.einsum('bqhgd,bkhd->bhgqk', qg, k).astype(F32) * d ** -0.5
    s_sink = jnp.broadcast_to(sink.astype(F32).reshape(1, A_KV_HEADS, G, 1, 1), s.shape[:-1] + (1,))
    p = jax.nn.softmax(jnp.concatenate([s, s_sink], axis=-1), axis=-1)[..., :-1].astype(v.dtype)
    return jnp.einsum('bhgqk,bkhd->bqhgd', p, v).reshape(Bsz, Lq, H * d)


def depthwise_conv_centred(u, w, b):
    out = lax.conv_general_dilated(u, w[:, None, :].astype(u.dtype), window_strides=(1,),
                                   padding=[(B_CONV // 2, B_CONV // 2)],
                                   dimension_numbers=('NWC', 'WIO', 'NWC'),
                                   feature_group_count=u.shape[-1])
    return out + b.astype(u.dtype)


def ssd_chunked(x, dt, a, bm, cm, h0, want_y):
    Bsz, L, H, P = x.shape
    nc = L // B_CHUNK
    rep = H // B_GROUPS
    bh = jnp.repeat(bm, rep, axis=2).astype(F32).reshape(Bsz, nc, B_CHUNK, H, B_STATE)
    xdt = (x.astype(F32) * dt[..., None]).reshape(Bsz, nc, B_CHUNK, H, P)
    a_cum = jnp.cumsum((dt * a).reshape(Bsz, nc, B_CHUNK, H), axis=2)
    a_end = a_cum[:, :, -1]
    states = jnp.einsum('bclhn,bclh,bclhp->bchpn', bh, jnp.exp(a_end[:, :, None] - a_cum), xdt)

    def step(h, inp):
        dec, st = inp
        return dec[..., None, None] * h + st, h

    h_final, h_start = lax.scan(step, h0, (jnp.moveaxis(jnp.exp(a_end), 1, 0), jnp.moveaxis(states, 1, 0)))
    if not want_y:
        return None, h_final
    h_start = jnp.moveaxis(h_start, 0, 1)
    ch = jnp.repeat(cm, rep, axis=2).astype(F32).reshape(Bsz, nc, B_CHUNK, H, B_STATE)
    tri = jnp.tril(jnp.ones((B_CHUNK, B_CHUNK), bool))[None, None, :, :, None]
    seg = a_cum[:, :, :, None, :] - a_cum[:, :, None, :, :]
    decay = jnp.where(tri, jnp.exp(jnp.where(tri, seg, 0.0)), 0.0)
    scores = jnp.einsum('bclhn,bcshn->bclsh', ch, bh) * decay
    y_diag = jnp.einsum('bclsh,bcshp->bclhp', scores, xdt)
    y_off = jnp.einsum('bclhn,bchpn,bclh->bclhp', ch, h_start, jnp.exp(a_cum))
    return (y_diag + y_off).reshape(Bsz, L, H, P), h_final


def ssd_bidirectional(z, xbc, dt_raw, z_ctx, xbc_ctx, dt_raw_ctx, conv_w, conv_b, dt_bias, a_log,
                      d_skip, norm_w, need_ctx):
    a = -jnp.exp(a_log.astype(F32))

    def prep(xbc_, dt_raw_):
        Bsz, L, _ = xbc_.shape
        u = jax.nn.silu(depthwise_conv_centred(xbc_, conv_w, conv_b))
        xs, bm, cm = jnp.split(u, [B_D_INNER, B_D_INNER + B_GROUPS * B_STATE], axis=-1)
        xs = xs.reshape(Bsz, L, B_HEADS, B_HEADDIM)
        bm = bm.reshape(Bsz, L, B_GROUPS, B_STATE)
        cm = cm.reshape(Bsz, L, B_GROUPS, B_STATE)
        dt = jax.nn.softplus(dt_raw_.astype(F32).reshape(Bsz, L, 2, B_HEADS) + dt_bias.astype(F32))
        return xs, bm, cm, dt

    def scan_dir(xs, bm, cm, dt, dirn, h0, want_y):
        dt_d = dt[:, :, dirn]
        if dirn == 1:
            xs, bm, cm, dt_d = (jnp.flip(xs, 1), jnp.flip(bm, 1), jnp.flip(cm, 1), jnp.flip(dt_d, 1))
        y, h_t = ssd_chunked(xs, dt_d, a[dirn], bm, cm, h0, want_y)
        if dirn == 1 and y is not None:
            y = jnp.flip(y, 1)
        return y, h_t

    def finish(y_f, y_b, xs, z_):
        Bsz, L = xs.shape[:2]
        y = y_f + y_b + xs.astype(F32) * (d_skip[0] + d_skip[1]).astype(F32)[:, None]
        g = y.reshape(Bsz, L, B_D_INNER) * jax.nn.silu(z_.astype(F32))
        g = g.reshape(Bsz, L, B_GROUPS, B_D_INNER // B_GROUPS)
        g = g * lax.rsqrt(jnp.mean(g * g, axis=-1, keepdims=True) + EPS)
        return (g.reshape(Bsz, L, B_D_INNER) * norm_w.astype(F32)).astype(z_.dtype)

    xs_c, bm_c, cm_c, dt_c = prep(xbc_ctx, dt_raw_ctx)
    h0 = jnp.zeros((xs_c.shape[0], B_HEADS, B_HEADDIM, B_STATE), F32)
    yc_f, hc_f = scan_dir(xs_c, bm_c, cm_c, dt_c, 0, h0, need_ctx)
    yc_b, hc_b = scan_dir(xs_c, bm_c, cm_c, dt_c, 1, h0, need_ctx)
    xs, bm, cm, dt = prep(xbc, dt_raw)
    y_f, _ = scan_dir(xs, bm, cm, dt, 0, hc_f, True)
    y_b, _ = scan_dir(xs, bm, cm, dt, 1, hc_b, True)
    out = finish(y_f, y_b, xs, z)
    out_ctx = finish(yc_f, yc_b, xs_c, z_ctx) if need_ctx else None
    return out, out_ctx


def diff_attend(q, k, v, lam):
    s = jnp.einsum('bqhmd,bkhmd->bhmqk', q, k).astype(F32) * q.shape[-1] ** -0.5
    p = jax.nn.softmax(s, axis=-1)
    w = (p[:, :, 0] - lam * p[:, :, 1]).astype(v.dtype)
    return jnp.einsum('bhqk,bkhd->bqhd', w, v)


def diff_attention_latent(q, k, v, k_ctx, v_ctx, lam):
    Bsz, L = q.shape[:2]
    nb = L // QBLK
    k_all = jnp.concatenate([k, k_ctx], axis=1)
    v_all = jnp.concatenate([v, v_ctx], axis=1)
    qb = jnp.moveaxis(q.reshape(Bsz, nb, QBLK, C_HEADS, 2, C_QK_DIM), 1, 0)
    out = lax.map(lambda qblk: diff_attend(qblk, k_all, v_all, lam), qb)
    return jnp.moveaxis(out, 0, 1).reshape(Bsz, L, C_HEADS, C_V_DIM)


def diff_post(o, subln_w, lambda_init):
    return (rmsnorm(o, subln_w) * (1.0 - lambda_init)).reshape(o.shape[0], o.shape[1], C_WIDTH)


def token_mixing(h, h_ctx, w_in, w_out, a_sink, conv_w, conv_b, dt_bias, a_log, d_skip, b_norm_w,
                 c_lambda, c_subln_w, lambda_init, row_pos, col_pos, need_ctx):
    Bsz, L, _ = h.shape
    Lc = h_ctx.shape[1]
    aq, ak, av, bz, bx, bdt, cq, ck, cv = jnp.split(jnp.dot(h, w_in), IN_SPLITS, axis=-1)
    aq_c, ak_c, av_c, bz_c, bx_c, bdt_c, cq_c, ck_c, cv_c = jnp.split(jnp.dot(h_ctx, w_in), IN_SPLITS, axis=-1)
    q = axial_rope(aq.reshape(Bsz, L, A_HEADS, A_HEAD_DIM), row_pos, col_pos)
    k = axial_rope(ak.reshape(Bsz, L, A_KV_HEADS, A_HEAD_DIM), row_pos, col_pos)
    v = av.reshape(Bsz, L, A_KV_HEADS, A_HEAD_DIM)
    k_c = ak_c.reshape(Bsz, Lc, A_KV_HEADS, A_HEAD_DIM)
    v_c = av_c.reshape(Bsz, Lc, A_KV_HEADS, A_HEAD_DIM)
    out_a = window_attention_sink(q, k, v, k_c, v_c, a_sink)
    out_b, out_b_c = ssd_bidirectional(bz, bx, bdt, bz_c, bx_c, bdt_c, conv_w, conv_b, dt_bias, a_log,
                                       d_skip, b_norm_w, need_ctx)
    lq1, lk1, lq2, lk2 = c_lambda.astype(F32)
    lam = jnp.exp(jnp.sum(lq1 * lk1)) - jnp.exp(jnp.sum(lq2 * lk2)) + lambda_init
    qd = axial_rope(cq.reshape(Bsz, L, 2 * C_HEADS, C_QK_DIM), row_pos, col_pos).reshape(Bsz, L, C_HEADS, 2, C_QK_DIM)
    kd = axial_rope(ck.reshape(Bsz, L, 2 * C_HEADS, C_QK_DIM), row_pos, col_pos).reshape(Bsz, L, C_HEADS, 2, C_QK_DIM)
    vd = cv.reshape(Bsz, L, C_HEADS, C_V_DIM)
    kd_c = ck_c.reshape(Bsz, Lc, C_HEADS, 2, C_QK_DIM)
    vd_c = cv_c.reshape(Bsz, Lc, C_HEADS, C_V_DIM)
    out_c = diff_post(diff_attention_latent(qd, kd, vd, kd_c, vd_c, lam), c_subln_w, lambda_init)
    out = jnp.dot(jnp.concatenate([out_a, out_b, out_c], axis=-1), w_out)
    if not need_ctx:
        return out, None
    out_a_c = ctx_attention_sink(aq_c.reshape(Bsz, Lc, A_HEADS, A_HEAD_DIM), k_c, v_c, a_sink)
    qd_c = cq_c.reshape(Bsz, Lc, C_HEADS, 2, C_QK_DIM)
    out_c_c = diff_post(diff_attend(qd_c, kd_c, vd_c, lam), c_subln_w, lambda_init)
    out_ctx = jnp.dot(jnp.concatenate([out_a_c, out_b_c, out_c_c], axis=-1), w_out)
    return out, out_ctx


def grouped_expert_mlp(t, e_idx, gate, w_gate, w_up, w_down):
    N, D = t.shape
    A = N * TOP_K
    flat_e = e_idx.reshape(-1)
    order = jnp.argsort(flat_e)
    sorted_e = flat_e[order]
    counts = jnp.bincount(flat_e, length=N_EXPERTS)
    padded = (counts + MOE_BLK - 1) // MOE_BLK * MOE_BLK
    pad_end = jnp.cumsum(padded)
    pad_start = pad_end - padded
    start = jnp.cumsum(counts) - counts
    dest = pad_start[sorted_e] + jnp.arange(A) - start[sorted_e]
    n_blocks = -(-A // MOE_BLK) + N_EXPERTS
    tok = order // TOP_K
    buf = jnp.zeros((n_blocks * MOE_BLK, D), t.dtype).at[dest].set(t[tok])
    block_e = jnp.minimum(jnp.searchsorted(pad_end, jnp.arange(n_blocks) * MOE_BLK, side='right'), N_EXPERTS - 1)

    def run(args):
        xb, e = args
        hid = jax.nn.silu(jnp.dot(xb, w_gate[e])) * jnp.dot(xb, w_up[e])
        return jnp.dot(hid, w_down[e])

    out = lax.map(run, (buf.reshape(n_blocks, MOE_BLK, D), block_e)).reshape(-1, D)
    w_sorted = gate.reshape(-1)[order].astype(t.dtype)
    return jnp.zeros_like(t).at[tok].add(out[dest] * w_sorted[:, None])


def hier_moe(t, w_group_router, w_router, w_gate, w_up, w_down):
    N = t.shape[0]
    g_prob = jax.nn.softmax(jnp.dot(t, w_group_router).astype(F32), axis=-1)
    g_p, g_sel = lax.top_k(g_prob, 1)
    e_logits = jnp.dot(t, w_router).astype(F32).reshape(N, N_GROUPS, EXPERTS_PER_GROUP)
    in_grp = jnp.take_along_axis(e_logits, g_sel[:, :, None], axis=1)[:, 0]
    e_val, e_loc = lax.top_k(in_grp, TOP_K)
    gate = g_p * jax.nn.softmax(e_val, axis=-1)
    e_idx = g_sel * EXPERTS_PER_GROUP + e_loc
    return grouped_expert_mlp(t, e_idx, gate, w_gate, w_up, w_down)


def setup_inputs(seed: int = 0) -> dict:
    key = jax.random.key(seed)
    ks = jax.random.split(key, 26)

    def nrm(k, shape, scale):
        return jax.random.normal(k, shape, F32) * scale

    dt0 = jnp.exp(jax.random.uniform(ks[12], (DEPTH, 2, B_HEADS), F32, math.log(1e-3), math.log(1e-1)))
    return {
        'x': nrm(ks[0], (BATCH, SEQ, D_MODEL), 1.0),
        'c': nrm(ks[1], (BATCH, D_MODEL), 1.0),
        'ctx': nrm(ks[2], (BATCH, CTX_LEN, D_MODEL), 1.0),
        'c_ctx': nrm(ks[3], (D_MODEL,), 1.0),
        'w_mod': nrm(ks[4], (DEPTH, D_MODEL, 6 * D_MODEL), 0.3 * D_MODEL ** -0.5),
        'b_mod': nrm(ks[5], (DEPTH, 6 * D_MODEL), 0.01),
        'norm1_w': 1.0 + nrm(ks[6], (DEPTH, D_MODEL), 0.02),
        'norm2_w': 1.0 + nrm(ks[7], (DEPTH, D_MODEL), 0.02),
        'w_in': nrm(ks[8], (DEPTH, D_MODEL, IN_COLS), D_MODEL ** -0.5),
        'w_out': nrm(ks[9], (DEPTH, MIX_WIDTH, D_MODEL), MIX_WIDTH ** -0.5),
        'a_sink': nrm(ks[10], (DEPTH, A_HEADS), 0.5),
        'b_conv_w': nrm(ks[11], (DEPTH, B_CONV, B_XBC), B_CONV ** -0.5),
        'b_conv_b': nrm(ks[13], (DEPTH, B_XBC), 0.01),
        'b_dt_bias': dt0 + jnp.log(-jnp.expm1(-dt0)),
        'b_a_log': jnp.log(jax.random.uniform(ks[14], (DEPTH, 2, B_HEADS), F32, 1.0, 16.0)),
        'b_d': 0.5 + nrm(ks[15], (DEPTH, 2, B_HEADS), 0.1),
        'b_norm_w': 1.0 + nrm(ks[16], (DEPTH, B_D_INNER), 0.02),
        'c_lambda': nrm(ks[17], (DEPTH, 4, C_QK_DIM), 0.1),
        'c_subln_w': 1.0 + nrm(ks[18], (DEPTH, C_V_DIM), 0.02),
        'moe_group_router': nrm(ks[19], (DEPTH, D_MODEL, N_GROUPS), D_MODEL ** -0.5),
        'moe_router': nrm(ks[20], (DEPTH, D_MODEL, N_EXPERTS), D_MODEL ** -0.5),
        'moe_w_gate': nrm(ks[21], (DEPTH, N_EXPERTS, D_MODEL, D_EXPERT), D_MODEL ** -0.5),
        'moe_w_up': nrm(ks[22], (DEPTH, N_EXPERTS, D_MODEL, D_EXPERT), D_MODEL ** -0.5),
        'moe_w_down': nrm(ks[23], (DEPTH, N_EXPERTS, D_EXPERT, D_MODEL), D_EXPERT ** -0.5),
        'final_norm_w': 1.0 + nrm(ks[24], (D_MODEL,), 0.02),
    }


def reference(x, c, ctx, c_ctx, w_mod, b_mod, norm1_w, norm2_w, w_in, w_out, a_sink, b_conv_w, b_conv_b,
              b_dt_bias, b_a_log, b_d, b_norm_w, c_lambda, c_subln_w, moe_group_router, moe_router,
              moe_w_gate, moe_w_up, moe_w_down, final_norm_w):
    Bsz, L, D = x.shape
    rows = L // GRID_W
    row_pos = jnp.repeat(jnp.arange(rows, dtype=jnp.int32), GRID_W)
    col_pos = jnp.tile(jnp.arange(GRID_W, dtype=jnp.int32), rows)
    x_ctx = ctx
    for l in range(DEPTH):
        last = l == DEPTH - 1
        mod = jnp.dot(jax.nn.silu(c), w_mod[l]) + b_mod[l]
        mod_ctx = jnp.dot(jax.nn.silu(c_ctx), w_mod[l]) + b_mod[l]
        sh1, sc1, g1, sh2, sc2, g2 = jnp.split(mod[:, None, :], 6, axis=-1)
        sh1c, sc1c, g1c, sh2c, sc2c, g2c = jnp.split(mod_ctx, 6, axis=-1)
        h = modulate(rmsnorm(x, norm1_w[l]), sh1, sc1)
        h_ctx = modulate(rmsnorm(x_ctx, norm1_w[l]), sh1c, sc1c)
        lambda_init = 0.8 - 0.6 * math.exp(-0.3 * l)
        mix, mix_ctx = token_mixing(h, h_ctx, w_in[l], w_out[l], a_sink[l], b_conv_w[l], b_conv_b[l],
                                    b_dt_bias[l], b_a_log[l], b_d[l], b_norm_w[l], c_lambda[l], c_subln_w[l],
                                    lambda_init, row_pos, col_pos, not last)
        x = x + g1 * mix
        h2 = modulate(rmsnorm(x, norm2_w[l]), sh2, sc2).reshape(-1, D)
        if last:
            y = hier_moe(h2, moe_group_router[l], moe_router[l], moe_w_gate[l], moe_w_up[l], moe_w_down[l])
            x = x + g2 * y.reshape(Bsz, L, D)
        else:
            x_ctx = x_ctx + g1c * mix_ctx
            h2c = modulate(rmsnorm(x_ctx, norm2_w[l]), sh2c, sc2c).reshape(-1, D)
            y = hier_moe(jnp.concatenate([h2, h2c], axis=0), moe_group_router[l], moe_router[l],
                         moe_w_gate[l], moe_w_up[l], moe_w_down[l])
            x = x + g2 * y[:Bsz * L].reshape(Bsz, L, D)
            x_ctx = x_ctx + g2c * y[Bsz * L:].reshape(Bsz, -1, D)
    return rmsnorm(x, final_norm_w)
```

```python
import math
from contextlib import ExitStack

import numpy as np
import ml_dtypes
import concourse.bass as bass
import concourse.mybir as mybir
from concourse.bass_utils import run_bass_kernel_spmd

F32 = mybir.dt.float32
BF16 = mybir.dt.bfloat16
AF = mybir.ActivationFunctionType
ALU = mybir.AluOpType
AX = mybir.AxisListType

D = 1024
DEPTH = 4
NEG = -30000.0
EPS = 1e-6
EPOCH = 12000
import os as _os
NOSELF = set(_os.environ.get('NOSELF', 'pe').split(','))


class Dep:
    __slots__ = ("w", "r", "dsem", "dcnt")

    def __init__(self):
        self.w = None
        self.r = {}
        self.dsem = None
        self.dcnt = 0


class Sched:
    def __init__(self, nc, stack):
        self.nc = nc
        self.stack = stack
        self.eng = {"pe": nc.tensor, "dve": nc.vector, "act": nc.scalar, "pool": nc.gpsimd, "sp": nc.sync}
        self.sems = {}
        self.cnt = {e: 0 for e in self.eng}
        self.epoch = {e: 0 for e in self.eng}
        self.waited = {e: {} for e in self.eng}
        self.ndsem = 0
        self.n_ins = 0
        self.n_wait = 0
        self.dma_deps = []
        self.free_dsems = []

    def _sem(self, key):
        if key not in self.sems:
            self.sems[key] = self.stack.enter_context(self.nc.semaphore("s_%s_%s" % key))
        return self.sems[key]

    def _wait(self, eng, need):
        wd = self.waited[eng]
        e = self.eng[eng]
        for k, v in need.items():
            if wd.get(k, 0) >= v:
                continue
            e.wait_ge(self._sem(k), v)
            wd[k] = v
            self.n_wait += 1

    def _collect(self, eng, reads, writes, pe_acc):
        need = {}
        for d in reads:
            if d.w is not None:
                k, v = d.w
                if need.get(k, 0) < v:
                    need[k] = v
        for d in writes:
            if d.w is not None:
                k, v = d.w
                if not (pe_acc and k[0] == "pe" and eng == "pe"):
                    if need.get(k, 0) < v:
                        need[k] = v
            for k, v in d.r.items():
                if need.get(k, 0) < v:
                    need[k] = v
        if eng in NOSELF:
            need = {k: v for k, v in need.items() if k[0] != eng}
        self._wait(eng, need)

    def op(self, eng, emit, reads=(), writes=(), pe_acc=False):
        self._collect(eng, reads, writes, pe_acc)
        ins = emit(self.eng[eng])
        if self.cnt[eng] >= EPOCH:
            self.epoch[eng] += 1
            self.cnt[eng] = 0
        self.cnt[eng] += 1
        key = (eng, self.epoch[eng])
        ins.then_inc(self._sem(key), 1)
        c = self.cnt[eng]
        for d in reads:
            d.r[key] = c
        for d in writes:
            d.w = (key, c)
            d.r = {}
        self.n_ins += 1
        return ins

    def dma(self, q, out, in_, reads=(), writes=(), **kw):
        self._collect(q, reads, writes, False)
        d0 = writes[0]
        if d0.dsem is None:
            if self.free_dsems:
                d0.dsem, d0.dcnt = self.free_dsems.pop()
            else:
                d0.dsem = ("dma", self.ndsem)
                self.ndsem += 1
            self.dma_deps.append(d0)
        d0.dcnt += 16
        ins = self.eng[q].dma_start(out=out, in_=in_, **kw)
        ins.then_inc(self._sem(d0.dsem), 16)
        for d in reads:
            d.r[d0.dsem] = d0.dcnt
        for d in writes:
            d.w = (d0.dsem, d0.dcnt)
            d.r = {}
        self.n_ins += 1
        return ins

    def idma(self, out, in_, idx_ap, gather, nslot, reads=(), writes=()):
        self._collect("pool", reads, writes, False)
        d0 = writes[0]
        if d0.dsem is None:
            if self.free_dsems:
                d0.dsem, d0.dcnt = self.free_dsems.pop()
            else:
                d0.dsem = ("dma", self.ndsem)
                self.ndsem += 1
            self.dma_deps.append(d0)
        d0.dcnt += 16
        off = bass.IndirectOffsetOnAxis(ap=idx_ap, axis=0)
        if gather:
            ins = self.nc.gpsimd.indirect_dma_start(out=out, out_offset=None, in_=in_, in_offset=off, bounds_check=None)
        else:
            ins = self.nc.gpsimd.indirect_dma_start(out=out, out_offset=off, in_=in_, in_offset=None, bounds_check=None)
        ins.then_inc(self._sem(d0.dsem), 16)
        for d in reads:
            d.r[d0.dsem] = d0.dcnt
        for d in writes:
            d.w = (d0.dsem, d0.dcnt)
            d.r = {}
        self.n_ins += 1
        return ins

    def barrier(self):
        need = {}
        for e in self.eng:
            if self.cnt[e] > 0:
                need[(e, self.epoch[e])] = self.cnt[e]
        for d in self.dma_deps:
            need[d.dsem] = d.dcnt
        for e in self.eng:
            self._wait(e, dict(need))
        for d in self.dma_deps:
            self.free_dsems.append((d.dsem, d.dcnt))
            d.dsem = None
            d.dcnt = 0
            if d.w is not None and d.w[0][0] == "dma":
                d.w = None
            d.r = {k: v for k, v in d.r.items() if k[0] != "dma"}
        self.dma_deps = []

    def finish(self, deps, eng="sp"):
        self._collect(eng, deps, (), False)


class Ctx:
    K = [0]

    def __init__(self, nc, st):
        self.nc, self.st = nc, st

    def sb(self, shape, dt, st=None):
        Ctx.K[0] += 1
        return (st or self.st).enter_context(self.nc.sbuf_tensor("t%d" % Ctx.K[0], list(shape), dt))

    def ps(self, shape, dt, st=None):
        Ctx.K[0] += 1
        return (st or self.st).enter_context(self.nc.psum_tensor("p%d" % Ctx.K[0], list(shape), dt))


def rstd_from_ss(S, ss_ap, out_ap, dep, n, eps_ap):
    S.op("act", lambda e: e.activation(out_ap, ss_ap, AF.Ln, bias=eps_ap, scale=1.0 / n), reads=[dep], writes=[dep])
    S.op("act", lambda e: e.activation(out_ap, out_ap, AF.Exp, scale=-0.5), reads=[dep], writes=[dep])


def build_k0():
    nc = bass.Bass("TRN2", target_bir_lowering=False)
    ccT = nc.dram_tensor("ccT", [D, 5], F32, kind="ExternalInput").ap()
    w = nc.dram_tensor("w", [D, 3072], F32, kind="ExternalInput").ap()
    b = nc.dram_tensor("b", [1, 3072], F32, kind="ExternalInput").ap()
    out = nc.dram_tensor("out", [5, 3072], F32, kind="ExternalOutput").ap()
    with ExitStack() as st:
        S = Sched(nc, st)
        C = Ctx(nc, st)
        cs = C.sb([128, 8, 5], F32); dcs = Dep()
        sg = C.sb([128, 8, 5], F32)
        ws = C.sb([128, 8, 3072], F32); dws = [Dep() for _ in range(8)]
        bs = C.sb([5, 3072], F32); dbs = Dep()
        os_ = C.sb([5, 3072], F32); dos = Dep()
        pp = [C.ps([5, 512], F32) for _ in range(2)]; dpp = [Dep(), Dep()]
        dout = Dep()
        S.dma("sp", cs[:], ccT.rearrange("(c p) m -> p c m", p=128), writes=[dcs])
        S.dma("sp", bs[:], b.partition_broadcast(5), writes=[dbs])
        wv = w.rearrange("(c p) n -> p c n", p=128)
        for c in range(8):
            S.dma("sp", ws[:, c, :], wv[:, c, :], writes=[dws[c]])
        S.op("act", lambda e: e.activation(sg[:], cs[:], AF.Sigmoid), reads=[dcs], writes=[dcs])
        S.op("dve", lambda e: e.tensor_tensor(cs[:], cs[:], sg[:], op=ALU.mult), reads=[dcs], writes=[dcs])
        for n in range(6):
            p = pp[n % 2]; dp = dpp[n % 2]
            for c in range(8):
                S.op("pe", lambda e: e.matmul(p[:], cs[:, c, :], ws[:, c, n * 512:(n + 1) * 512], start=(c == 0), stop=(c == 7)),
                     reads=[dcs, dws[c]], writes=[dp], pe_acc=(c > 0))
            S.op("dve", lambda e: e.tensor_tensor(os_[:, n * 512:(n + 1) * 512], p[:], bs[:, n * 512:(n + 1) * 512], op=ALU.add),
                 reads=[dp, dbs], writes=[dos])
        S.dma("sp", out, os_[:], reads=[dos], writes=[dout])
        S.finish([dout])
    return nc


NT2 = 17
NTOK2 = NT2 * 128
NEXP = 32


def emit_k2(nc, S, I, dX, dF, dmix, dmods, do_fin=True):
    with ExitStack() as st:
        C = Ctx(nc, st)
        h2T = C.sb([128, 8, NTOK2], BF16); dh2T = [Dep() for _ in range(NT2)]
        gates = C.sb([128, NT2, NEXP], F32); dgates = [Dep() for _ in range(NT2)]
        yacc = C.sb([128, NT2, D], F32); dyacc = [Dep() for _ in range(NT2)]
        bc = [C.sb([128, D], F32) for _ in range(6)]; dbc = [Dep() for _ in range(6)]
        epst = C.sb([128, 1], F32); deps_ = Dep()
        idf = C.sb([128, 128], F32); didf = Dep()
        idb = C.sb([128, 128], BF16); didb = Dep()
        dxout = dX
        dxfin = dF
        S.op("dve", lambda e: e.memset(epst[:], EPS), writes=[deps_])
        S.dma("sp", idf[:], I.ident, writes=[didf])
        S.dma("pool", idb[:], I.ident, writes=[didb])

        with ExitStack() as st1:
            woutb = C.sb([128, 8, D], BF16, st1); dwoutb = Dep()
            wrs = C.sb([128, 8, 36], F32, st1); dwrs = Dep()
            tmpw = C.sb([128, D], F32, st1); dtmpw = Dep()
            xt = [C.sb([128, D], F32, st1) for _ in range(2)]; dxt = [Dep(), Dep()]
            mt = [C.sb([128, 8, 128], BF16, st1) for _ in range(2)]; dmt = [Dep(), Dep()]
            tmp = C.sb([128, D], F32, st1); dtmp = Dep()
            x1 = [C.sb([128, D], F32, st1) for _ in range(2)]; dx1 = [Dep(), Dep()]
            h2f = C.sb([128, D], F32, st1); dh2f = Dep()
            h2b = C.sb([128, D], BF16, st1); dh2b = Dep()
            h2Tf = C.sb([128, 8, 128], F32, st1); dh2Tf = Dep()
            junk = C.sb([128, D], F32, st1); djunk = Dep()
            sm = C.sb([128, 16], F32, st1); dsm = Dep()
            lg = C.sb([128, 36], F32, st1); dlg = Dep()
            elm = C.sb([128, 32], F32, st1); delm = Dep()
            elm2 = C.sb([128, 32], F32, st1)
            mk1 = C.sb([128, 32], F32, st1)
            mk2 = C.sb([128, 32], F32, st1)
            gm = C.sb([128, 8], F32, st1)
            pmix = [C.ps([128, 512], F32, st1) for _ in range(2)]; dpmix = [Dep(), Dep()]
            ptb = C.ps([128, 8, 128], BF16, st1); dptb = Dep()
            ptf = C.ps([128, 8, 128], F32, st1); dptf = Dep()
            prt = C.ps([128, 36], F32, st1); dprt = Dep()

            S.dma("pool", woutb[:], I.wout.rearrange("(c p) n -> p c n", p=128), writes=[dwoutb])
            S.dma("sp", wrs[:], I.wr.rearrange("(c p) n -> p c n", p=128), writes=[dwrs])
            S.dma("sp", tmpw[:], I.n2w.partition_broadcast(128), writes=[dtmpw])
            for v in range(2):
                S.dma("sp", bc[v][:], I.modrow(v, 2).partition_broadcast(128), reads=dmods, writes=[dbc[v]])
                S.dma("sp", bc[2 + v][:], I.modrow(v, 4).partition_broadcast(128), reads=dmods, writes=[dbc[2 + v]])
                S.dma("sp", bc[4 + v][:], I.modrow(v, 3).partition_broadcast(128), reads=dmods, writes=[dbc[4 + v]])
                S.op("dve", lambda e: e.scalar_tensor_tensor(bc[2 + v][:], bc[2 + v][:], 1.0, tmpw[:], op0=ALU.add, op1=ALU.mult),
                     reads=[dtmpw], writes=[dbc[2 + v]])

            for t in range(NT2):
                v = 1 if t == NT2 - 1 else 0
                b2 = t % 2
                tsl = slice(t * 128, (t + 1) * 128)
                S.dma("sp", xt[b2][:], I.xrow(t), reads=[dxout[t]], writes=[dxt[b2]])
                S.dma("sp", mt[b2][:], I.mtv(t), reads=dmix, writes=[dmt[b2]])
                for hf in range(2):
                    for c in range(8):
                        S.op("pe", lambda e: e.matmul(pmix[hf][:], mt[b2][:, c, :], woutb[:, c, hf * 512:(hf + 1) * 512],
                                                      start=(c == 0), stop=(c == 7)),
                             reads=[dmt[b2], dwoutb], writes=[dpmix[hf]], pe_acc=(c > 0))
                    S.op("dve", lambda e: e.tensor_tensor(tmp[:, hf * 512:(hf + 1) * 512], pmix[hf][:], bc[v][:, hf * 512:(hf + 1) * 512], op=ALU.mult),
                         reads=[dpmix[hf], dbc[v]], writes=[dtmp])
                S.op("pool", lambda e: e.tensor_tensor(x1[b2][:], tmp[:], xt[b2][:], op=ALU.add), reads=[dtmp, dxt[b2]], writes=[dx1[b2]])
                S.dma("sp", I.orow(t), x1[b2][:], reads=[dx1[b2]], writes=[dxout[t]])
                S.op("dve", lambda e: e.memset(sm[:], 0.0), writes=[dsm])
                S.op("act", lambda e: e.activation(junk[:], x1[b2][:], AF.Square, accum_out=sm[:, 0:1]), reads=[dx1[b2], dsm], writes=[djunk, dsm])
                rstd_from_ss(S, sm[:, 0:1], sm[:, 1:2], dsm, D, epst[:])
                S.op("dve", lambda e: e.scalar_tensor_tensor(h2f[:], x1[b2][:], sm[:, 1:2], bc[2 + v][:], op0=ALU.mult, op1=ALU.mult),
                     reads=[dx1[b2], dsm, dbc[2 + v]], writes=[dh2f])
                S.op("pool", lambda e: e.tensor_tensor(h2f[:], h2f[:], bc[4 + v][:], op=ALU.add), reads=[dbc[4 + v]], writes=[dh2f])
                S.op("act", lambda e: e.copy(h2b[:], h2f[:]), reads=[dh2f], writes=[dh2b])
                for c in range(8):
                    S.op("pe", lambda e: e.transpose(ptb[:, c, :], h2b[:, c * 128:(c + 1) * 128], idb[:]), reads=[dh2b, didb], writes=[dptb], pe_acc=(c > 0))
                S.op("act", lambda e: e.copy(h2T[:, :, tsl], ptb[:]), reads=[dptb], writes=[dh2T[t]])
                for c in range(8):
                    S.op("pe", lambda e: e.transpose(ptf[:, c, :], h2f[:, c * 128:(c + 1) * 128], idf[:]), reads=[dh2f, didf], writes=[dptf], pe_acc=(c > 0))
                S.op("dve", lambda e: e.tensor_copy(h2Tf[:], ptf[:]), reads=[dptf], writes=[dh2Tf])
                for c in range(8):
                    S.op("pe", lambda e: e.matmul(prt[:], h2Tf[:, c, :], wrs[:, c, :], start=(c == 0), stop=(c == 7)),
                         reads=[dh2Tf, dwrs], writes=[dprt], pe_acc=(c > 0))
                S.op("act", lambda e: e.copy(lg[:], prt[:]), reads=[dprt], writes=[dlg])
                R = [dlg, dsm]
                V = lambda f: S.op("dve", f, reads=R, writes=R)
                V(lambda e: e.reduce_max(sm[:, 2:3], lg[:, 0:4], axis=AX.X))
                V(lambda e: e.tensor_scalar(sm[:, 3:4], sm[:, 2:3], -1.0, None, op0=ALU.mult))
                S.op("act", lambda e: e.activation(gm[:, 0:4], lg[:, 0:4], AF.Exp, bias=sm[:, 3:4], scale=1.0, accum_out=sm[:, 4:5]), reads=R, writes=R)
                V(lambda e: e.reciprocal(sm[:, 5:6], sm[:, 4:5]))
                V(lambda e: e.tensor_scalar(gm[:, 4:8], lg[:, 0:4], sm[:, 2:3], None, op0=ALU.is_equal))
                V(lambda e: e.tensor_scalar(gm[:, 4:8], gm[:, 4:8], 1e30, -1e30, op0=ALU.mult, op1=ALU.add))
                for g in range(4):
                    V(lambda e: e.tensor_scalar(elm[:, g * 8:(g + 1) * 8], lg[:, 4 + g * 8:12 + g * 8], gm[:, 4 + g:5 + g], None, op0=ALU.add))
                V(lambda e: e.reduce_max(sm[:, 6:7], elm[:], axis=AX.X))
                V(lambda e: e.tensor_scalar(mk1[:], elm[:], sm[:, 6:7], None, op0=ALU.is_equal))
                V(lambda e: e.scalar_tensor_tensor(elm2[:], mk1[:], -1e30, elm[:], op0=ALU.mult, op1=ALU.add))
                V(lambda e: e.reduce_max(sm[:, 7:8], elm2[:], axis=AX.X))
                V(lambda e: e.tensor_scalar(mk2[:], elm2[:], sm[:, 7:8], None, op0=ALU.is_equal))
                V(lambda e: e.tensor_tensor(sm[:, 8:9], sm[:, 7:8], sm[:, 6:7], op=ALU.subtract))
                S.op("act", lambda e: e.activation(sm[:, 9:10], sm[:, 8:9], AF.Exp), reads=R, writes=R)
                V(lambda e: e.tensor_scalar(sm[:, 10:11], sm[:, 9:10], 1.0, None, op0=ALU.add))
                V(lambda e: e.reciprocal(sm[:, 11:12], sm[:, 10:11]))
                V(lambda e: e.tensor_tensor(sm[:, 12:13], sm[:, 11:12], sm[:, 5:6], op=ALU.mult))
                V(lambda e: e.tensor_tensor(sm[:, 13:14], sm[:, 12:13], sm[:, 9:10], op=ALU.mult))
                S.op("dve", lambda e: e.tensor_scalar(gates[:, t, :], mk1[:], sm[:, 12:13], None, op0=ALU.mult), reads=R, writes=[dgates[t]])
                S.op("dve", lambda e: e.scalar_tensor_tensor(gates[:, t, :], mk2[:], sm[:, 13:14], gates[:, t, :], op0=ALU.mult, op1=ALU.add),
                     reads=R, writes=[dgates[t]])
            S.barrier()

        with ExitStack() as st2:
            wgb = [C.sb([128, 8, 512], BF16, st2) for _ in range(2)]
            wub = [C.sb([128, 8, 512], BF16, st2) for _ in range(2)]
            wdb = [C.sb([128, 4, D], BF16, st2) for _ in range(2)]
            dwg = [Dep(), Dep()]; dwu = [Dep(), Dep()]; dwd = [Dep(), Dep()]
            sgt = [C.sb([128, 512], F32, st2) for _ in range(2)]; dsgt = [Dep(), Dep()]
            hid = [C.sb([128, 4, 512], BF16, st2) for _ in range(2)]; dhid = [Dep(), Dep()]
            pg = [C.ps([128, 512], F32, st2) for _ in range(2)]; dpg = [Dep(), Dep()]
            pu = [C.ps([128, 512], F32, st2) for _ in range(2)]; dpu = [Dep(), Dep()]
            py = [C.ps([128, 512], F32, st2) for _ in range(4)]; dpy = [Dep() for _ in range(4)]
            blocks = [(0, 512), (512, 512), (1024, 512), (1536, 512), (2048, 128)]

            def load_w(e):
                b = e % 2
                S.dma("pool", wgb[b][:], I.wg[e].rearrange("(c p) n -> p c n", p=128), writes=[dwg[b]])
                S.dma("pool", wub[b][:], I.wu[e].rearrange("(c p) n -> p c n", p=128), writes=[dwu[b]])
                S.dma("pool", wdb[b][:], I.wd[e].rearrange("(c p) n -> p c n", p=128), writes=[dwd[b]])

            load_w(0)
            k = 0
            ky = 0
            for e_ in range(NEXP):
                b = e_ % 2
                if e_ + 1 < NEXP:
                    load_w(e_ + 1)
                for bi, (t0, tn) in enumerate(blocks):
                    hb = bi % 2
                    tiles = list(range(t0 // 128, (t0 + tn) // 128))
                    hdeps = [dh2T[t] for t in tiles]
                    for j in range(4):
                        kk = k % 2
                        k += 1
                        for c in range(8):
                            S.op("pe", lambda e: e.matmul(pg[kk][:, 0:tn], wgb[b][:, c, j * 128:(j + 1) * 128], h2T[:, c, t0:t0 + tn],
                                                          start=(c == 0), stop=(c == 7)),
                                 reads=[dwg[b]] + hdeps, writes=[dpg[kk]], pe_acc=(c > 0))
                        for c in range(8):
                            S.op("pe", lambda e: e.matmul(pu[kk][:, 0:tn], wub[b][:, c, j * 128:(j + 1) * 128], h2T[:, c, t0:t0 + tn],
                                                          start=(c == 0), stop=(c == 7)),
                                 reads=[dwu[b]] + hdeps, writes=[dpu[kk]], pe_acc=(c > 0))
                        S.op("act", lambda e: e.activation(sgt[kk][:, 0:tn], pg[kk][:, 0:tn], AF.Silu), reads=[dpg[kk]], writes=[dsgt[kk]])
                        S.op("dve", lambda e: e.tensor_tensor(hid[hb][:, j, 0:tn], pu[kk][:, 0:tn], sgt[kk][:, 0:tn], op=ALU.mult),
                             reads=[dpu[kk], dsgt[kk]], writes=[dhid[hb]])
                    for ti, t in enumerate(tiles):
                        for hf in range(2):
                            q = ky % 4
                            ky += 1
                            for j in range(4):
                                S.op("pe", lambda e: e.matmul(py[q][:], hid[hb][:, j, ti * 128:(ti + 1) * 128], wdb[b][:, j, hf * 512:(hf + 1) * 512],
                                                              start=(j == 0), stop=(j == 3)),
                                     reads=[dhid[hb], dwd[b]], writes=[dpy[q]], pe_acc=(j > 0))
                            ysl = yacc[:, t, hf * 512:(hf + 1) * 512]
                            if e_ == 0:
                                S.op("dve", lambda e: e.tensor_scalar(ysl, py[q][:], gates[:, t, e_:e_ + 1], None, op0=ALU.mult),
                                     reads=[dpy[q], dgates[t]], writes=[dyacc[t]])
                            else:
                                S.op("dve", lambda e: e.scalar_tensor_tensor(ysl, py[q][:], gates[:, t, e_:e_ + 1], ysl, op0=ALU.mult, op1=ALU.add),
                                     reads=[dpy[q], dgates[t]], writes=[dyacc[t]])
            S.barrier()

        with ExitStack() as st3:
            x1r = [C.sb([128, D], F32, st3) for _ in range(2)]; dx1r = [Dep(), Dep()]
            xo = [C.sb([128, D], F32, st3) for _ in range(2)]; dxo = [Dep(), Dep()]
            xf = [C.sb([128, D], F32, st3) for _ in range(2)]; dxf = [Dep(), Dep()]
            junk = C.sb([128, D], F32, st3); djunk = Dep()
            sm = C.sb([128, 4], F32, st3); dsm = Dep()
            for v in range(2):
                S.dma("sp", bc[v][:], I.modrow(v, 5).partition_broadcast(128), reads=dmods, writes=[dbc[v]])
            S.dma("sp", bc[2][:], I.fnw.partition_broadcast(128), writes=[dbc[2]])
            for t in range(NT2):
                v = 1 if t == NT2 - 1 else 0
                b2 = t % 2
                tsl = slice(t * 128, (t + 1) * 128)
                S.dma("sp", x1r[b2][:], I.orow(t), reads=[dxout[t]], writes=[dx1r[b2]])
                S.op("dve", lambda e: e.tensor_tensor(yacc[:, t, :], yacc[:, t, :], bc[v][:], op=ALU.mult), reads=[dbc[v]], writes=[dyacc[t]])
                S.op("pool", lambda e: e.tensor_tensor(xo[b2][:], yacc[:, t, :], x1r[b2][:], op=ALU.add), reads=[dyacc[t], dx1r[b2]], writes=[dxo[b2]])
                S.dma("sp", I.orow(t), xo[b2][:], reads=[dxo[b2], dx1r[b2]], writes=[dxout[t]])
                if not do_fin:
                    continue
                S.op("dve", lambda e: e.memset(sm[:], 0.0), writes=[dsm])
                S.op("act", lambda e: e.activation(junk[:], xo[b2][:], AF.Square, accum_out=sm[:, 0:1]), reads=[dxo[b2], dsm], writes=[djunk, dsm])
                rstd_from_ss(S, sm[:, 0:1], sm[:, 1:2], dsm, D, epst[:])
                S.op("dve", lambda e: e.scalar_tensor_tensor(xf[b2][:], xo[b2][:], sm[:, 1:2], bc[2][:], op0=ALU.mult, op1=ALU.mult),
                     reads=[dxo[b2], dsm, dbc[2]], writes=[dxf[b2]])
                S.dma("sp", I.frow(t), xf[b2][:], reads=[dxf[b2]], writes=[dxfin[t]])
            S.barrier()


CAP = 256
NBLK = (2 * 34 * 128 + CAP - 1) // CAP + NEXP
NSLOT = NBLK * CAP
I32 = mybir.dt.int32


def emit_k2s(nc, S, I, dX, dF, dmix, dmods, dxs, dys, do_fin=True):
    NT = NT1
    with ExitStack() as st:
        C = Ctx(nc, st)
        G12 = C.sb([128, NT, 2], F32); dG = [Dep() for _ in range(NT)]
        SL = C.sb([128, NT * 2], I32); dSL = [Dep() for _ in range(NT)]
        cnt = C.sb([128, 32], F32); dcnt = Dep()
        RK = C.sb([128, NT * 2], F32); EK = C.sb([128, NT * 2], F32); dRK = [Dep() for _ in range(NT)]
        H2B = C.sb([128, NT, D], BF16); dH2B = [Dep() for _ in range(NT)]
        pstart = C.sb([128, 32], F32); dps = Dep()
        IDXG = C.sb([128, NBLK], I32); dIDX = Dep()
        bc = [C.sb([128, D], F32) for _ in range(6)]; dbc = [Dep() for _ in range(6)]
        epst = C.sb([128, 1], F32); deps_ = Dep()
        cs2 = C.sb([128, 4, 128], F32); dcs2 = Dep()
        idb = C.sb([128, 128], BF16); didb = Dep()
        S.op("dve", lambda e: e.memset(epst[:], EPS), writes=[deps_])
        S.op("dve", lambda e: e.memset(cnt[:], 0.0), writes=[dcnt])
        S.dma("sp", cs2[:], I.cst2, writes=[dcs2])
        S.dma("pool", idb[:], I.ident, writes=[didb])
        idf = cs2[:, 0, :]; ustr = cs2[:, 1, :]; onesf = cs2[:, 2, :]; iota = cs2[:, 3, 0:32]; wbase = cs2[:, 3, 32:44]

        with ExitStack() as st1:
            woutb = C.sb([128, 8, D], BF16, st1); dwoutb = Dep()
            wrs = C.sb([128, 8, 36], F32, st1); dwrs = Dep()
            tmpw = C.sb([128, D], F32, st1); dtmpw = Dep()
            xt = [C.sb([128, D], F32, st1) for _ in range(2)]; dxt = [Dep(), Dep()]
            mt = [C.sb([128, 8, 128], BF16, st1) for _ in range(2)]; dmt = [Dep(), Dep()]
            tmp = C.sb([128, D], F32, st1); dtmp = Dep()
            x1 = [C.sb([128, D], F32, st1) for _ in range(2)]; dx1 = [Dep(), Dep()]
            h2f = C.sb([128, D], F32, st1); dh2f = Dep()
            h2Tf = C.sb([128, 8, 128], F32, st1); dh2Tf = Dep()
            junk = C.sb([128, D], F32, st1); djunk = Dep()
            sm = C.sb([128, 32], F32, st1); dsm = Dep()
            lg = C.sb([128, 36], F32, st1); dlg = Dep()
            elm = C.sb([128, 32], F32, st1)
            elm2 = C.sb([128, 32], F32, st1)
            mk1 = C.sb([128, 32], F32, st1)
            mk2 = C.sb([128, 32], F32, st1)
            mm_ = C.sb([128, 32], F32, st1)
            pos = C.sb([128, 32], F32, st1)
            t32 = C.sb([128, 32], F32, st1)
            gm = C.sb([128, 8], F32, st1)
            pmix = [C.ps([128, 512], F32, st1) for _ in range(2)]; dpmix = [Dep(), Dep()]
            ptf = C.ps([128, 8, 128], F32, st1); dptf = Dep()
            prt = C.ps([128, 36], F32, st1); dprt = Dep()
            pq = C.ps([128, 64], F32, st1); dpq = Dep()

            S.dma("pool", woutb[:], I.wout.rearrange("(c p) n -> p c n", p=128), writes=[dwoutb])
            S.dma("sp", wrs[:], I.wr.rearrange("(c p) n -> p c n", p=128), writes=[dwrs])
            S.dma("sp", tmpw[:], I.n2w.partition_broadcast(128), writes=[dtmpw])
            for v in range(2):
                S.dma("sp", bc[v][:], I.modrow(v, 2).partition_broadcast(128), reads=dmods, writes=[dbc[v]])
                S.dma("sp", bc[2 + v][:], I.modrow(v, 4).partition_broadcast(128), reads=dmods, writes=[dbc[2 + v]])
                S.dma("sp", bc[4 + v][:], I.modrow(v, 3).partition_broadcast(128), reads=dmods, writes=[dbc[4 + v]])
                S.op("dve", lambda e: e.scalar_tensor_tensor(bc[2 + v][:], bc[2 + v][:], 1.0, tmpw[:], op0=ALU.add, op1=ALU.mult),
                     reads=[dtmpw], writes=[dbc[2 + v]])
            for t in range(NT):
                v = 1 if t >= 32 else 0
                b2 = t % 2
                S.dma("sp", xt[b2][:], I.xrow(t), reads=[dX[t]], writes=[dxt[b2]])
                S.dma("sp", mt[b2][:], I.mtv(t), reads=dmix, writes=[dmt[b2]])
                for hf in range(2):
                    for c in range(8):
                        S.op("pe", lambda e: e.matmul(pmix[hf][:], mt[b2][:, c, :], woutb[:, c, hf * 512:(hf + 1) * 512], start=(c == 0), stop=(c == 7)),
                             reads=[dmt[b2], dwoutb], writes=[dpmix[hf]], pe_acc=(c > 0))
                    S.op("dve", lambda e: e.tensor_tensor(tmp[:, hf * 512:(hf + 1) * 512], pmix[hf][:], bc[v][:, hf * 512:(hf + 1) * 512], op=ALU.mult),
                         reads=[dpmix[hf], dbc[v]], writes=[dtmp])
                S.op("dve", lambda e: e.tensor_tensor(x1[b2][:], tmp[:], xt[b2][:], op=ALU.add), reads=[dtmp, dxt[b2]], writes=[dx1[b2]])
                S.dma("sp", I.orow(t), x1[b2][:], reads=[dx1[b2]], writes=[dX[t]])
                S.op("dve", lambda e: e.memset(sm[:], 0.0), writes=[dsm])
                S.op("act", lambda e: e.activation(junk[:], x1[b2][:], AF.Square, accum_out=sm[:, 0:1]), reads=[dx1[b2], dsm], writes=[djunk, dsm])
                rstd_from_ss(S, sm[:, 0:1], sm[:, 1:2], dsm, D, epst[:])
                S.op("dve", lambda e: e.scalar_tensor_tensor(h2f[:], x1[b2][:], sm[:, 1:2], bc[2 + v][:], op0=ALU.mult, op1=ALU.mult),
                     reads=[dx1[b2], dsm, dbc[2 + v]], writes=[dh2f])
                S.op("dve", lambda e: e.tensor_tensor(h2f[:], h2f[:], bc[4 + v][:], op=ALU.add), reads=[dbc[4 + v]], writes=[dh2f])
                S.op("act", lambda e: e.copy(H2B[:, t, :], h2f[:]), reads=[dh2f], writes=[dH2B[t]])
                for c in range(8):
                    S.op("pe", lambda e: e.transpose(ptf[:, c, :], h2f[:, c * 128:(c + 1) * 128], idf), reads=[dh2f, dcs2], writes=[dptf], pe_acc=(c > 0))
                S.op("act", lambda e: e.copy(h2Tf[:], ptf[:]), reads=[dptf], writes=[dh2Tf])
                for c in range(8):
                    S.op("pe", lambda e: e.matmul(prt[:], h2Tf[:, c, :], wrs[:, c, :], start=(c == 0), stop=(c == 7)),
                         reads=[dh2Tf, dwrs], writes=[dprt], pe_acc=(c > 0))
                S.op("act", lambda e: e.copy(lg[:], prt[:]), reads=[dprt], writes=[dlg])
                R = [dlg, dsm]
                V = lambda f: S.op("dve", f, reads=R, writes=R)
                V(lambda e: e.reduce_max(sm[:, 2:3], lg[:, 0:4], axis=AX.X))
                V(lambda e: e.tensor_scalar(sm[:, 3:4], sm[:, 2:3], -1.0, None, op0=ALU.mult))
                S.op("act", lambda e: e.activation(gm[:, 0:4], lg[:, 0:4], AF.Exp, bias=sm[:, 3:4], scale=1.0, accum_out=sm[:, 4:5]), reads=R, writes=R)
                V(lambda e: e.reciprocal(sm[:, 5:6], sm[:, 4:5]))
                V(lambda e: e.tensor_scalar(gm[:, 4:8], lg[:, 0:4], sm[:, 2:3], None, op0=ALU.is_equal))
                V(lambda e: e.tensor_scalar(gm[:, 4:8], gm[:, 4:8], 1e30, -1e30, op0=ALU.mult, op1=ALU.add))
                for g in range(4):
                    V(lambda e: e.tensor_scalar(elm[:, g * 8:(g + 1) * 8], lg[:, 4 + g * 8:12 + g * 8], gm[:, 4 + g:5 + g], None, op0=ALU.add))
                V(lambda e: e.reduce_max(sm[:, 6:7], elm[:], axis=AX.X))
                V(lambda e: e.tensor_scalar(mk1[:], elm[:], sm[:, 6:7], None, op0=ALU.is_equal))
                V(lambda e: e.scalar_tensor_tensor(elm2[:], mk1[:], -1e30, elm[:], op0=ALU.mult, op1=ALU.add))
                V(lambda e: e.reduce_max(sm[:, 7:8], elm2[:], axis=AX.X))
                V(lambda e: e.tensor_scalar(mk2[:], elm2[:], sm[:, 7:8], None, op0=ALU.is_equal))
                V(lambda e: e.tensor_tensor(sm[:, 8:9], sm[:, 7:8], sm[:, 6:7], op=ALU.subtract))
                S.op("act", lambda e: e.activation(sm[:, 9:10], sm[:, 8:9], AF.Exp), reads=R, writes=R)
                V(lambda e: e.tensor_scalar(sm[:, 10:11], sm[:, 9:10], 1.0, None, op0=ALU.add))
                V(lambda e: e.reciprocal(sm[:, 11:12], sm[:, 10:11]))
                S.op("dve", lambda e: e.tensor_tensor(G12[:, t, 0:1], sm[:, 11:12], sm[:, 5:6], op=ALU.mult), reads=R, writes=R + [dG[t]])
                S.op("dve", lambda e: e.tensor_tensor(G12[:, t, 1:2], G12[:, t, 0:1], sm[:, 9:10], op=ALU.mult), reads=R, writes=R + [dG[t]])
                V(lambda e: e.tensor_tensor(mm_[:], mk1[:], mk2[:], op=ALU.add))
                S.op("pe", lambda e: e.matmul(pq[:, 0:32], ustr, mm_[:], start=True, stop=True), reads=R + [dcs2], writes=[dpq])
                S.op("pe", lambda e: e.matmul(pq[:, 32:64], onesf, mm_[:], start=True, stop=True), reads=R + [dcs2], writes=[dpq], pe_acc=True)
                RC = R + [dcnt]
                S.op("dve", lambda e: e.tensor_tensor(pos[:], pq[:, 0:32], cnt[:], op=ALU.add), reads=[dpq] + RC, writes=RC)
                S.op("dve", lambda e: e.tensor_tensor(cnt[:], pq[:, 32:64], cnt[:], op=ALU.add), reads=[dpq] + RC, writes=RC)
                for k_, mk in enumerate((mk1, mk2)):
                    V(lambda e: e.tensor_tensor(t32[:], mk[:], pos[:], op=ALU.mult))
                    S.op("dve", lambda e: e.reduce_sum(RK[:, 2 * t + k_:2 * t + k_ + 1], t32[:], axis=AX.X), reads=R, writes=R + [dRK[t]])
                    V(lambda e: e.tensor_tensor(t32[:], mk[:], iota, op=ALU.mult))
                    S.op("dve", lambda e: e.reduce_sum(EK[:, 2 * t + k_:2 * t + k_ + 1], t32[:], axis=AX.X), reads=R, writes=R + [dRK[t]])
            pa = C.sb([128, 32], F32, st1); pb = C.sb([128, 32], F32, st1); pc_ = C.sb([128, 32], F32, st1)
            ebk = C.sb([128, NBLK], F32, st1); fi = C.sb([128, 12], F32, st1)
            Z = [dcnt, dps]
            W = lambda f: S.op("dve", f, reads=Z, writes=Z)
            W(lambda e: e.memset(pc_[:], 0.0))
            for m_ in range(2 * NT * 128 // CAP + 1):
                W(lambda e: e.scalar_tensor_tensor(pc_[:], cnt[:], float(m_ * CAP), pc_[:], op0=ALU.is_gt, op1=ALU.add))
            W(lambda e: e.tensor_scalar(pc_[:], pc_[:], float(CAP), None, op0=ALU.mult))
            W(lambda e: e.tensor_copy(pa[:], pc_[:]))
            src, dst = pa, pb
            for sh in (1, 2, 4, 8, 16):
                W(lambda e: e.tensor_copy(dst[:, 0:sh], src[:, 0:sh]))
                W(lambda e: e.tensor_tensor(dst[:, sh:32], src[:, sh:32], src[:, 0:32 - sh], op=ALU.add))
                src, dst = dst, src
            pend = src
            W(lambda e: e.tensor_tensor(pstart[:], pend[:], pc_[:], op=ALU.subtract))
            for b_ in range(NBLK):
                W(lambda e: e.tensor_scalar(t32[:], pend[:], float(b_ * CAP), None, op0=ALU.is_le))
                W(lambda e: e.reduce_sum(ebk[:, b_:b_ + 1], t32[:], axis=AX.X))
            W(lambda e: e.tensor_scalar(ebk[:], ebk[:], float(NEXP - 1), None, op0=ALU.min))
            W(lambda e: e.tensor_scalar(ebk[:], ebk[:], 128.0, float(I.wl * NEXP * 128), op0=ALU.mult, op1=ALU.add))
            W(lambda e: e.tensor_tensor(ebk[:], ebk[:], wbase[:, 0:1].to_broadcast([128, NBLK]), op=ALU.add))
            S.op("dve", lambda e: e.tensor_copy(IDXG[:], ebk[:]), reads=Z, writes=Z + [dIDX])
            for t in range(NT):
                for k_ in range(2):
                    j_ = 2 * t + k_
                    W(lambda e: e.tensor_scalar(t32[:], iota, EK[:, j_:j_ + 1], None, op0=ALU.is_equal))
                    W(lambda e: e.tensor_tensor(t32[:], t32[:], pstart[:], op=ALU.mult))
                    W(lambda e: e.reduce_sum(fi[:, 0:1], t32[:], axis=AX.X))
                    W(lambda e: e.tensor_tensor(fi[:, 0:1], fi[:, 0:1], RK[:, j_:j_ + 1], op=ALU.add))
                    S.op("dve", lambda e: e.tensor_copy(SL[:, j_:j_ + 1], fi[:, 0:1]), reads=Z + [dRK[t]], writes=Z + [dSL[t]])
                    S.idma(I.xs, H2B[:, t, :], SL[:, j_:j_ + 1], False, NSLOT, reads=[dH2B[t], dSL[t]], writes=[dxs])
            S.barrier()

        with ExitStack() as st2:
            wgb = [C.sb([128, 8, 512], BF16, st2) for _ in range(2)]
            wub = [C.sb([128, 8, 512], BF16, st2) for _ in range(2)]
            wdb = [C.sb([128, 4, D], BF16, st2) for _ in range(2)]
            dwg = [Dep(), Dep()]; dwu = [Dep(), Dep()]; dwd = [Dep(), Dep()]
            xr = [C.sb([128, D], BF16, st2) for _ in range(2)]; dxr = [Dep(), Dep()]
            xT = [C.sb([128, 8, CAP], BF16, st2) for _ in range(2)]; dxT = [[Dep() for _ in range(CAP // 128)] for _ in range(2)]
            sgt = [C.sb([128, 512], F32, st2) for _ in range(2)]; dsgt = [Dep(), Dep()]
            hid = [C.sb([128, 4, CAP], BF16, st2) for _ in range(2)]; dhid = [Dep(), Dep()]
            yo = [C.sb([128, D], F32, st2) for _ in range(2)]; dyo = [Dep(), Dep()]
            ptb = C.ps([128, 8, 128], BF16, st2); dptb = Dep()
            pg = [C.ps([128, 512], F32, st2) for _ in range(2)]; dpg = [Dep(), Dep()]
            pu = [C.ps([128, 512], F32, st2) for _ in range(2)]; dpu = [Dep(), Dep()]
            py = [C.ps([128, 512], F32, st2) for _ in range(2)]; dpy = [Dep(), Dep()]

            wgf, wuf, wdf = I.wgf, I.wuf, I.wdf

            def load_w(e):
                b = e % 2
                ix = IDXG[:, e:e + 1]
                S.idma(wgb[b][:].rearrange("p c n -> p (c n)"), wgf, ix, True, 0, reads=[dIDX], writes=[dwg[b]])
                S.idma(wub[b][:].rearrange("p c n -> p (c n)"), wuf, ix, True, 0, reads=[dIDX], writes=[dwu[b]])
                S.idma(wdb[b][:].rearrange("p c n -> p (c n)"), wdf, ix, True, 0, reads=[dIDX], writes=[dwd[b]])

            def load_x(e):
                b = e % 2
                for j in range(CAP // 128):
                    r0 = e * CAP + j * 128
                    xb_ = xr[j % 2]; dxb_ = dxr[j % 2]
                    S.dma("sp", xb_[:], I.xs[r0:r0 + 128, :], reads=[dxs], writes=[dxb_])
                    for c in range(8):
                        S.op("pe", lambda e_: e_.transpose(ptb[:, c, :], xb_[:, c * 128:(c + 1) * 128], idb[:]), reads=[dxb_, didb], writes=[dptb], pe_acc=(c > 0))
                    if j % 2 == 0:
                        S.op("act", lambda e_: e_.copy(xT[b][:, :, j * 128:(j + 1) * 128], ptb[:]), reads=[dptb], writes=[dxT[b][j]])
                    else:
                        S.op("dve", lambda e_: e_.tensor_copy(xT[b][:, :, j * 128:(j + 1) * 128], ptb[:]), reads=[dptb], writes=[dxT[b][j]])

            load_w(0)
            load_x(0)
            k = 0
            ky = 0
            for e_ in range(NBLK):
                b = e_ % 2
                if e_ + 1 < NBLK:
                    load_w(e_ + 1)
                hb = e_ % 2
                for j in range(4):
                    kk = k % 2
                    k += 1
                    for c in range(8):
                        S.op("pe", lambda e: e.matmul(pg[kk][:, 0:CAP], wgb[b][:, c, j * 128:(j + 1) * 128], xT[b][:, c, :], start=(c == 0), stop=(c == 7)),
                             reads=[dwg[b]] + dxT[b], writes=[dpg[kk]], pe_acc=(c > 0))
                    for c in range(8):
                        S.op("pe", lambda e: e.matmul(pu[kk][:, 0:CAP], wub[b][:, c, j * 128:(j + 1) * 128], xT[b][:, c, :], start=(c == 0), stop=(c == 7)),
                             reads=[dwu[b]] + dxT[b], writes=[dpu[kk]], pe_acc=(c > 0))
                    S.op("act", lambda e: e.activation(sgt[kk][:, 0:CAP], pg[kk][:, 0:CAP], AF.Silu), reads=[dpg[kk]], writes=[dsgt[kk]])
                    S.op("dve", lambda e: e.tensor_tensor(hid[hb][:, j, :], pu[kk][:, 0:CAP], sgt[kk][:, 0:CAP], op=ALU.mult),
                         reads=[dpu[kk], dsgt[kk]], writes=[dhid[hb]])
                if e_ + 1 < NBLK:
                    load_x(e_ + 1)
                for ti in range(CAP // 128):
                    yb = yo[ti % 2]; dyb = dyo[ti % 2]
                    for hf in range(2):
                        q = ky % 2
                        ky += 1
                        for j in range(4):
                            S.op("pe", lambda e: e.matmul(py[q][:], hid[hb][:, j, ti * 128:(ti + 1) * 128], wdb[b][:, j, hf * 512:(hf + 1) * 512], start=(j == 0), stop=(j == 3)),
                                 reads=[dhid[hb], dwd[b]], writes=[dpy[q]], pe_acc=(j > 0))
                        if hf == 0:
                            S.op("act", lambda e: e.copy(yb[:, 0:512], py[q][:]), reads=[dpy[q]], writes=[dyb])
                        else:
                            S.op("dve", lambda e: e.tensor_copy(yb[:, 512:1024], py[q][:]), reads=[dpy[q]], writes=[dyb])
                    r0 = e_ * CAP + ti * 128
                    S.dma("sp", I.ys[r0:r0 + 128, :], yb[:], reads=[dyb], writes=[dys])
            S.barrier()

        with ExitStack() as st3:
            x1r = [C.sb([128, D], F32, st3) for _ in range(2)]; dx1r = [Dep(), Dep()]
            o1 = [C.sb([128, D], F32, st3) for _ in range(2)]; do1 = [Dep(), Dep()]
            o2 = [C.sb([128, D], F32, st3) for _ in range(2)]; do2 = [Dep(), Dep()]
            xo = [C.sb([128, D], F32, st3) for _ in range(2)]; dxo = [Dep(), Dep()]
            xf = [C.sb([128, D], F32, st3) for _ in range(2)]; dxf = [Dep(), Dep()]
            junk = C.sb([128, D], F32, st3); djunk = Dep()
            sm = C.sb([128, 4], F32, st3); dsm = Dep()
            for v in range(2):
                S.dma("sp", bc[v][:], I.modrow(v, 5).partition_broadcast(128), reads=dmods, writes=[dbc[v]])
            S.dma("sp", bc[2][:], I.fnw.partition_broadcast(128), writes=[dbc[2]])
            for t in range(NT):
                v = 1 if t >= 32 else 0
                b2 = t % 2
                S.dma("sp", x1r[b2][:], I.orow(t), reads=[dX[t]], writes=[dx1r[b2]])
                S.idma(o1[b2][:], I.ys, SL[:, 2 * t:2 * t + 1], True, NSLOT, reads=[dys, dSL[t]], writes=[do1[b2]])
                S.idma(o2[b2][:], I.ys, SL[:, 2 * t + 1:2 * t + 2], True, NSLOT, reads=[dys, dSL[t]], writes=[do2[b2]])
                S.op("dve", lambda e: e.tensor_scalar(o1[b2][:], o1[b2][:], G12[:, t, 0:1], None, op0=ALU.mult), reads=[dG[t]], writes=[do1[b2]])
                S.op("dve", lambda e: e.scalar_tensor_tensor(o1[b2][:], o2[b2][:], G12[:, t, 1:2], o1[b2][:], op0=ALU.mult, op1=ALU.add),
                     reads=[do2[b2], dG[t]], writes=[do1[b2]])
                S.op("dve", lambda e: e.tensor_tensor(o1[b2][:], o1[b2][:], bc[v][:], op=ALU.mult), reads=[dbc[v]], writes=[do1[b2]])
                S.op("dve", lambda e: e.tensor_tensor(xo[b2][:], o1[b2][:], x1r[b2][:], op=ALU.add), reads=[do1[b2], dx1r[b2]], writes=[dxo[b2]])
                S.dma("sp", I.orow(t), xo[b2][:], reads=[dxo[b2], dx1r[b2]], writes=[dX[t]])
                if not do_fin:
                    continue
                S.op("dve", lambda e: e.memset(sm[:], 0.0), writes=[dsm])
                S.op("act", lambda e: e.activation(junk[:], xo[b2][:], AF.Square, accum_out=sm[:, 0:1]), reads=[dxo[b2], dsm], writes=[djunk, dsm])
                rstd_from_ss(S, sm[:, 0:1], sm[:, 1:2], dsm, D, epst[:])
                S.op("dve", lambda e: e.scalar_tensor_tensor(xf[b2][:], xo[b2][:], sm[:, 1:2], bc[2][:], op0=ALU.mult, op1=ALU.mult),
                     reads=[dxo[b2], dsm, dbc[2]], writes=[dxf[b2]])
                S.dma("sp", I.frow(t), xf[b2][:], reads=[dxf[b2]], writes=[dF[t]])
            S.barrier()


def moe_relayout(w):
    sh = w.shape
    c = sh[-2] // 128
    w5 = w.reshape(-1, c, 128, sh[-1]).transpose(0, 2, 1, 3)
    return np.ascontiguousarray(w5).reshape(-1, c * sh[-1])


def _consts2():
    t_ = np.arange(128)[:, None]
    u_ = np.arange(128)[None, :]
    io = np.zeros((128, 128), np.float32)
    io[:, 0:32] = np.arange(32, dtype=np.float32)[None, :]
    io[:, 32:44] = (np.arange(12) % 8 * 128 + np.where(np.arange(12) < 8, 0, 0))[None, :] + np.arange(128)[:, None]
    io[:, 40:44] = (np.arange(4) * 128)[None, :] + np.arange(128)[:, None]
    return np.stack([np.eye(128, dtype=np.float32), (t_ < u_).astype(np.float32), np.ones((128, 128), np.float32), io], axis=1)


def build_k2s():
    nc = bass.Bass("TRN2", target_bir_lowering=False)
    di = lambda n, s, d=F32: nc.dram_tensor(n, list(s), d, kind="ExternalInput").ap()
    xin = di("xin", [NTOK1, D]); mixT = di("mixT", [D, NTOK1], BF16); wout = di("wout", [D, D]); modr = di("modr", [8, D])
    n2w = di("n2w", [1, D]); fnw = di("fnw", [1, D]); wr = di("wr", [D, 36]); ident_in = di("ident", [128, 128]); cst2 = di("cst2", [128, 4, 128])
    wg = di("wg", [NEXP * 128, 8 * 512]); wu = di("wu", [NEXP * 128, 8 * 512]); wd = di("wd", [NEXP * 128, 4 * D])
    xout = nc.dram_tensor("xout", [NTOK1, D], F32, kind="ExternalOutput").ap()
    xfin = nc.dram_tensor("xfin", [NTOK1, D], F32, kind="ExternalOutput").ap()
    xs = nc.dram_tensor("xs", [NSLOT, D], BF16).ap()
    ys = nc.dram_tensor("ys", [NSLOT, D], F32).ap()
    with ExitStack() as st:
        S = Sched(nc, st)
        dX = [Dep() for _ in range(NT1)]
        dF = [Dep() for _ in range(NT1)]
        for t in range(NT1):
            S.dma("sp", xout[t * 128:(t + 1) * 128, :], xin[t * 128:(t + 1) * 128, :], writes=[dX[t]])
        I = type("I", (), dict(
            xrow=staticmethod(lambda t: xout[t * 128:(t + 1) * 128, :]), orow=staticmethod(lambda t: xout[t * 128:(t + 1) * 128, :]),
            frow=staticmethod(lambda t: xfin[t * 128:(t + 1) * 128, :]),
            mtv=staticmethod(lambda t: mixT.rearrange("(c p) t -> p c t", p=128)[:, :, t * 128:(t + 1) * 128]),
            modrow=staticmethod(lambda v, k: modr[4 * v + k - 2:4 * v + k - 1, :]),
            wout=wout, n2w=n2w, fnw=fnw, wr=wr, ident=ident_in, cst2=cst2, wl=0, wgf=wg, wuf=wu, wdf=wd, xs=xs, ys=ys))
        emit_k2s(nc, S, I, dX, dF, [], [], Dep(), Dep())
        S.finish(dX + dF)
        print("K2s instrs", S.n_ins, "waits", S.n_wait, "dma sems", S.ndsem, "sems", len(S.sems))
    return nc


def build_k2():
    nc = bass.Bass("TRN2", target_bir_lowering=False)
    di = lambda n, s, d=F32: nc.dram_tensor(n, list(s), d, kind="ExternalInput").ap()
    xin = di("xin", [NTOK2, D])
    mixT = di("mixT", [D, NTOK2], BF16)
    wout = di("wout", [D, D])
    modr = di("modr", [8, D])
    n2w = di("n2w", [1, D])
    fnw = di("fnw", [1, D])
    wr = di("wr", [D, 36])
    ident_in = di("ident", [128, 128])
    wg = di("wg", [NEXP, D, 512])
    wu = di("wu", [NEXP, D, 512])
    wd = di("wd", [NEXP, 512, D])
    xout = nc.dram_tensor("xout", [NTOK2, D], F32, kind="ExternalOutput").ap()
    xfin = nc.dram_tensor("xfin", [NTOK2, D], F32, kind="ExternalOutput").ap()

    with ExitStack() as st:
        S = Sched(nc, st)
        I = type("I", (), dict(
            xrow=staticmethod(lambda t: xin[t * 128:(t + 1) * 128, :]), orow=staticmethod(lambda t: xout[t * 128:(t + 1) * 128, :]),
            frow=staticmethod(lambda t: xfin[t * 128:(t + 1) * 128, :]),
            mtv=staticmethod(lambda t: mixT.rearrange("(c p) t -> p c t", p=128)[:, :, t * 128:(t + 1) * 128]),
            modrow=staticmethod(lambda v, k: modr[4 * v + k - 2:4 * v + k - 1, :]),
            wout=wout, n2w=n2w, fnw=fnw, wr=wr, ident=ident_in, wg=wg, wu=wu, wd=wd))
        dX = [Dep() for _ in range(NT2)]
        dF = [Dep() for _ in range(NT2)]
        emit_k2(nc, S, I, dX, dF, [], [])
        S.finish(dX + dF)
        print("K2 instrs", S.n_ins, "waits", S.n_wait, "dma sems", S.ndsem, "sems", len(S.sems))
    return nc


NT1 = 34
NTOK1 = NT1 * 128
TPAD = 4360
FM = [("qA", 128), ("qAp", 128), ("kA", 128), ("kAp", 128),
      ("qC0", 64), ("qC0p", 64), ("qC1", 64), ("qC1p", 64),
      ("kC0", 64), ("kC0p", 64), ("kC1", 64), ("kC1p", 64),
      ("xs0", 128), ("xs1", 128), ("bm", 64), ("cm", 64)]
FMOFF = {}
_o = 0
for _n, _m in FM:
    FMOFF[_n] = (_o, _m)
    _o += _m
NFM = _o
NTM = 456
NCOL1 = NFM + NTM


def xcol(t):
    return t + 2 if t < 4096 else t + 6


def ucol(t):
    return t if t < 4096 else t + 4


def emit_k1(nc, S, I, dX, dmix, dmods, hc=None):
    hmode = hc[0] if hc else None
    xin_unused = None
    convw, convb, dtb, alog, d0b, d1b, nwb, mixo, zs = I.convw, I.convb, I.dtb, I.alog, I.d0b, I.d1b, I.nwb, I.mixo, I.zs
    with ExitStack() as st:
        C = Ctx(nc, st)
        QA = C.sb([128, NTOK1], BF16); KA = C.sb([128, NTOK1], BF16)
        QC = [C.sb([64, NTOK1], BF16) for _ in range(2)]
        KC = [C.sb([64, NTOK1], BF16) for _ in range(2)]
        dQA = [Dep() for _ in range(9)]; dKA = [Dep() for _ in range(9)]
        dQC = [[Dep() for _ in range(9)] for _ in range(2)]; dKC = [[Dep() for _ in range(9)] for _ in range(2)]
        VA = C.sb([128, NT1, 128], BF16); dVA = [Dep() for _ in range(NT1)]
        VC = C.sb([128, NT1, 256], BF16); dVC = [Dep() for _ in range(NT1)]
        DT = C.sb([128, NT1, 8], F32); dDT = Dep()
        DTS = C.sb([128, NT1, 8], F32); DTA = C.sb([128, NT1, 8], F32); AN = C.sb([128, NT1 * 8], F32)
        XP = C.sb([128, 4, TPAD], BF16); dXP = [Dep() for _ in range(4)]
        cs = C.sb([128, 6, 128], F32); dcs = Dep()
        csb = C.sb([128, 6, 128], BF16); dcsb = Dep()
        epst = C.sb([128, 1], F32); dmisc = Dep()
        es = C.sb([128, 2], F32)
        lam = C.sb([128, 8], F32)
        cl = C.sb([128, 128], F32)
        wsc = C.sb([64, 1], F32)
        dzs = [Dep() for _ in range(NT1)]
        identf = cs[:, 0, :]; Ud = [cs[:, 1, :], cs[:, 2, :]]; onesf = cs[:, 3, :]; NEGd = [cs[:, 4, :], cs[:, 5, :]]
        identb = csb[:, 0, :]
        negprev = csb[:, 5, :]
        negnext = csb[:, 4, :]

        S.dma("sp", cs[:], I.cst, writes=[dcs])
        S.dma("pool", csb[:], I.cst, writes=[dcsb])
        S.op("dve", lambda e: e.memset(epst[:], EPS), writes=[dmisc])
        S.dma("sp", es[:], I.sink, writes=[dmisc])
        S.op("act", lambda e: e.activation(es[:], es[:], AF.Exp), reads=[dmisc], writes=[dmisc])
        S.dma("sp", cl[:], I.clam, writes=[dmisc])
        S.dma("sp", lam[:, 0:2], I.lconst, writes=[dmisc])
        S.dma("sp", wsc[:], I.subw, writes=[dmisc])
        M_ = [dmisc]
        S.op("dve", lambda e: e.tensor_tensor(cl[:, 0:32], cl[:, 0:32], cl[:, 32:64], op=ALU.mult), reads=M_, writes=M_)
        S.op("dve", lambda e: e.tensor_tensor(cl[:, 64:96], cl[:, 64:96], cl[:, 96:128], op=ALU.mult), reads=M_, writes=M_)
        S.op("dve", lambda e: e.reduce_sum(lam[:, 2:3], cl[:, 0:32], axis=AX.X), reads=M_, writes=M_)
        S.op("dve", lambda e: e.reduce_sum(lam[:, 3:4], cl[:, 64:96], axis=AX.X), reads=M_, writes=M_)
        S.op("act", lambda e: e.activation(lam[:, 2:4], lam[:, 2:4], AF.Exp), reads=M_, writes=M_)
        S.op("dve", lambda e: e.tensor_tensor(lam[:, 4:5], lam[:, 3:4], lam[:, 2:3], op=ALU.subtract), reads=M_, writes=M_)
        S.op("dve", lambda e: e.tensor_tensor(lam[:, 5:6], lam[:, 4:5], lam[:, 0:1], op=ALU.subtract), reads=M_, writes=M_)
        S.op("dve", lambda e: e.tensor_tensor(wsc[:], wsc[:], lam[0:64, 1:2], op=ALU.mult), reads=M_, writes=M_)
        nlam = lam[0:64, 5:6]
        S.op("pool", lambda e: e.memset(VA[:, :, 64:128], 1.0), writes=dVA)
        S.op("pool", lambda e: e.memset(VC[:, :, 64:128], 1.0), writes=dVC)
        S.op("pool", lambda e: e.memset(VC[:, :, 192:256], 1.0), writes=dVC)
        for s_ in range(4):
            S.op("pool", lambda e: e.memset(XP[:, s_, :], 0.0), writes=[dXP[s_]])

        with ExitStack() as st1:
            w1b = C.sb([128, 8, NCOL1], BF16, st1); dw1 = Dep()
            tmpw = C.sb([128, D], F32, st1); dtmpw = Dep()
            Abc = C.sb([128, D], F32, st1); Bbc = C.sb([128, D], F32, st1); dAbc = Dep(); dBbc = Dep()
            xt = [C.sb([128, D], F32, st1) for _ in range(2)]; dxt = [Dep(), Dep()]
            hb = C.sb([128, D], BF16, st1); dhb = Dep()
            sm = C.sb([128, 4], F32, st1); dsm = Dep()
            hT = [C.sb([128, 8, 512], BF16, st1) for _ in range(2)]; dhT = [[Dep() for _ in range(4)] for _ in range(2)]
            tA = C.sb([128, 2, 512], F32, st1); dtA = Dep()
            tC = C.sb([64, 2, 512], F32, st1); dtC = Dep()
            r1 = [C.sb([128, 512], F32, st1)] * 2; r2 = [C.sb([128, 512], F32, st1)] * 2
            dr1 = [Dep()] * 2; dr2 = [Dep()] * 2
            zt = [C.sb([128, 256], BF16, st1) for _ in range(2)]; dzt = [Dep(), Dep()]
            ptb = C.ps([128, 8, 128], BF16, st1); dptb = Dep()
            pf = [C.ps([128, 512], F32, st1) for _ in range(4)]; dpf = [Dep() for _ in range(4)]
            ptm = [C.ps([128, 512], F32, st1) for _ in range(2)]; dptm = [Dep(), Dep()]

            S.dma("pool", w1b[:, 0:4, :], I.w1.rearrange("(c p) n -> p c n", p=128)[:, 0:4, :], writes=[dw1])
            S.dma("pool", w1b[:, 4:8, :], I.w1.rearrange("(c p) n -> p c n", p=128)[:, 4:8, :], writes=[dw1])
            S.dma("sp", tmpw[:], I.n1w.partition_broadcast(128), writes=[dtmpw])

            def load_mod(v):
                S.dma("sp", Abc[:], I.modrow(v, 1).partition_broadcast(128), reads=dmods, writes=[dAbc])
                S.dma("sp", Bbc[:], I.modrow(v, 0).partition_broadcast(128), reads=dmods, writes=[dBbc])
                S.op("dve", lambda e: e.scalar_tensor_tensor(Abc[:], Abc[:], 1.0, tmpw[:], op0=ALU.add, op1=ALU.mult), reads=[dtmpw], writes=[dAbc])

            load_mod(0)
            kf = 0
            import os
            STG = int(os.environ.get('K1_STG', '9'))
            for blk in range(9):
                if blk >= int(os.environ.get('K1_BLK', '9')):
                    break
                ctxb = blk == 8
                nt = 2 if ctxb else 4
                ntok = nt * 128
                t0 = blk * 4
                tok0 = t0 * 128
                hb_i = blk % 2
                if ctxb:
                    load_mod(1)
                else:
                    S.dma(os.environ.get("K1_RQ", "sp"), tA[:], I.ropeA[:, :, tok0:tok0 + 512].rearrange("a p t -> p a t"), writes=[dtA])
                    S.dma(os.environ.get("K1_RQ", "sp"), tC[:], I.ropeC[:, :, tok0:tok0 + 512].rearrange("a p t -> p a t"), writes=[dtC])
                for j in range(nt):
                    t = t0 + j
                    b2 = t % 2
                    if hmode == "read":
                        S.dma("sp", hT[hb_i][:, :, j * 128:(j + 1) * 128], hc[1][t].rearrange("p (c k) -> p c k", c=8), reads=[hc[2][t]], writes=[dhT[hb_i][j]])
                    else:
                      S.dma("sp", xt[b2][:], I.xrow(t), reads=[dX[t]], writes=[dxt[b2]])
                    if hmode == "read":
                        pass
                    elif True:
                        S.op("dve", lambda e: e.memset(sm[:], 0.0), writes=[dsm])
                        S.op("act", lambda e: e.activation(hb[:], xt[b2][:], AF.Square, accum_out=sm[:, 0:1]), reads=[dxt[b2], dsm], writes=[dhb, dsm])
                        rstd_from_ss(S, sm[:, 0:1], sm[:, 1:2], dsm, D, epst[:])
                        S.op("dve", lambda e: e.scalar_tensor_tensor(xt[b2][:], xt[b2][:], sm[:, 1:2], Abc[:], op0=ALU.mult, op1=ALU.mult),
                             reads=[dsm, dAbc], writes=[dxt[b2]])
                        S.op("dve", lambda e: e.tensor_tensor(hb[:], xt[b2][:], Bbc[:], op=ALU.add), reads=[dxt[b2], dBbc], writes=[dhb])
                        for c in range(8):
                            S.op("pe", lambda e: e.transpose(ptb[:, c, :], hb[:, c * 128:(c + 1) * 128], identb), reads=[dhb, dcsb], writes=[dptb], pe_acc=(c > 0))
                        S.op("act", lambda e: e.copy(hT[hb_i][:, :, j * 128:(j + 1) * 128], ptb[:]), reads=[dptb], writes=[dhT[hb_i][j]])

                        if hmode == "write":
                            S.dma("sp", hc[1][t].rearrange("p (c k) -> p c k", c=8), hT[hb_i][:, :, j * 128:(j + 1) * 128], reads=[dhT[hb_i][j]], writes=[hc[2][t]])
                    if STG < 2:
                        continue
                    pt_ = ptm[t % 2]; dpt_ = dptm[t % 2]
                    for c in range(8):
                        S.op("pe", lambda e: e.matmul(pt_[:, 0:NTM], hT[hb_i][:, c, j * 128:(j + 1) * 128], w1b[:, c, NFM:NCOL1], start=(c == 0), stop=(c == 7)),
                             reads=[dhT[hb_i][j], dw1], writes=[dpt_], pe_acc=(c > 0))
                    SB2 = int(os.environ.get('K1_SB2', '9'))
                    if SB2 >= 2:
                        S.op("act", lambda e: e.copy(VA[:, t, 0:64], pt_[:, 0:64]), reads=[dpt_], writes=[dVA[t]])
                    if SB2 >= 3:
                        for hh in range(2):
                            S.op("act", lambda e: e.copy(VC[:, t, hh * 128:hh * 128 + 64], pt_[:, 64 + hh * 64:128 + hh * 64]), reads=[dpt_], writes=[dVC[t]])
                    if SB2 >= 4:
                        S.op("act", lambda e: e.copy(zt[t % 2][:], pt_[:, 192:448]), reads=[dpt_], writes=[dzt[t % 2]])
                        S.dma("sp", zs[t * 128:(t + 1) * 128, :], zt[t % 2][:], reads=[dzt[t % 2]], writes=[dzs[t]])
                    if SB2 >= 5:
                        S.op("act", lambda e: e.copy(DT[:, t, :], pt_[:, 448:456]), reads=[dpt_], writes=[dDT])
                hdeps = dhT[hb_i][0:nt]

                def fm_group(name, pidx):
                    off, m = FMOFF[name]
                    p = pf[pidx]
                    for c in range(8):
                        S.op("pe", lambda e: e.matmul(p[0:m, 0:ntok], w1b[:, c, off:off + m], hT[hb_i][:, c, 0:ntok], start=(c == 0), stop=(c == 7)),
                             reads=hdeps + [dw1], writes=[dpf[pidx]], pe_acc=(c > 0))
                    return p

                def rope_pair(name, dst, ddst, tab, dtab, m):
                    nonlocal kf
                    i0 = (kf % 2) * 2
                    kf += 1
                    p = fm_group(name, i0)
                    if ctxb or os.environ.get('K1_NOROPE'):
                        S.op("act", lambda e: e.copy(dst[0:m, tok0:tok0 + ntok], p[0:m, 0:ntok]), reads=[dpf[i0]], writes=[ddst])
                        return
                    pp = fm_group(name + "p", i0 + 1)
                    rr = (kf % 2)
                    S.op("dve", lambda e: e.tensor_tensor(r1[rr][0:m, :], p[0:m, :], tab[0:m, 0, :], op=ALU.mult), reads=[dpf[i0], dtab], writes=[dr1[rr]])
                    S.op("dve", lambda e: e.tensor_tensor(r2[rr][0:m, :], pp[0:m, :], tab[0:m, 1, :], op=ALU.mult), reads=[dpf[i0 + 1], dtab], writes=[dr2[rr]])
                    S.op("dve", lambda e: e.tensor_tensor(dst[0:m, tok0:tok0 + ntok], r1[rr][0:m, :], r2[rr][0:m, :], op=ALU.add),
                         reads=[dr1[rr], dr2[rr]], writes=[ddst])

                if STG < 3:
                    continue
                pairs = [("qA", QA, dQA[blk], tA, dtA, 128), ("kA", KA, dKA[blk], tA, dtA, 128)]
                for hh in range(2):
                    pairs.append(("qC%d" % hh, QC[hh], dQC[hh][blk], tC, dtC, 64))
                    pairs.append(("kC%d" % hh, KC[hh], dKC[hh][blk], tC, dtC, 64))
                for pi_, pr_ in enumerate(pairs):
                    if pi_ < int(os.environ.get('K1_NP', '9')):
                        rope_pair(*pr_)
                if STG < 4:
                    continue
                c0 = xcol(tok0)
                for si, name in enumerate(["xs0", "xs1", "bm", "cm"]):
                    i0 = kf % 4
                    kf += 1
                    m = FMOFF[name][1]
                    p = fm_group(name, i0)
                    if si % 2 == 0:
                        S.op("act", lambda e: e.copy(XP[0:m, si, c0:c0 + ntok], p[0:m, 0:ntok]), reads=[dpf[i0]], writes=[dXP[si]])
                    else:
                        S.op("dve", lambda e: e.tensor_copy(XP[0:m, si, c0:c0 + ntok], p[0:m, 0:ntok]), reads=[dpf[i0]], writes=[dXP[si]])
            S.barrier()
        print("K1 after phase1: instrs", S.n_ins, "waits", S.n_wait)
        import os
        if int(os.environ.get("K1_UPTO", "9")) >= 2:
            _k1_rest(nc, S, C, locals())


def build_k1():
    nc = bass.Bass("TRN2", target_bir_lowering=False)
    di = lambda n, s, d=F32: nc.dram_tensor(n, list(s), d, kind="ExternalInput").ap()
    xin = di("xin", [NTOK1, D])
    modv = di("modv", [4, D])
    n1w = di("n1w", [1, D])
    w1 = di("w1", [D, NCOL1])
    convw = di("convw", [128, 20])
    convb = di("convb", [128, 4])
    dtb = di("dtb", [128, NT1 * 8])
    alog = di("alog", [128, NT1 * 8])
    d0b = di("d0b", [128, 256])
    d1b = di("d1b", [128, 256])
    nwb = di("nwb", [128, 256])
    sink = di("sink", [128, 2])
    clam = di("clam", [128, 128])
    lconst = di("lconst", [128, 2])
    subw = di("subw", [64, 1])
    cst = di("cst", [128, 6, 128])
    ropeA = di("ropeA", [2, 128, 4096])
    ropeC = di("ropeC", [2, 64, 4096])
    mixo = nc.dram_tensor("mixo", [512, NTOK1], BF16, kind="ExternalOutput").ap()
    zs = nc.dram_tensor("zs", [NTOK1, 256], BF16, kind="ExternalOutput").ap()

    with ExitStack() as st:
        S = Sched(nc, st)
        I = type("I", (), dict(
            xrow=staticmethod(lambda t: xin[t * 128:(t + 1) * 128, :]), modrow=staticmethod(lambda v, k: modv[2 * v + k:2 * v + k + 1, :]),
            n1w=n1w, w1=w1, convw=convw, convb=convb, dtb=dtb, alog=alog, d0b=d0b, d1b=d1b, nwb=nwb, sink=sink, clam=clam, lconst=lconst,
            subw=subw, cst=cst, ropeA=ropeA, ropeC=ropeC, mixo=mixo, zs=zs))
        dmix = Dep()
        emit_k1(nc, S, I, [Dep() for _ in range(NT1)], dmix, [])
        S.finish([dmix])
        print("K1 instrs", S.n_ins, "waits", S.n_wait, "dma sems", S.ndsem, "sems", len(S.sems))
    return nc


def _k1_rest(nc, S, C, L):
    V = type("V", (), L)
    QA, KA, QC, KC, VA, VC, XP = V.QA, V.KA, V.QC, V.KC, V.VA, V.VC, V.XP
    dQA, dKA, dQC, dKC, dVA, dVC, dXP = V.dQA, V.dKA, V.dQC, V.dKC, V.dVA, V.dVC, V.dXP
    DT, DTS, DTA, AN, dDT = V.DT, V.DTS, V.DTA, V.AN, V.dDT
    cs, dcs, csb, dcsb, dmisc = V.cs, V.dcs, V.csb, V.dcsb, V.dmisc
    identf, Ud, onesf, NEGd, identb, negprev, negnext = V.identf, V.Ud, V.onesf, V.NEGd, V.identb, V.negprev, V.negnext
    es, wsc, nlam, epst, mixo, zs, dzs, dmix = V.es, V.wsc, V.nlam, V.epst, V.mixo, V.zs, V.dzs, V.dmix
    allQA = list(dQA); allKA = list(dKA)
    blk_of = lambda tok: min(tok // 512, 8)

    with ExitStack() as sa:
        E = [C.sb([128, 5, 2, 128], BF16, sa) for _ in range(2)]; dE = [Dep(), Dep()]
        rd = [C.sb([64, 128], F32, sa) for _ in range(2)]; drd = [Dep(), Dep()]
        oa = [C.sb([64, 128], BF16, sa) for _ in range(4)]; doa = [Dep() for _ in range(4)]
        pss = [C.ps([128, 5, 128], F32, sa) for _ in range(2)]; dpss = [Dep(), Dep()]
        po = [C.ps([128, 2, 128], F32, sa) for _ in range(2)]; dpo = [Dep(), Dep()]
        ko = 0
        for qb in range(NT1):
            if qb < 32:
                kts = []
                if qb > 0:
                    kts.append((qb - 1, negprev))
                kts.append((qb, None))
                if qb < 31:
                    kts.append((qb + 1, negnext))
                kts += [(32, None), (33, None)]
            else:
                kts = [(32, None), (33, None)]
            nk = len(kts)
            qsl = slice(qb * 128, (qb + 1) * 128)
            eb = qb % 2
            for hh in range(2):
                hs = slice(hh * 64, (hh + 1) * 64)
                p = pss[hh]
                for i, (kt, msk) in enumerate(kts):
                    S.op("pe", lambda e: e.matmul(p[:, i, :], KA[hs, kt * 128:(kt + 1) * 128], QA[hs, qsl], start=True, stop=(msk is None)),
                         reads=[dKA[blk_of(kt * 128)], dQA[blk_of(qb * 128)]], writes=[dpss[hh]], pe_acc=(i > 0))
                    if msk is not None:
                        S.op("pe", lambda e: e.matmul(p[:, i, :], identb, msk, start=False, stop=True), reads=[dcsb], writes=[dpss[hh]], pe_acc=True)
                S.op("act", lambda e: e.activation(E[eb][:, 0:nk, hh, :], p[:, 0:nk, :], AF.Exp, scale=0.125), reads=[dpss[hh]], writes=[dE[eb]])
            pv = po[qb % 2]; dpv = dpo[qb % 2]
            for i, (kt, msk) in enumerate(kts):
                S.op("pe", lambda e: e.matmul(pv[:], VA[:, kt, :], E[eb][:, i, :, :], start=(i == 0), stop=(i == nk - 1)),
                     reads=[dVA[kt], dE[eb]], writes=[dpv], pe_acc=(i > 0))
            for hh in range(2):
                r_ = rd[hh]; o_ = oa[ko % 4]; do_ = doa[ko % 4]
                ko += 1
                S.op("dve", lambda e: e.tensor_scalar(r_[:], pv[64:128, hh, :], es[64:128, hh:hh + 1], None, op0=ALU.add), reads=[dpv, dmisc], writes=[drd[hh]])
                S.op("dve", lambda e: e.reciprocal(r_[:], r_[:]), reads=[drd[hh]], writes=[drd[hh]])
                S.op("dve", lambda e: e.tensor_tensor(o_[:], pv[0:64, hh, :], r_[:], op=ALU.mult), reads=[dpv, drd[hh]], writes=[do_])
                S.dma("sp", mixo[hh * 64:(hh + 1) * 64, qsl], o_[:], reads=[do_], writes=[dmix])
        S.barrier()
    print("K1 after A: instrs", S.n_ins, "waits", S.n_wait)
    import os
    if int(os.environ.get("K1_UPTO", "9")) < 3:
        return

    with ExitStack() as sc:
        Et = [C.sb([128, 512], BF16, sc) for _ in range(4)]; dEt = [Dep() for _ in range(4)]
        f32t = lambda: C.sb([64, 512], F32, sc)
        rd0, rd1, t0_, t1_, o_, sq_, rs_ = [f32t() for _ in range(7)]
        dfin = Dep()
        oc = [C.sb([64, 512], BF16, sc) for _ in range(2)]; doc = [Dep(), Dep()]
        ones64 = cs[0:64, 3, 0:64]
        acc = [[C.ps([128, 512], F32, sc) for _ in range(2)] for _ in range(2)]
        dacc = [[Dep(), Dep()] for _ in range(2)]
        psc = [C.ps([128, 512], F32, sc) for _ in range(4)]; dpsc = [Dep() for _ in range(4)]
        kk = 0
        qblocks = [(i * 512, 512, list(range(NT1))) for i in range(8)] + [(4096, 256, [32, 33])]
        LOOK = 3
        for (q0, qn, kts) in qblocks:
            qblk = blk_of(q0)
            items = [(ki, kt, hh, m) for ki, kt in enumerate(kts) for hh in range(2) for m in range(2)]
            bufs = {}

            def qk_exp(i):
                nonlocal kk
                ki, kt, hh, m = items[i]
                ms = slice(m * 32, (m + 1) * 32)
                bi = kk % 4
                kk += 1
                bufs[i] = bi
                p = psc[bi]; dp = dpsc[bi]; e_ = Et[bi]; de_ = dEt[bi]
                S.op("pe", lambda e: e.matmul(p[:, 0:qn], KC[hh][ms, kt * 128:(kt + 1) * 128], QC[hh][ms, q0:q0 + qn], start=True, stop=True),
                     reads=[dKC[hh][blk_of(kt * 128)], dQC[hh][qblk]], writes=[dp])
                S.op("act", lambda e: e.activation(e_[:, 0:qn], p[:, 0:qn], AF.Exp, scale=32 ** -0.5), reads=[dp], writes=[de_])

            def pv(i):
                ki, kt, hh, m = items[i]
                bi = bufs[i]
                e_ = Et[bi]; de_ = dEt[bi]
                S.op("pe", lambda e: e.matmul(acc[hh][m][:, 0:qn], VC[:, kt, hh * 128:(hh + 1) * 128], e_[:, 0:qn], start=(ki == 0), stop=(ki == len(kts) - 1)),
                     reads=[dVC[kt], de_], writes=[dacc[hh][m]], pe_acc=(ki > 0))

            for i in range(len(items) + LOOK):
                if i < len(items):
                    qk_exp(i)
                if i - LOOK >= 0:
                    pv(i - LOOK)
            for hh in range(2):
                F = [dfin]
                a0 = acc[hh][0]; a1 = acc[hh][1]
                S.op("dve", lambda e: e.reciprocal(rd0[:, 0:qn], a0[64:128, 0:qn]), reads=[dacc[hh][0]] + F, writes=F)
                S.op("dve", lambda e: e.reciprocal(rd1[:, 0:qn], a1[64:128, 0:qn]), reads=[dacc[hh][1]] + F, writes=F)
                S.op("dve", lambda e: e.tensor_tensor(t0_[:, 0:qn], a0[0:64, 0:qn], rd0[:, 0:qn], op=ALU.mult), reads=[dacc[hh][0]] + F, writes=F)
                S.op("dve", lambda e: e.tensor_tensor(t1_[:, 0:qn], a1[0:64, 0:qn], rd1[:, 0:qn], op=ALU.mult), reads=[dacc[hh][1]] + F, writes=F)
                S.op("dve", lambda e: e.scalar_tensor_tensor(o_[:, 0:qn], t1_[:, 0:qn], nlam, t0_[:, 0:qn], op0=ALU.mult, op1=ALU.add), reads=F + [dmisc], writes=F)
                S.op("pool", lambda e: e.tensor_tensor(sq_[:, 0:qn], o_[:, 0:qn], o_[:, 0:qn], op=ALU.mult), reads=F, writes=F)
                p = psc[kk % 4]; dp = dpsc[kk % 4]
                kk += 1
                S.op("pe", lambda e: e.matmul(p[0:64, 0:qn], ones64, sq_[:, 0:qn], start=True, stop=True), reads=F + [dcs], writes=[dp])
                S.op("act", lambda e: e.activation(rs_[:, 0:qn], p[0:64, 0:qn], AF.Ln, bias=epst[0:64, :], scale=1.0 / 64), reads=[dp, dmisc] + F, writes=F)
                S.op("act", lambda e: e.activation(rs_[:, 0:qn], rs_[:, 0:qn], AF.Exp, scale=-0.5), reads=F, writes=F)
                ob = oc[hh]
                S.op("dve", lambda e: e.scalar_tensor_tensor(ob[:, 0:qn], o_[:, 0:qn], wsc[:, 0:1], rs_[:, 0:qn], op0=ALU.mult, op1=ALU.mult),
                     reads=F + [dmisc], writes=[doc[hh]])
                S.dma("sp", mixo[384 + hh * 64:384 + (hh + 1) * 64, q0:q0 + qn], ob[:, 0:qn], reads=[doc[hh]], writes=[dmix])
        S.barrier()
    print("K1 after C: instrs", S.n_ins, "waits", S.n_wait)
    if int(os.environ.get("K1_UPTO", "9")) < 4:
        return

    with ExitStack() as sb_:
        cw = C.sb([128, 20], F32, sb_); cb = C.sb([128, 4], F32, sb_); dcw = Dep()
        ctmp = [C.sb([128, 1024], F32, sb_)] * 2; dctmp = [Dep()] * 2
        XTM = C.sb([128, NT1, 320], BF16, sb_); dXTM = [Dep() for _ in range(NT1)]
        Y = C.sb([128, NT1, 256], F32, sb_); dY = [Dep() for _ in range(NT1)]
        dtbt = C.sb([128, NT1 * 8], F32, sb_)
        dsum = C.sb([128, 256], F32, sb_); d1t = C.sb([128, 256], F32, sb_); nwt = C.sb([128, 256], F32, sb_); dpar = Dep()
        H = C.sb([64, 256], F32, sb_); Hb = C.sb([64, 256], BF16, sb_); dH = Dep(); dHb = Dep()
        sm = [C.sb([128, 24], F32, sb_) for _ in range(2)]; dsmm = [Dep(), Dep()]
        xdt = [C.sb([128, 4, 64], BF16, sb_) for _ in range(2)]; dxdt = [Dep(), Dep()]
        xdw = [C.sb([128, 4, 64], BF16, sb_) for _ in range(2)]; dxdw = [Dep(), Dep()]
        GT = [C.sb([128, 128], F32, sb_) for _ in range(2)]; dGT = [Dep(), Dep()]
        lrep4 = C.sb([128, 4, 128], F32, sb_); dlrep4 = Dep()
        X4 = C.sb([128, 4, 128], F32, sb_); dX4 = Dep()
        dec4 = C.sb([128, 4, 128], F32, sb_); ddec4 = Dep()
        MT4 = C.sb([128, 4, 128], BF16, sb_); dMT4 = Dep()
        tmpy = C.sb([128, 4, 64], F32, sb_); dtmpy = Dep()
        NDTA = C.sb([128, NT1, 8], F32, sb_)
        zr = [C.sb([128, 256], BF16, sb_) for _ in range(2)]; dzr = [Dep(), Dep()]
        f1 = C.sb([128, 256], F32, sb_); f2 = C.sb([128, 256], F32, sb_); f3 = C.sb([128, 256], F32, sb_); dff = Dep()
        fjunk = C.sb([128, 256], F32, sb_)
        fs = C.sb([128, 4], F32, sb_)
        obt = C.sb([128, 256], BF16, sb_); dobt = Dep()
        obT = [C.sb([128, 2, 128], BF16, sb_) for _ in range(2)]; dobT = [Dep(), Dep()]
        pc = C.ps([128, 8], F32, sb_); dpc = Dep()
        pgt = C.ps([128, 128], F32, sb_); dpgt = Dep()
        pd4 = C.ps([128, 4, 128], F32, sb_); dpd4 = Dep()
        pyd = C.ps([128, 4, 64], F32, sb_); dpyd = Dep()
        pyo = C.ps([128, 256], F32, sb_); dpyo = Dep()
        pst = C.ps([64, 256], F32, sb_); dpst = Dep()
        ptr = C.ps([128, 512], BF16, sb_); dptr = Dep()

        S.dma("sp", cw[:], V.convw, writes=[dcw])
        S.dma("sp", cb[:], V.convb, writes=[dcw])
        S.dma("sp", dtbt[:], V.dtb, writes=[dpar])
        S.dma("sp", AN[:], V.alog, writes=[dpar])
        S.dma("sp", dsum[:], V.d0b, writes=[dpar])
        S.dma("sp", d1t[:], V.d1b, writes=[dpar])
        S.dma("sp", nwt[:], V.nwb, writes=[dpar])
        P_ = [dpar, dDT]
        DTf = DT[:].rearrange("p a b -> p (a b)"); DTSf = DTS[:].rearrange("p a b -> p (a b)"); DTAf = DTA[:].rearrange("p a b -> p (a b)")
        S.op("dve", lambda e: e.tensor_tensor(dsum[:], dsum[:], d1t[:], op=ALU.add), reads=P_, writes=P_)
        S.op("dve", lambda e: e.tensor_tensor(DTf, DTf, dtbt[:], op=ALU.add), reads=P_, writes=P_)
        S.op("act", lambda e: e.activation(DTSf, DTf, AF.Exp), reads=P_, writes=P_)
        S.op("act", lambda e: e.activation(DTSf, DTSf, AF.Ln, bias=1.0), reads=P_, writes=P_)
        S.op("act", lambda e: e.activation(AN[:], AN[:], AF.Exp), reads=P_, writes=P_)
        S.op("dve", lambda e: e.scalar_tensor_tensor(DTAf, DTSf, -1.0, AN[:], op0=ALU.mult, op1=ALU.mult), reads=P_, writes=P_)
        S.op("dve", lambda e: e.tensor_scalar(NDTA[:].rearrange("p a b -> p (a b)"), DTAf, -1.0, None, op0=ALU.mult), reads=P_, writes=P_)

        segs = [(i * 1024, 1024) for i in range(4)] + [(4096, 256)]
        kc = 0
        for si in range(4):
            m = 128 if si < 2 else 64
            for (tk0, n) in segs:
                eng = "dve"
                tb = ctmp[kc % 2]; dtb_ = dctmp[kc % 2]
                kc += 1
                c0 = xcol(tk0) - 2
                S.op(eng, lambda e: e.tensor_scalar(tb[0:m, 0:n], XP[0:m, si, c0:c0 + n], cw[0:m, si * 5:si * 5 + 1], None, op0=ALU.mult),
                     reads=[dXP[si], dcw], writes=[dtb_])
                for k in range(1, 5):
                    S.op(eng, lambda e: e.scalar_tensor_tensor(tb[0:m, 0:n], XP[0:m, si, c0 + k:c0 + k + n], cw[0:m, si * 5 + k:si * 5 + k + 1], tb[0:m, 0:n],
                                                               op0=ALU.mult, op1=ALU.add), reads=[dXP[si], dcw], writes=[dtb_])
                u0 = ucol(tk0)
                S.op("act", lambda e: e.activation(XP[0:m, si, u0:u0 + n], tb[0:m, 0:n], AF.Silu, bias=cb[0:m, si:si + 1], scale=1.0),
                     reads=[dtb_, dcw], writes=[dXP[si]])
        for t in range(NT1):
            u0 = ucol(t * 128)
            S.op("pe", lambda e: e.transpose(ptr[:, 0:128], XP[:, 0, u0:u0 + 128], identb), reads=[dXP[0], dcsb], writes=[dptr])
            S.op("pe", lambda e: e.transpose(ptr[:, 128:256], XP[:, 1, u0:u0 + 128], identb), reads=[dXP[1], dcsb], writes=[dptr], pe_acc=True)
            S.op("pe", lambda e: e.transpose(ptr[:, 256:320], XP[0:64, 2, u0:u0 + 128], csb[0:64, 0, 0:64]), reads=[dXP[2], dcsb], writes=[dptr], pe_acc=True)
            if t % 2 == 0:
                S.op("act", lambda e: e.copy(XTM[:, t, :], ptr[:, 0:320]), reads=[dptr], writes=[dXTM[t]])
            else:
                S.op("dve", lambda e: e.tensor_copy(XTM[:, t, :], ptr[:, 0:320]), reads=[dptr], writes=[dXTM[t]])

        order = {0: [32, 33] + list(range(32)), 1: [33, 32] + list(range(31, -1, -1))}
        it = 0
        for d in range(2):
            S.op("dve", lambda e: e.memset(H[:], 0.0), writes=[dH])
            S.op("dve", lambda e: e.memset(Hb[:], 0.0), writes=[dHb])
            for c in order[d]:
                u0 = ucol(c * 128)
                dta = DTA[:, c, d * 4:(d + 1) * 4]
                dts = DTS[:, c, d * 4:(d + 1) * 4]
                s_ = sm[it % 2]; ds_ = dsmm[it % 2]
                xd = xdt[it % 2]; dxd = dxdt[it % 2]; xw = xdw[it % 2]; dxw = dxdw[it % 2]
                g_ = GT[it % 2]; dg_ = dGT[it % 2]
                it += 1
                S.op("pe", lambda e: e.matmul(pc[:, 0:4], Ud[d], dta, start=True, stop=True), reads=[dcs, dpar], writes=[dpc])
                S.op("pe", lambda e: e.matmul(pc[:, 4:8], onesf, dta, start=True, stop=True), reads=[dcs, dpar], writes=[dpc], pe_acc=True)
                Q_ = [ds_]
                S.op("act", lambda e: e.copy(s_[:, 0:4], pc[:, 0:4]), reads=[dpc], writes=Q_)
                S.op("act", lambda e: e.copy(s_[:, 16:20], pc[:, 4:8]), reads=[dpc] + Q_, writes=Q_)
                S.op("dve", lambda e: e.tensor_scalar(s_[:, 4:8], s_[:, 0:4], -1.0, None, op0=ALU.mult), reads=Q_, writes=Q_)
                S.op("act", lambda e: e.activation(s_[:, 8:12], s_[:, 0:4], AF.Exp), reads=Q_, writes=Q_)
                S.op("dve", lambda e: e.tensor_tensor(s_[:, 12:16], s_[:, 16:20], s_[:, 0:4], op=ALU.subtract), reads=Q_, writes=Q_)
                S.op("act", lambda e: e.activation(s_[:, 12:16], s_[:, 12:16], AF.Exp), reads=Q_, writes=Q_)
                S.op("act", lambda e: e.activation(s_[:, 16:20], s_[:, 16:20], AF.Exp), reads=Q_, writes=Q_)
                S.op("dve", lambda e: e.tensor_tensor(s_[:, 20:24], dts, s_[:, 12:16], op=ALU.mult), reads=Q_ + [dpar], writes=Q_)
                xv = XTM[:, c, 0:256].rearrange("p (a b) -> p a b", a=4)
                S.op("pool", lambda e: e.tensor_tensor(xd[:], xv, dts.unsqueeze(2).to_broadcast([128, 4, 64]), op=ALU.mult), reads=[dXTM[c], dpar], writes=[dxd])
                S.op("pool", lambda e: e.tensor_tensor(xw[:], xv, s_[:, 20:24].unsqueeze(2).to_broadcast([128, 4, 64]), op=ALU.mult), reads=[dXTM[c]] + Q_, writes=[dxw])
                S.op("pe", lambda e: e.matmul(pgt[:], XP[0:64, 2, u0:u0 + 128], XP[0:64, 3, u0:u0 + 128], start=True, stop=True),
                     reads=[dXP[2], dXP[3]], writes=[dpgt])
                S.op("act", lambda e: e.copy(g_[:], pgt[:]), reads=[dpgt], writes=[dg_])
                S.op("dve", lambda e: e.tensor_copy(lrep4[:], dta.unsqueeze(2).to_broadcast([128, 4, 128])), reads=[dpar], writes=[dlrep4])
                S.op("dve", lambda e: e.tensor_tensor(X4[:], Ud[d].unsqueeze(1).to_broadcast([128, 4, 128]), NDTA[:, c, d * 4:(d + 1) * 4].unsqueeze(2).to_broadcast([128, 4, 128]), op=ALU.mult),
                     reads=[dcs, dpar], writes=[dX4])
                for h in range(4):
                    S.op("pe", lambda e: e.matmul(pd4[:, h, :], lrep4[:, h, :], Ud[d], start=True, stop=False), reads=[dlrep4, dcs], writes=[dpd4], pe_acc=(h > 0))
                    S.op("pe", lambda e: e.matmul(pd4[:, h, :], X4[:, h, :], onesf, start=False, stop=False), reads=[dX4, dcs], writes=[dpd4], pe_acc=True)
                    S.op("pe", lambda e: e.matmul(pd4[:, h, :], identf, NEGd[d], start=False, stop=True), reads=[dcs], writes=[dpd4], pe_acc=True)
                S.op("act", lambda e: e.activation(dec4[:], pd4[:], AF.Exp), reads=[dpd4], writes=[ddec4])
                S.op("dve", lambda e: e.tensor_tensor(MT4[:], dec4[:], g_[:].unsqueeze(1).to_broadcast([128, 4, 128]), op=ALU.mult), reads=[ddec4, dg_], writes=[dMT4])
                for h in range(4):
                    S.op("pe", lambda e: e.matmul(pyd[:, h, :], MT4[:, h, :], xd[:, h, :], start=True, stop=True), reads=[dMT4, dxd], writes=[dpyd], pe_acc=(h > 0))
                S.op("pe", lambda e: e.matmul(pyo[:], XP[0:64, 3, u0:u0 + 128], Hb[:], start=True, stop=True), reads=[dXP[3], dHb], writes=[dpyo])
                S.op("pe", lambda e: e.matmul(pst[:], XTM[:, c, 256:320], xw[:].rearrange("p a b -> p (a b)"), start=True, stop=True),
                     reads=[dXTM[c], dxw], writes=[dpst])
                Yc = Y[:, c, :]
                if d == 0:
                    S.op("act", lambda e: e.copy(Yc, pyd[:].rearrange("p a b -> p (a b)")), reads=[dpyd], writes=[dY[c]])
                else:
                    S.op("dve", lambda e: e.tensor_tensor(Yc, pyd[:].rearrange("p a b -> p (a b)"), Yc, op=ALU.add), reads=[dpyd], writes=[dY[c]])
                S.op("dve", lambda e: e.tensor_tensor(tmpy[:], pyo[:].rearrange("p (a b) -> p a b", a=4), s_[:, 8:12].unsqueeze(2).to_broadcast([128, 4, 64]), op=ALU.mult),
                     reads=[dpyo] + Q_, writes=[dtmpy])
                S.op("pool", lambda e: e.tensor_tensor(Yc, Yc, tmpy[:].rearrange("p a b -> p (a b)"), op=ALU.add), reads=[dtmpy], writes=[dY[c]])
                Hv = H[:].rearrange("p (a b) -> p a b", a=4)
                S.op("dve", lambda e: e.tensor_tensor(Hv, Hv, s_[0:64, 16:20].unsqueeze(2).to_broadcast([64, 4, 64]), op=ALU.mult), reads=Q_, writes=[dH])
                S.op("dve", lambda e: e.tensor_tensor(H[:], H[:], pst[:], op=ALU.add), reads=[dpst], writes=[dH])
                S.op("act", lambda e: e.copy(Hb[:], H[:]), reads=[dH], writes=[dHb])
                if d == 1:
                    zb = zr[c % 2]; dzb = dzr[c % 2]
                    S.dma("sp", zb[:], zs[c * 128:(c + 1) * 128, :], reads=[dzs[c]], writes=[dzb])
                    Fd = [dff]
                    S.op("dve", lambda e: e.tensor_tensor(f1[:], XTM[:, c, 0:256], dsum[:], op=ALU.mult), reads=[dXTM[c], dpar] + Fd, writes=Fd)
                    S.op("pool", lambda e: e.tensor_tensor(f1[:], f1[:], Yc, op=ALU.add), reads=[dY[c]] + Fd, writes=Fd)
                    S.op("act", lambda e: e.activation(f2[:], zb[:], AF.Silu), reads=[dzb] + Fd, writes=Fd)
                    S.op("pool", lambda e: e.tensor_tensor(f3[:], f1[:], f2[:], op=ALU.mult), reads=Fd, writes=Fd)
                    S.op("dve", lambda e: e.memset(fs[:], 0.0), reads=Fd, writes=Fd)
                    S.op("act", lambda e: e.activation(fjunk[:], f3[:], AF.Square, accum_out=fs[:, 0:1]), reads=Fd, writes=Fd)
                    rstd_from_ss(S, fs[:, 0:1], fs[:, 1:2], dff, 256, epst[:])
                    S.op("dve", lambda e: e.scalar_tensor_tensor(obt[:], f3[:], fs[:, 1:2], nwt[:], op0=ALU.mult, op1=ALU.mult), reads=Fd + [dpar], writes=[dobt])
                    S.op("pe", lambda e: e.transpose(ptr[:, 0:128], obt[:, 0:128], identb), reads=[dobt, dcsb], writes=[dptr])
                    S.op("pe", lambda e: e.transpose(ptr[:, 128:256], obt[:, 128:256], identb), reads=[dobt, dcsb], writes=[dptr], pe_acc=True)
                    ot = obT[c % 2]; dot_ = dobT[c % 2]
                    S.op("act", lambda e: e.copy(ot[:], ptr[:, 0:256].rearrange("p (a b) -> p a b", a=2)), reads=[dptr], writes=[dot_])
                    S.dma("sp", mixo[128:384, c * 128:(c + 1) * 128].rearrange("(a p) t -> p a t", p=128), ot[:], reads=[dot_], writes=[dmix])
        S.barrier()


OFF_AK, OFF_AV, OFF_BZ, OFF_BX, OFF_BDT, OFF_CQ, OFF_CK, OFF_CV = 256, 384, 512, 1024, 1792, 1808, 2064, 2320


def _rope_perm(dim):
    q = dim // 4
    idx = np.arange(dim)
    return np.where((idx // q) % 2 == 0, idx + q, idx - q)


def k1_cols(g):
    pa, pc = _rope_perm(64), _rope_perm(32)
    a64 = np.arange(64)
    cols = []
    cols.append(np.concatenate([(2 * g + hh) * 64 + a64 for hh in range(2)]))
    cols.append(np.concatenate([(2 * g + hh) * 64 + pa for hh in range(2)]))
    cols.append(np.concatenate([OFF_AK + g * 64 + a64] * 2))
    cols.append(np.concatenate([OFF_AK + g * 64 + pa] * 2))
    pc2 = np.concatenate([pc, 32 + pc])
    for off in (OFF_CQ, OFF_CK):
        for hh in range(2):
            cols.append(off + (2 * g + hh) * 64 + a64)
            cols.append(off + (2 * g + hh) * 64 + pc2)
    cols.append(OFF_BX + g * 256 + np.arange(128))
    cols.append(OFF_BX + g * 256 + 128 + np.arange(128))
    cols.append(OFF_BX + 512 + g * 64 + a64)
    cols.append(OFF_BX + 640 + g * 64 + a64)
    cols.append(OFF_AV + g * 64 + a64)
    cols.append(OFF_CV + 2 * g * 64 + np.arange(128))
    cols.append(OFF_BZ + g * 256 + np.arange(256))
    cols.append(OFF_BDT + 4 * g + np.arange(4))
    cols.append(OFF_BDT + 8 + 4 * g + np.arange(4))
    c = np.concatenate(cols)
    assert c.shape[0] == NCOL1
    return c


def _rope_tables():
    t = np.arange(4096)
    row, col = (t // 64).astype(np.float64), (t % 64).astype(np.float64)

    def tab(dim, reps):
        q = dim // 4
        cosr = np.zeros((dim, 4096)); sinr = np.zeros((dim, 4096))
        for j in range(dim):
            blk, i = j // q, j % q
            f = np.float32(10000.0) ** (-np.float32(i) / np.float32(q))
            pos = row if blk < 2 else col
            ang = (pos.astype(np.float32) * np.float32(f)).astype(np.float32)
            cosr[j] = np.cos(ang)
            sinr[j] = np.sin(ang) * (-1.0 if blk % 2 == 0 else 1.0)
        return np.stack([np.tile(cosr, (reps, 1)), np.tile(sinr, (reps, 1))]).astype(np.float32)

    return tab(64, 2), tab(32, 2)


def _consts():
    s = np.arange(128)[:, None]
    l = np.arange(128)[None, :]
    z = np.zeros((128, 128), np.float32)
    return np.stack([np.eye(128, dtype=np.float32), (s <= l).astype(np.float32), (s >= l).astype(np.float32),
                     np.ones((128, 128), np.float32), np.where(s > l, NEG, z).astype(np.float32),
                     np.where(s < l, NEG, z).astype(np.float32)], axis=1)


def k1_inputs(p, l, b, g, xfull, modrow, modctx, ropeA, ropeC, cst):
    f = np.float32
    rep = lambda v, n=128: np.ascontiguousarray(np.broadcast_to(np.asarray(v, f).reshape(1, -1), (n, np.asarray(v).size)))
    sh1, sc1 = modrow[0:D], modrow[D:2 * D]
    sh1c, sc1c = modctx[0:D], modctx[D:2 * D]
    cw = np.zeros((128, 4, 5), f)
    cb = np.zeros((128, 4), f)
    chs = [g * 256 + np.arange(128), g * 256 + 128 + np.arange(128), 512 + g * 64 + np.arange(64), 640 + g * 64 + np.arange(64)]
    for si, ch in enumerate(chs):
        cw[:len(ch), si, :] = p['b_conv_w'][l][:, ch].T
        cb[:len(ch), si] = p['b_conv_b'][l][ch]
    hsel = 4 * g + np.arange(4)
    dtb = np.tile(np.concatenate([p['b_dt_bias'][l][0, hsel], p['b_dt_bias'][l][1, hsel]]), NT1)
    alog = np.tile(np.concatenate([p['b_a_log'][l][0, hsel], p['b_a_log'][l][1, hsel]]), NT1)
    lam0 = 0.8 - 0.6 * math.exp(-0.3 * l)
    return dict(
        xin=xfull, modv=np.stack([sh1, sc1, sh1c, sc1c]).astype(f), n1w=p['norm1_w'][l][None].astype(f),
        w1=np.ascontiguousarray(p['w_in'][l][:, k1_cols(g)]),
        convw=cw.reshape(128, 20), convb=cb, dtb=rep(dtb), alog=rep(alog),
        d0b=rep(np.repeat(p['b_d'][l][0, hsel], 64)), d1b=rep(np.repeat(p['b_d'][l][1, hsel], 64)),
        nwb=rep(p['b_norm_w'][l][g * 256:(g + 1) * 256]), sink=rep(p['a_sink'][l][2 * g:2 * g + 2]),
        clam=rep(p['c_lambda'][l].reshape(-1)), lconst=rep(np.array([lam0, 1.0 - lam0], f)),
        subw=np.ascontiguousarray(p['c_subln_w'][l].reshape(64, 1).astype(f)), cst=cst, ropeA=ropeA, ropeC=ropeC)


def mix_from_k1(o0, o1):
    return np.concatenate([o0[0:128], o1[0:128], o0[128:384], o1[128:384], o0[384:512], o1[384:512]], axis=0)


WOUT_ORDER = [0, 2, 3, 6, 1, 4, 5, 7]


def emit_k0f(nc, S, ccT, wmod, bmod, mods, dmods):
    with ExitStack() as st:
        C = Ctx(nc, st)
        cs = C.sb([128, 8, 2], F32); dcs = Dep()
        sg = C.sb([128, 8, 2], F32)
        ws = [C.sb([128, 8, 512], F32) for _ in range(2)]; dws = [Dep(), Dep()]
        bs = C.sb([2, 6144], F32); dbs = Dep()
        os_ = C.sb([2, 6144], F32); dos = Dep()
        pp = [C.ps([2, 512], F32) for _ in range(2)]; dpp = [Dep(), Dep()]
        S.dma("sp", cs[:], ccT.rearrange("(c p) m -> p c m", p=128), writes=[dcs])
        S.op("act", lambda e: e.activation(sg[:], cs[:], AF.Sigmoid), reads=[dcs], writes=[dcs])
        S.op("dve", lambda e: e.tensor_tensor(cs[:], cs[:], sg[:], op=ALU.mult), reads=[dcs], writes=[dcs])
        k = 0
        for l in range(DEPTH):
            S.dma("sp", bs[:], bmod[l].partition_broadcast(2), writes=[dbs])
            for n in range(12):
                w_ = ws[k % 2]; dw_ = dws[k % 2]; p = pp[k % 2]; dp = dpp[k % 2]
                k += 1
                S.dma("sp", w_[:], wmod[l].rearrange("(c p) n -> p c n", p=128)[:, :, n * 512:(n + 1) * 512], writes=[dw_])
                for c in range(8):
                    S.op("pe", lambda e: e.matmul(p[:], cs[:, c, :], w_[:, c, :], start=(c == 0), stop=(c == 7)), reads=[dcs, dw_], writes=[dp], pe_acc=(c > 0))
                S.op("dve", lambda e: e.tensor_tensor(os_[:, n * 512:(n + 1) * 512], p[:], bs[:, n * 512:(n + 1) * 512], op=ALU.add), reads=[dp, dbs], writes=[dos])
            S.dma("sp", mods[l], os_[:], reads=[dos], writes=[dmods])
        S.barrier()


def emit_zero_rows(nc, S, dram, dep, rows):
    with ExitStack() as st:
        C = Ctx(nc, st)
        n = rows // 128
        g = 11 if n % 11 == 0 else (8 if n % 8 == 0 else 1)
        z = C.sb([128, g, D], BF16); dz = Dep()
        S.op("pool", lambda e: e.memset(z[:], 0.0), writes=[dz])
        v = dram.rearrange("(a p) d -> p a d", p=128)
        for i in range(n // g):
            S.dma("sp", v[:, i * g:(i + 1) * g, :], z[:], reads=[dz], writes=[dep])
        S.barrier()


def build_fused():
    nc = bass.Bass("TRN2", target_bir_lowering=False)
    di = lambda n, s, d=F32: nc.dram_tensor(n, list(s), d, kind="ExternalInput").ap()
    do = lambda n, s, d=F32: nc.dram_tensor(n, list(s), d, kind="ExternalOutput").ap()
    x0 = di("x0", [NTOK1, D]); ccT = di("ccT", [D, 2]); wmod = di("wmod", [DEPTH, D, 6 * D]); bmod = di("bmod", [DEPTH, 1, 6 * D])
    n1w = di("n1w", [DEPTH, 1, D]); w1 = di("w1", [DEPTH, 2, D, NCOL1])
    convw = di("convw", [DEPTH, 2, 128, 20]); convb = di("convb", [DEPTH, 2, 128, 4])
    dtb = di("dtb", [DEPTH, 2, 128, NT1 * 8]); alog = di("alog", [DEPTH, 2, 128, NT1 * 8])
    d0b = di("d0b", [DEPTH, 2, 128, 256]); d1b = di("d1b", [DEPTH, 2, 128, 256]); nwb = di("nwb", [DEPTH, 2, 128, 256])
    sink = di("sink", [DEPTH, 2, 128, 2]); clam = di("clam", [DEPTH, 128, 128]); lconst = di("lconst", [DEPTH, 128, 2]); subw = di("subw", [DEPTH, 64, 1])
    cst = di("cst", [128, 6, 128]); ropeA = di("ropeA", [2, 128, 4096]); ropeC = di("ropeC", [2, 64, 4096])
    wout = di("wout", [DEPTH, D, D]); n2w = di("n2w", [DEPTH, 1, D]); fnw = di("fnw", [1, D]); wr = di("wr", [DEPTH, D, 36]); ident = di("ident", [128, 128])
    wg = di("wg", [DEPTH * NEXP * 128, 8 * 512]); wu = di("wu", [DEPTH * NEXP * 128, 8 * 512]); wd = di("wd", [DEPTH * NEXP * 128, 4 * D])
    out = do("out", [4096, D])
    xcur = do("xcur", [NTOK1, D]); mixo = do("mixo", [2, 512, NTOK1], BF16); zs = do("zs", [NTOK1, 256], BF16)
    mods = do("mods", [DEPTH, 2, 6 * D]); fscr = do("fscr", [128, D])
    cst2 = di("cst2", [128, 4, 128])
    xs = nc.dram_tensor("xs", [NSLOT, D], BF16).ap()
    ys = nc.dram_tensor("ys", [NSLOT, D], F32).ap()
    hcache = nc.dram_tensor("hcache", [NT1, 128, D], BF16).ap()
    with ExitStack() as st:
        S = Sched(nc, st)
        dX = [Dep() for _ in range(NT1)]
        dF = [Dep() for _ in range(NT1)]
        dmods = Dep()
        dmixg = [Dep(), Dep()]
        dxs = Dep(); dys = Dep()
        dhc = [Dep() for _ in range(NT1)]
        for t in range(NT1):
            S.dma("sp", xcur[t * 128:(t + 1) * 128, :], x0[t * 128:(t + 1) * 128, :], writes=[dX[t]])
        emit_zero_rows(nc, S, xs, dxs, NSLOT)
        emit_k0f(nc, S, ccT, wmod, bmod, mods, dmods)
        for l in range(DEPTH):
            for g in range(2):
                I1 = type("I1", (), dict(
                    xrow=staticmethod(lambda t: xcur[t * 128:(t + 1) * 128, :]),
                    modrow=staticmethod(lambda v, k, l=l: mods[l, v:v + 1, k * D:(k + 1) * D]),
                    n1w=n1w[l], w1=w1[l, g], convw=convw[l, g], convb=convb[l, g], dtb=dtb[l, g], alog=alog[l, g], d0b=d0b[l, g], d1b=d1b[l, g],
                    nwb=nwb[l, g], sink=sink[l, g], clam=clam[l], lconst=lconst[l], subw=subw[l], cst=cst, ropeA=ropeA, ropeC=ropeC,
                    mixo=mixo[g], zs=zs))
                emit_k1(nc, S, I1, dX, dmixg[g], [dmods], hc=("write" if g == 0 else "read", hcache, dhc))
            last = l == DEPTH - 1
            I2 = type("I2", (), dict(
                xrow=staticmethod(lambda t: xcur[t * 128:(t + 1) * 128, :]),
                orow=staticmethod(lambda t: xcur[t * 128:(t + 1) * 128, :]),
                frow=staticmethod(lambda t: out[t * 128:(t + 1) * 128, :] if t < 32 else fscr),
                mtv=staticmethod(lambda t: mixo.rearrange("g (k p) t -> p (g k) t", p=128)[:, :, t * 128:(t + 1) * 128]),
                modrow=staticmethod(lambda v, k, l=l: mods[l, v:v + 1, k * D:(k + 1) * D]),
                wout=wout[l], n2w=n2w[l], fnw=fnw, wr=wr[l], ident=ident, cst2=cst2, wl=l, wgf=wg, wuf=wu, wdf=wd, xs=xs, ys=ys))
            emit_k2s(nc, S, I2, dX, dF, dmixg, [dmods], dxs, dys, do_fin=last)
            print("fused: layer", l, "instrs", S.n_ins, "waits", S.n_wait, "sems", len(S.sems))
        S.finish(dF + dX + dmixg + [dmods])
    return nc


def fused_inputs(p, b, ropeA, ropeC, cst):
    f = np.float32
    K1 = [[k1_inputs(p, l, b, g, None, np.zeros(2 * D, f), np.zeros(2 * D, f), ropeA, ropeC, cst) for g in range(2)] for l in range(DEPTH)]
    stk = lambda key: np.ascontiguousarray(np.stack([np.stack([K1[l][g][key] for g in range(2)]) for l in range(DEPTH)]))
    stl = lambda key: np.ascontiguousarray(np.stack([K1[l][0][key] for l in range(DEPTH)]))
    im = dict(
        x0=np.ascontiguousarray(np.concatenate([p['x'][b], p['ctx'][b]], 0).astype(f)),
        ccT=np.ascontiguousarray(np.stack([p['c'][b], p['c_ctx']], 0).T.astype(f)),
        wmod=p['w_mod'], bmod=np.ascontiguousarray(p['b_mod'][:, None, :]),
        n1w=np.ascontiguousarray(p['norm1_w'][:, None, :]), w1=stk('w1'), convw=stk('convw'), convb=stk('convb'), dtb=stk('dtb'), alog=stk('alog'),
        d0b=stk('d0b'), d1b=stk('d1b'), nwb=stk('nwb'), sink=stk('sink'), clam=stl('clam'), lconst=stl('lconst'), subw=stl('subw'),
        cst=cst, ropeA=ropeA, ropeC=ropeC,
        wout=np.ascontiguousarray(np.stack([np.concatenate([p['w_out'][l][c * 128:(c + 1) * 128] for c in WOUT_ORDER], 0) for l in range(DEPTH)])),
        n2w=np.ascontiguousarray(p['norm2_w'][:, None, :]), fnw=np.ascontiguousarray(p['final_norm_w'][None]),
        wr=np.ascontiguousarray(np.concatenate([p['moe_group_router'], p['moe_router']], axis=2).astype(f)),
        ident=np.eye(128, dtype=f), cst2=_consts2(), wg=p['_wg_r'], wu=p['_wu_r'], wd=p['_wd_r'])
    return im


_NC = {}


def _get(name, fn):
    if name not in _NC:
        _NC[name] = fn()
    return _NC[name]


def kernel_unfused(**inp):
    p = {k: np.asarray(v) for k, v in inp.items()}
    f = np.float32
    B, L = 4, 4096
    cores = list(range(8))
    ccT = np.ascontiguousarray(np.concatenate([p['c'], p['c_ctx'][None]], 0).T.astype(f))
    im0 = []
    for j in cores:
        l, hf = j // 2, j % 2
        im0.append(dict(ccT=ccT, w=np.ascontiguousarray(p['w_mod'][l][:, hf * 3072:(hf + 1) * 3072]),
                        b=np.ascontiguousarray(p['b_mod'][l][None, hf * 3072:(hf + 1) * 3072])))
    r0 = run_bass_kernel_spmd(_get('k0', build_k0), im0, core_ids=cores).results
    mods = [np.concatenate([r0[2 * l]['out'], r0[2 * l + 1]['out']], axis=1) for l in range(DEPTH)]

    ropeA, ropeC = _rope_tables()
    cst = _consts()
    ident = np.eye(128, dtype=f)
    x = p['x'].astype(f)
    xc = p['ctx'].astype(f)
    out = None
    for l in range(DEPTH):
        m = mods[l]
        im1 = []
        for j in cores:
            b, g = j // 2, j % 2
            xfull = np.ascontiguousarray(np.concatenate([x[b], xc[b]], 0))
            im1.append(k1_inputs(p, l, b, g, xfull, m[b], m[4], ropeA, ropeC, cst))
        r1 = run_bass_kernel_spmd(_get('k1', build_k1), im1, core_ids=cores).results
        wr = np.ascontiguousarray(np.concatenate([p['moe_group_router'][l], p['moe_router'][l]], axis=1).astype(f))
        im2 = []
        for j in cores:
            b, s = j // 2, j % 2
            mix = mix_from_k1(r1[2 * b]['mixo'], r1[2 * b + 1]['mixo'])
            mixT = np.ascontiguousarray(np.concatenate([mix[:, s * 2048:(s + 1) * 2048], mix[:, 4096 + s * 128:4096 + (s + 1) * 128]], 1))
            xin = np.ascontiguousarray(np.concatenate([x[b, s * 2048:(s + 1) * 2048], xc[b, s * 128:(s + 1) * 128]], 0))
            sp6 = lambda v: [v[i * D:(i + 1) * D] for i in range(6)]
            ml, mc = sp6(m[b]), sp6(m[4])
            modr = np.stack([ml[2], ml[3], ml[4], ml[5], mc[2], mc[3], mc[4], mc[5]]).astype(f)
            im2.append(dict(xin=xin, mixT=mixT, wout=p['w_out'][l], modr=modr, n2w=p['norm2_w'][l][None], fnw=p['final_norm_w'][None],
                            wr=wr, ident=ident, wg=p['moe_w_gate'][l], wu=p['moe_w_up'][l], wd=p['moe_w_down'][l]))
        r2 = run_bass_kernel_spmd(_get('k2', build_k2), im2, core_ids=cores).results
        key = 'xfin' if l == DEPTH - 1 else 'xout'
        xn = np.empty_like(x)
        xcn = np.empty_like(xc)
        for j in cores:
            b, s = j // 2, j % 2
            o = r2[j][key]
            xn[b, s * 2048:(s + 1) * 2048] = o[0:2048]
            xcn[b, s * 128:(s + 1) * 128] = o[2048:2176]
        x, xc = xn, xcn
    return x


def kernel(**inp):
    p = {k: np.asarray(v) for k, v in inp.items()}
    ropeA, ropeC = _rope_tables()
    cst = _consts()
    p['_wg_r'] = moe_relayout(p['moe_w_gate']); p['_wu_r'] = moe_relayout(p['moe_w_up']); p['_wd_r'] = moe_relayout(p['moe_w_down'])
    per_b = [fused_inputs(p, b, ropeA, ropeC, cst) for b in range(4)]
    in_maps = [per_b[j // 2] for j in range(8)]
    res = run_bass_kernel_spmd(_get('fused', build_fused), in_maps, core_ids=list(range(8))).results
    return np.stack([res[2 * b]['out'] for b in range(4)]).astype(np.float32)
```

```python
import math
from contextlib import ExitStack

import numpy as np
import ml_dtypes
import concourse.bass as bass
import concourse.mybir as mybir
from concourse.bass_utils import run_bass_kernel_spmd

F32 = mybir.dt.float32
BF16 = mybir.dt.bfloat16
AF = mybir.ActivationFunctionType
ALU = mybir.AluOpType
AX = mybir.AxisListType

D = 1024
DEPTH = 4
NEG = -30000.0
EPS = 1e-6
EPOCH = 12000
import os as _os
NOSELF = set(_os.environ.get('NOSELF', 'pe').split(','))


class Dep:
    __slots__ = ("w", "r", "dsem", "dcnt")

    def __init__(self):
        self.w = None
        self.r = {}
        self.dsem = None
        self.dcnt = 0


class Sched:
    def __init__(self, nc, stack):
        self.nc = nc
        self.stack = stack
        self.eng = {"pe": nc.tensor, "dve": nc.vector, "act": nc.scalar, "pool": nc.gpsimd, "sp": nc.sync}
        self.sems = {}
        self.cnt = {e: 0 for e in self.eng}
        self.epoch = {e: 0 for e in self.eng}
        self.waited = {e: {} for e in self.eng}
        self.ndsem = 0
        self.n_ins = 0
        self.n_wait = 0
        self.dma_deps = []
        self.free_dsems = []

    def _sem(self, key):
        if key not in self.sems:
            self.sems[key] = self.stack.enter_context(self.nc.semaphore("s_%s_%s" % key))
        return self.sems[key]

    def _wait(self, eng, need):
        wd = self.waited[eng]
        e = self.eng[eng]
        for k, v in need.items():
            if wd.get(k, 0) >= v:
                continue
            e.wait_ge(self._sem(k), v)
            wd[k] = v
            self.n_wait += 1

    def _collect(self, eng, reads, writes, pe_acc):
        need = {}
        for d in reads:
            if d.w is not None:
                k, v = d.w
                if need.get(k, 0) < v:
                    need[k] = v
        for d in writes:
            if d.w is not None:
                k, v = d.w
                if not (pe_acc and k[0] == "pe" and eng == "pe"):
                    if need.get(k, 0) < v:
                        need[k] = v
            for k, v in d.r.items():
                if need.get(k, 0) < v:
                    need[k] = v
        if eng in NOSELF:
            need = {k: v for k, v in need.items() if k[0] != eng}
        self._wait(eng, need)

    def op(self, eng, emit, reads=(), writes=(), pe_acc=False):
        self._collect(eng, reads, writes, pe_acc)
        ins = emit(self.eng[eng])
        if self.cnt[eng] >= EPOCH:
            self.epoch[eng] += 1
            self.cnt[eng] = 0
        self.cnt[eng] += 1
        key = (eng, self.epoch[eng])
        ins.then_inc(self._sem(key), 1)
        c = self.cnt[eng]
        for d in reads:
            d.r[key] = c
        for d in writes:
            d.w = (key, c)
            d.r = {}
        self.n_ins += 1
        return ins

    def dma(self, q, out, in_, reads=(), writes=(), **kw):
        self._collect(q, reads, writes, False)
        d0 = writes[0]
        if d0.dsem is None:
            if self.free_dsems:
                d0.dsem, d0.dcnt = self.free_dsems.pop()
            else:
                d0.dsem = ("dma", self.ndsem)
                self.ndsem += 1
            self.dma_deps.append(d0)
        d0.dcnt += 16
        ins = self.eng[q].dma_start(out=out, in_=in_, **kw)
        ins.then_inc(self._sem(d0.dsem), 16)
        for d in reads:
            d.r[d0.dsem] = d0.dcnt
        for d in writes:
            d.w = (d0.dsem, d0.dcnt)
            d.r = {}
        self.n_ins += 1
        return ins

    def idma(self, out, in_, idx_ap, gather, nslot, reads=(), writes=()):
        self._collect("pool", reads, writes, False)
        d0 = writes[0]
        if d0.dsem is None:
            if self.free_dsems:
                d0.dsem, d0.dcnt = self.free_dsems.pop()
            else:
                d0.dsem = ("dma", self.ndsem)
                self.ndsem += 1
            self.dma_deps.append(d0)
        d0.dcnt += 16
        off = bass.IndirectOffsetOnAxis(ap=idx_ap, axis=0)
        if gather:
            ins = self.nc.gpsimd.indirect_dma_start(out=out, out_offset=None, in_=in_, in_offset=off, bounds_check=None)
        else:
            ins = self.nc.gpsimd.indirect_dma_start(out=out, out_offset=off, in_=in_, in_offset=None, bounds_check=None)
        ins.then_inc(self._sem(d0.dsem), 16)
        for d in reads:
            d.r[d0.dsem] = d0.dcnt
        for d in writes:
            d.w = (d0.dsem, d0.dcnt)
            d.r = {}
        self.n_ins += 1
        return ins

    def barrier(self):
        need = {}
        for e in self.eng:
            if self.cnt[e] > 0:
                need[(e, self.epoch[e])] = self.cnt[e]
        for d in self.dma_deps:
            need[d.dsem] = d.dcnt
        for e in self.eng:
            self._wait(e, dict(need))
        for d in self.dma_deps:
            self.free_dsems.append((d.dsem, d.dcnt))
            d.dsem = None
            d.dcnt = 0
            if d.w is not None and d.w[0][0] == "dma":
                d.w = None
            d.r = {k: v for k, v in d.r.items() if k[0] != "dma"}
        self.dma_deps = []

    def finish(self, deps, eng="sp"):
        self._collect(eng, deps, (), False)


class Ctx:
    K = [0]

    def __init__(self, nc, st):
        self.nc, self.st = nc, st

    def sb(self, shape, dt, st=None):
        Ctx.K[0] += 1
        return (st or self.st).enter_context(self.nc.sbuf_tensor("t%d" % Ctx.K[0], list(shape), dt))

    def ps(self, shape, dt, st=None):
        Ctx.K[0] += 1
        return (st or self.st).enter_context(self.nc.psum_tensor("p%d" % Ctx.K[0], list(shape), dt))


def rstd_from_ss(S, ss_ap, out_ap, dep, n, eps_ap):
    S.op("act", lambda e: e.activation(out_ap, ss_ap, AF.Ln, bias=eps_ap, scale=1.0 / n), reads=[dep], writes=[dep])
    S.op("act", lambda e: e.activation(out_ap, out_ap, AF.Exp, scale=-0.5), reads=[dep], writes=[dep])


def build_k0():
    nc = bass.Bass("TRN2", target_bir_lowering=False)
    ccT = nc.dram_tensor("ccT", [D, 5], F32, kind="ExternalInput").ap()
    w = nc.dram_tensor("w", [D, 3072], F32, kind="ExternalInput").ap()
    b = nc.dram_tensor("b", [1, 3072], F32, kind="ExternalInput").ap()
    out = nc.dram_tensor("out", [5, 3072], F32, kind="ExternalOutput").ap()
    with ExitStack() as st:
        S = Sched(nc, st)
        C = Ctx(nc, st)
        cs = C.sb([128, 8, 5], F32); dcs = Dep()
        sg = C.sb([128, 8, 5], F32)
        ws = C.sb([128, 8, 3072], F32); dws = [Dep() for _ in range(8)]
        bs = C.sb([5, 3072], F32); dbs = Dep()
        os_ = C.sb([5, 3072], F32); dos = Dep()
        pp = [C.ps([5, 512], F32) for _ in range(2)]; dpp = [Dep(), Dep()]
        dout = Dep()
        S.dma("sp", cs[:], ccT.rearrange("(c p) m -> p c m", p=128), writes=[dcs])
        S.dma("sp", bs[:], b.partition_broadcast(5), writes=[dbs])
        wv = w.rearrange("(c p) n -> p c n", p=128)
        for c in range(8):
            S.dma("sp", ws[:, c, :], wv[:, c, :], writes=[dws[c]])
        S.op("act", lambda e: e.activation(sg[:], cs[:], AF.Sigmoid), reads=[dcs], writes=[dcs])
        S.op("dve", lambda e: e.tensor_tensor(cs[:], cs[:], sg[:], op=ALU.mult), reads=[dcs], writes=[dcs])
        for n in range(6):
            p = pp[n % 2]; dp = dpp[n % 2]
            for c in range(8):
                S.op("pe", lambda e: e.matmul(p[:], cs[:, c, :], ws[:, c, n * 512:(n + 1) * 512], start=(c == 0), stop=(c == 7)),
                     reads=[dcs, dws[c]], writes=[dp], pe_acc=(c > 0))
            S.op("dve", lambda e: e.tensor_tensor(os_[:, n * 512:(n + 1) * 512], p[:], bs[:, n * 512:(n + 1) * 512], op=ALU.add),
                 reads=[dp, dbs], writes=[dos])
        S.dma("sp", out, os_[:], reads=[dos], writes=[dout])
        S.finish([dout])
    return nc


NT2 = 17
NTOK2 = NT2 * 128
NEXP = 32


def emit_k2(nc, S, I, dX, dF, dmix, dmods, do_fin=True):
    with ExitStack() as st:
        C = Ctx(nc, st)
        h2T = C.sb([128, 8, NTOK2], BF16); dh2T = [Dep() for _ in range(NT2)]
        gates = C.sb([128, NT2, NEXP], F32); dgates = [Dep() for _ in range(NT2)]
        yacc = C.sb([128, NT2, D], F32); dyacc = [Dep() for _ in range(NT2)]
        bc = [C.sb([128, D], F32) for _ in range(6)]; dbc = [Dep() for _ in range(6)]
        epst = C.sb([128, 1], F32); deps_ = Dep()
        idf = C.sb([128, 128], F32); didf = Dep()
        idb = C.sb([128, 128], BF16); didb = Dep()
        dxout = dX
        dxfin = dF
        S.op("dve", lambda e: e.memset(epst[:], EPS), writes=[deps_])
        S.dma("sp", idf[:], I.ident, writes=[didf])
        S.dma("pool", idb[:], I.ident, writes=[didb])

        with ExitStack() as st1:
            woutb = C.sb([128, 8, D], BF16, st1); dwoutb = Dep()
            wrs = C.sb([128, 8, 36], F32, st1); dwrs = Dep()
            tmpw = C.sb([128, D], F32, st1); dtmpw = Dep()
            xt = [C.sb([128, D], F32, st1) for _ in range(2)]; dxt = [Dep(), Dep()]
            mt = [C.sb([128, 8, 128], BF16, st1) for _ in range(2)]; dmt = [Dep(), Dep()]
            tmp = C.sb([128, D], F32, st1); dtmp = Dep()
            x1 = [C.sb([128, D], F32, st1) for _ in range(2)]; dx1 = [Dep(), Dep()]
            h2f = C.sb([128, D], F32, st1); dh2f = Dep()
            h2b = C.sb([128, D], BF16, st1); dh2b = Dep()
            h2Tf = C.sb([128, 8, 128], F32, st1); dh2Tf = Dep()
            junk = C.sb([128, D], F32, st1); djunk = Dep()
            sm = C.sb([128, 16], F32, st1); dsm = Dep()
            lg = C.sb([128, 36], F32, st1); dlg = Dep()
            elm = C.sb([128, 32], F32, st1); delm = Dep()
            elm2 = C.sb([128, 32], F32, st1)
            mk1 = C.sb([128, 32], F32, st1)
            mk2 = C.sb([128, 32], F32, st1)
            gm = C.sb([128, 8], F32, st1)
            pmix = [C.ps([128, 512], F32, st1) for _ in range(2)]; dpmix = [Dep(), Dep()]
            ptb = C.ps([128, 8, 128], BF16, st1); dptb = Dep()
            ptf = C.ps([128, 8, 128], F32, st1); dptf = Dep()
            prt = C.ps([128, 36], F32, st1); dprt = Dep()

            S.dma("pool", woutb[:], I.wout.rearrange("(c p) n -> p c n", p=128), writes=[dwoutb])
            S.dma("sp", wrs[:], I.wr.rearrange("(c p) n -> p c n", p=128), writes=[dwrs])
            S.dma("sp", tmpw[:], I.n2w.partition_broadcast(128), writes=[dtmpw])
            for v in range(2):
                S.dma("sp", bc[v][:], I.modrow(v, 2).partition_broadcast(128), reads=dmods, writes=[dbc[v]])
                S.dma("sp", bc[2 + v][:], I.modrow(v, 4).partition_broadcast(128), reads=dmods, writes=[dbc[2 + v]])
                S.dma("sp", bc[4 + v][:], I.modrow(v, 3).partition_broadcast(128), reads=dmods, writes=[dbc[4 + v]])
                S.op("dve", lambda e: e.scalar_tensor_tensor(bc[2 + v][:], bc[2 + v][:], 1.0, tmpw[:], op0=ALU.add, op1=ALU.mult),
                     reads=[dtmpw], writes=[dbc[2 + v]])

            for t in range(NT2):
                v = 1 if t == NT2 - 1 else 0
                b2 = t % 2
                tsl = slice(t * 128, (t + 1) * 128)
                S.dma("sp", xt[b2][:], I.xrow(t), reads=[dxout[t]], writes=[dxt[b2]])
                S.dma("sp", mt[b2][:], I.mtv(t), reads=dmix, writes=[dmt[b2]])
                for hf in range(2):
                    for c in range(8):
                        S.op("pe", lambda e: e.matmul(pmix[hf][:], mt[b2][:, c, :], woutb[:, c, hf * 512:(hf + 1) * 512],
                                                      start=(c == 0), stop=(c == 7)),
                             reads=[dmt[b2], dwoutb], writes=[dpmix[hf]], pe_acc=(c > 0))
                    S.op("dve", lambda e: e.tensor_tensor(tmp[:, hf * 512:(hf + 1) * 512], pmix[hf][:], bc[v][:, hf * 512:(hf + 1) * 512], op=ALU.mult),
                         reads=[dpmix[hf], dbc[v]], writes=[dtmp])
                S.op("pool", lambda e: e.tensor_tensor(x1[b2][:], tmp[:], xt[b2][:], op=ALU.add), reads=[dtmp, dxt[b2]], writes=[dx1[b2]])
                S.dma("sp", I.orow(t), x1[b2][:], reads=[dx1[b2]], writes=[dxout[t]])
                S.op("dve", lambda e: e.memset(sm[:], 0.0), writes=[dsm])
                S.op("act", lambda e: e.activation(junk[:], x1[b2][:], AF.Square, accum_out=sm[:, 0:1]), reads=[dx1[b2], dsm], writes=[djunk, dsm])
                rstd_from_ss(S, sm[:, 0:1], sm[:, 1:2], dsm, D, epst[:])
                S.op("dve", lambda e: e.scalar_tensor_tensor(h2f[:], x1[b2][:], sm[:, 1:2], bc[2 + v][:], op0=ALU.mult, op1=ALU.mult),
                     reads=[dx1[b2], dsm, dbc[2 + v]], writes=[dh2f])
                S.op("pool", lambda e: e.tensor_tensor(h2f[:], h2f[:], bc[4 + v][:], op=ALU.add), reads=[dbc[4 + v]], writes=[dh2f])
                S.op("act", lambda e: e.copy(h2b[:], h2f[:]), reads=[dh2f], writes=[dh2b])
                for c in range(8):
                    S.op("pe", lambda e: e.transpose(ptb[:, c, :], h2b[:, c * 128:(c + 1) * 128], idb[:]), reads=[dh2b, didb], writes=[dptb], pe_acc=(c > 0))
                S.op("act", lambda e: e.copy(h2T[:, :, tsl], ptb[:]), reads=[dptb], writes=[dh2T[t]])
                for c in range(8):
                    S.op("pe", lambda e: e.transpose(ptf[:, c, :], h2f[:, c * 128:(c + 1) * 128], idf[:]), reads=[dh2f, didf], writes=[dptf], pe_acc=(c > 0))
                S.op("dve", lambda e: e.tensor_copy(h2Tf[:], ptf[:]), reads=[dptf], writes=[dh2Tf])
                for c in range(8):
                    S.op("pe", lambda e: e.matmul(prt[:], h2Tf[:, c, :], wrs[:, c, :], start=(c == 0), stop=(c == 7)),
                         reads=[dh2Tf, dwrs], writes=[dprt], pe_acc=(c > 0))
                S.op("act", lambda e: e.copy(lg[:], prt[:]), reads=[dprt], writes=[dlg])
                R = [dlg, dsm]
                V = lambda f: S.op("dve", f, reads=R, writes=R)
                V(lambda e: e.reduce_max(sm[:, 2:3], lg[:, 0:4], axis=AX.X))
                V(lambda e: e.tensor_scalar(sm[:, 3:4], sm[:, 2:3], -1.0, None, op0=ALU.mult))
                S.op("act", lambda e: e.activation(gm[:, 0:4], lg[:, 0:4], AF.Exp, bias=sm[:, 3:4], scale=1.0, accum_out=sm[:, 4:5]), reads=R, writes=R)
                V(lambda e: e.reciprocal(sm[:, 5:6], sm[:, 4:5]))
                V(lambda e: e.tensor_scalar(gm[:, 4:8], lg[:, 0:4], sm[:, 2:3], None, op0=ALU.is_equal))
                V(lambda e: e.tensor_scalar(gm[:, 4:8], gm[:, 4:8], 1e30, -1e30, op0=ALU.mult, op1=ALU.add))
                for g in range(4):
                    V(lambda e: e.tensor_scalar(elm[:, g * 8:(g + 1) * 8], lg[:, 4 + g * 8:12 + g * 8], gm[:, 4 + g:5 + g], None, op0=ALU.add))
                V(lambda e: e.reduce_max(sm[:, 6:7], elm[:], axis=AX.X))
                V(lambda e: e.tensor_scalar(mk1[:], elm[:], sm[:, 6:7], None, op0=ALU.is_equal))
                V(lambda e: e.scalar_tensor_tensor(elm2[:], mk1[:], -1e30, elm[:], op0=ALU.mult, op1=ALU.add))
                V(lambda e: e.reduce_max(sm[:, 7:8], elm2[:], axis=AX.X))
                V(lambda e: e.tensor_scalar(mk2[:], elm2[:], sm[:, 7:8], None, op0=ALU.is_equal))
                V(lambda e: e.tensor_tensor(sm[:, 8:9], sm[:, 7:8], sm[:, 6:7], op=ALU.subtract))
                S.op("act", lambda e: e.activation(sm[:, 9:10], sm[:, 8:9], AF.Exp), reads=R, writes=R)
                V(lambda e: e.tensor_scalar(sm[:, 10:11], sm[:, 9:10], 1.0, None, op0=ALU.add))
                V(lambda e: e.reciprocal(sm[:, 11:12], sm[:, 10:11]))
                V(lambda e: e.tensor_tensor(sm[:, 12:13], sm[:, 11:12], sm[:, 5:6], op=ALU.mult))
                V(lambda e: e.tensor_tensor(sm[:, 13:14], sm[:, 12:13], sm[:, 9:10], op=ALU.mult))
                S.op("dve", lambda e: e.tensor_scalar(gates[:, t, :], mk1[:], sm[:, 12:13], None, op0=ALU.mult), reads=R, writes=[dgates[t]])
                S.op("dve", lambda e: e.scalar_tensor_tensor(gates[:, t, :], mk2[:], sm[:, 13:14], gates[:, t, :], op0=ALU.mult, op1=ALU.add),
                     reads=R, writes=[dgates[t]])
            S.barrier()

        with ExitStack() as st2:
            wgb = [C.sb([128, 8, 512], BF16, st2) for _ in range(2)]
            wub = [C.sb([128, 8, 512], BF16, st2) for _ in range(2)]
            wdb = [C.sb([128, 4, D], BF16, st2) for _ in range(2)]
            dwg = [Dep(), Dep()]; dwu = [Dep(), Dep()]; dwd = [Dep(), Dep()]
            sgt = [C.sb([128, 512], F32, st2) for _ in range(2)]; dsgt = [Dep(), Dep()]
            hid = [C.sb([128, 4, 512], BF16, st2) for _ in range(2)]; dhid = [Dep(), Dep()]
            pg = [C.ps([128, 512], F32, st2) for _ in range(2)]; dpg = [Dep(), Dep()]
            pu = [C.ps([128, 512], F32, st2) for _ in range(2)]; dpu = [Dep(), Dep()]
            py = [C.ps([128, 512], F32, st2) for _ in range(4)]; dpy = [Dep() for _ in range(4)]
            blocks = [(0, 512), (512, 512), (1024, 512), (1536, 512), (2048, 128)]

            def load_w(e):
                b = e % 2
                S.dma("pool", wgb[b][:], I.wg[e].rearrange("(c p) n -> p c n", p=128), writes=[dwg[b]])
                S.dma("pool", wub[b][:], I.wu[e].rearrange("(c p) n -> p c n", p=128), writes=[dwu[b]])
                S.dma("pool", wdb[b][:], I.wd[e].rearrange("(c p) n -> p c n", p=128), writes=[dwd[b]])

            load_w(0)
            k = 0
            ky = 0
            for e_ in range(NEXP):
                b = e_ % 2
                if e_ + 1 < NEXP:
                    load_w(e_ + 1)
                for bi, (t0, tn) in enumerate(blocks):
                    hb = bi % 2
                    tiles = list(range(t0 // 128, (t0 + tn) // 128))
                    hdeps = [dh2T[t] for t in tiles]
                    for j in range(4):
                        kk = k % 2
                        k += 1
                        for c in range(8):
                            S.op("pe", lambda e: e.matmul(pg[kk][:, 0:tn], wgb[b][:, c, j * 128:(j + 1) * 128], h2T[:, c, t0:t0 + tn],
                                                          start=(c == 0), stop=(c == 7)),
                                 reads=[dwg[b]] + hdeps, writes=[dpg[kk]], pe_acc=(c > 0))
                        for c in range(8):
                            S.op("pe", lambda e: e.matmul(pu[kk][:, 0:tn], wub[b][:, c, j * 128:(j + 1) * 128], h2T[:, c, t0:t0 + tn],
                                                          start=(c == 0), stop=(c == 7)),
                                 reads=[dwu[b]] + hdeps, writes=[dpu[kk]], pe_acc=(c > 0))
                        S.op("act", lambda e: e.activation(sgt[kk][:, 0:tn], pg[kk][:, 0:tn], AF.Silu), reads=[dpg[kk]], writes=[dsgt[kk]])
                        S.op("dve", lambda e: e.tensor_tensor(hid[hb][:, j, 0:tn], pu[kk][:, 0:tn], sgt[kk][:, 0:tn], op=ALU.mult),
                             reads=[dpu[kk], dsgt[kk]], writes=[dhid[hb]])
                    for ti, t in enumerate(tiles):
                        for hf in range(2):
                            q = ky % 4
                            ky += 1
                            for j in range(4):
                                S.op("pe", lambda e: e.matmul(py[q][:], hid[hb][:, j, ti * 128:(ti + 1) * 128], wdb[b][:, j, hf * 512:(hf + 1) * 512],
                                                              start=(j == 0), stop=(j == 3)),
                                     reads=[dhid[hb], dwd[b]], writes=[dpy[q]], pe_acc=(j > 0))
                            ysl = yacc[:, t, hf * 512:(hf + 1) * 512]
                            if e_ == 0:
                                S.op("dve", lambda e: e.tensor_scalar(ysl, py[q][:], gates[:, t, e_:e_ + 1], None, op0=ALU.mult),
                                     reads=[dpy[q], dgates[t]], writes=[dyacc[t]])
                            else:
                                S.op("dve", lambda e: e.scalar_tensor_tensor(ysl, py[q][:], gates[:, t, e_:e_ + 1], ysl, op0=ALU.mult, op1=ALU.add),
                                     reads=[dpy[q], dgates[t]], writes=[dyacc[t]])
            S.barrier()

        with ExitStack() as st3:
            x1r = [C.sb([128, D], F32, st3) for _ in range(2)]; dx1r = [Dep(), Dep()]
            xo = [C.sb([128, D], F32, st3) for _ in range(2)]; dxo = [Dep(), Dep()]
            xf = [C.sb([128, D], F32, st3) for _ in range(2)]; dxf = [Dep(), Dep()]
            junk = C.sb([128, D], F32, st3); djunk = Dep()
            sm = C.sb([128, 4], F32, st3); dsm = Dep()
            for v in range(2):
                S.dma("sp", bc[v][:], I.modrow(v, 5).partition_broadcast(128), reads=dmods, writes=[dbc[v]])
            S.dma("sp", bc[2][:], I.fnw.partition_broadcast(128), writes=[dbc[2]])
            for t in range(NT2):
                v = 1 if t == NT2 - 1 else 0
                b2 = t % 2
                tsl = slice(t * 128, (t + 1) * 128)
                S.dma("sp", x1r[b2][:], I.orow(t), reads=[dxout[t]], writes=[dx1r[b2]])
                S.op("dve", lambda e: e.tensor_tensor(yacc[:, t, :], yacc[:, t, :], bc[v][:], op=ALU.mult), reads=[dbc[v]], writes=[dyacc[t]])
                S.op("pool", lambda e: e.tensor_tensor(xo[b2][:], yacc[:, t, :], x1r[b2][:], op=ALU.add), reads=[dyacc[t], dx1r[b2]], writes=[dxo[b2]])
                S.dma("sp", I.orow(t), xo[b2][:], reads=[dxo[b2], dx1r[b2]], writes=[dxout[t]])
                if not do_fin:
                    continue
                S.op("dve", lambda e: e.memset(sm[:], 0.0), writes=[dsm])
                S.op("act", lambda e: e.activation(junk[:], xo[b2][:], AF.Square, accum_out=sm[:, 0:1]), reads=[dxo[b2], dsm], writes=[djunk, dsm])
                rstd_from_ss(S, sm[:, 0:1], sm[:, 1:2], dsm, D, epst[:])
                S.op("dve", lambda e: e.scalar_tensor_tensor(xf[b2][:], xo[b2][:], sm[:, 1:2], bc[2][:], op0=ALU.mult, op1=ALU.mult),
                     reads=[dxo[b2], dsm, dbc[2]], writes=[dxf[b2]])
                S.dma("sp", I.frow(t), xf[b2][:], reads=[dxf[b2]], writes=[dxfin[t]])
            S.barrier()


CAP = 256
NBLK = (2 * 34 * 128 + CAP - 1) // CAP + NEXP
NSLOT = NBLK * CAP
I32 = mybir.dt.int32


def emit_k2s(nc, S, I, dX, dF, dmix, dmods, dxs, dys, do_fin=True):
    NT = NT1
    with ExitStack() as st:
        C = Ctx(nc, st)
        G12 = C.sb([128, NT, 2], F32); dG = [Dep() for _ in range(NT)]
        SL = C.sb([128, NT * 2], I32); dSL = [Dep() for _ in range(NT)]
        cnt = C.sb([128, 32], F32); dcnt = Dep()
        RK = C.sb([128, NT * 2], F32); EK = C.sb([128, NT * 2], F32); dRK = [Dep() for _ in range(NT)]
        H2B = C.sb([128, NT, D], BF16); dH2B = [Dep() for _ in range(NT)]
        pstart = C.sb([128, 32], F32); dps = Dep()
        IDXG = C.sb([128, NBLK], I32); dIDX = Dep()
        bc = [C.sb([128, D], F32) for _ in range(6)]; dbc = [Dep() for _ in range(6)]
        epst = C.sb([128, 1], F32); deps_ = Dep()
        cs2 = C.sb([128, 4, 128], F32); dcs2 = Dep()
        idb = C.sb([128, 128], BF16); didb = Dep()
        S.op("dve", lambda e: e.memset(epst[:], EPS), writes=[deps_])
        S.op("dve", lambda e: e.memset(cnt[:], 0.0), writes=[dcnt])
        S.dma("sp", cs2[:], I.cst2, writes=[dcs2])
        S.dma("pool", idb[:], I.ident, writes=[didb])
        idf = cs2[:, 0, :]; ustr = cs2[:, 1, :]; onesf = cs2[:, 2, :]; iota = cs2[:, 3, 0:32]; wbase = cs2[:, 3, 32:44]

        with ExitStack() as st1:
            woutb = C.sb([128, 8, D], BF16, st1); dwoutb = Dep()
            wrs = C.sb([128, 8, 36], F32, st1); dwrs = Dep()
            tmpw = C.sb([128, D], F32, st1); dtmpw = Dep()
            xt = [C.sb([128, D], F32, st1) for _ in range(2)]; dxt = [Dep(), Dep()]
            mt = [C.sb([128, 8, 128], BF16, st1) for _ in range(2)]; dmt = [Dep(), Dep()]
            tmp = C.sb([128, D], F32, st1); dtmp = Dep()
            x1 = [C.sb([128, D], F32, st1) for _ in range(2)]; dx1 = [Dep(), Dep()]
            h2f = C.sb([128, D], F32, st1); dh2f = Dep()
            h2Tf = C.sb([128, 8, 128], F32, st1); dh2Tf = Dep()
            junk = C.sb([128, D], F32, st1); djunk = Dep()
            sm = C.sb([128, 32], F32, st1); dsm = Dep()
            lg = C.sb([128, 36], F32, st1); dlg = Dep()
            elm = C.sb([128, 32], F32, st1)
            elm2 = C.sb([128, 32], F32, st1)
            mk1 = C.sb([128, 32], F32, st1)
            mk2 = C.sb([128, 32], F32, st1)
            mm_ = C.sb([128, 32], F32, st1)
            pos = C.sb([128, 32], F32, st1)
            t32 = C.sb([128, 32], F32, st1)
            gm = C.sb([128, 8], F32, st1)
            pmix = [C.ps([128, 512], F32, st1) for _ in range(2)]; dpmix = [Dep(), Dep()]
            ptf = C.ps([128, 8, 128], F32, st1); dptf = Dep()
            prt = C.ps([128, 36], F32, st1); dprt = Dep()
            pq = C.ps([128, 64], F32, st1); dpq = Dep()

            S.dma("pool", woutb[:], I.wout.rearrange("(c p) n -> p c n", p=128), writes=[dwoutb])
            S.dma("sp", wrs[:], I.wr.rearrange("(c p) n -> p c n", p=128), writes=[dwrs])
            S.dma("sp", tmpw[:], I.n2w.partition_broadcast(128), writes=[dtmpw])
            for v in range(2):
                S.dma("sp", bc[v][:], I.modrow(v, 2).partition_broadcast(128), reads=dmods, writes=[dbc[v]])
                S.dma("sp", bc[2 + v][:], I.modrow(v, 4).partition_broadcast(128), reads=dmods, writes=[dbc[2 + v]])
                S.dma("sp", bc[4 + v][:], I.modrow(v, 3).partition_broadcast(128), reads=dmods, writes=[dbc[4 + v]])
                S.op("dve", lambda e: e.scalar_tensor_tensor(bc[2 + v][:], bc[2 + v][:], 1.0, tmpw[:], op0=ALU.add, op1=ALU.mult),
                     reads=[dtmpw], writes=[dbc[2 + v]])
            for t in range(NT):
                v = 1 if t >= 32 else 0
                b2 = t % 2
                S.dma("sp", xt[b2][:], I.xrow(t), reads=[dX[t]], writes=[dxt[b2]])
                S.dma("sp", mt[b2][:], I.mtv(t), reads=dmix, writes=[dmt[b2]])
                for hf in range(2):
                    for c in range(8):
                        S.op("pe", lambda e: e.matmul(pmix[hf][:], mt[b2][:, c, :], woutb[:, c, hf * 512:(hf + 1) * 512], start=(c == 0), stop=(c == 7)),
                             reads=[dmt[b2], dwoutb], writes=[dpmix[hf]], pe_acc=(c > 0))
                    S.op("dve", lambda e: e.tensor_tensor(tmp[:, hf * 512:(hf + 1) * 512], pmix[hf][:], bc[v][:, hf * 512:(hf + 1) * 512], op=ALU.mult),
                         reads=[dpmix[hf], dbc[v]], writes=[dtmp])
                S.op("dve", lambda e: e.tensor_tensor(x1[b2][:], tmp[:], xt[b2][:], op=ALU.add), reads=[dtmp, dxt[b2]], writes=[dx1[b2]])
                S.dma("sp", I.orow(t), x1[b2][:], reads=[dx1[b2]], writes=[dX[t]])
                S.op("dve", lambda e: e.memset(sm[:], 0.0), writes=[dsm])
                S.op("act", lambda e: e.activation(junk[:], x1[b2][:], AF.Square, accum_out=sm[:, 0:1]), reads=[dx1[b2], dsm], writes=[djunk, dsm])
                rstd_from_ss(S, sm[:, 0:1], sm[:, 1:2], dsm, D, epst[:])
                S.op("dve", lambda e: e.scalar_tensor_tensor(h2f[:], x1[b2][:], sm[:, 1:2], bc[2 + v][:], op0=ALU.mult, op1=ALU.mult),
                     reads=[dx1[b2], dsm, dbc[2 + v]], writes=[dh2f])
                S.op("dve", lambda e: e.tensor_tensor(h2f[:], h2f[:], bc[4 + v][:], op=ALU.add), reads=[dbc[4 + v]], writes=[dh2f])
                S.op("act", lambda e: e.copy(H2B[:, t, :], h2f[:]), reads=[dh2f], writes=[dH2B[t]])
                for c in range(8):
                    S.op("pe", lambda e: e.transpose(ptf[:, c, :], h2f[:, c * 128:(c + 1) * 128], idf), reads=[dh2f, dcs2], writes=[dptf], pe_acc=(c > 0))
                S.op("act", lambda e: e.copy(h2Tf[:], ptf[:]), reads=[dptf], writes=[dh2Tf])
                for c in range(8):
                    S.op("pe", lambda e: e.matmul(prt[:], h2Tf[:, c, :], wrs[:, c, :], start=(c == 0), stop=(c == 7)),
                         reads=[dh2Tf, dwrs], writes=[dprt], pe_acc=(c > 0))
                S.op("act", lambda e: e.copy(lg[:], prt[:]), reads=[dprt], writes=[dlg])
                R = [dlg, dsm]
                V = lambda f: S.op("dve", f, reads=R, writes=R)
                V(lambda e: e.reduce_max(sm[:, 2:3], lg[:, 0:4], axis=AX.X))
                V(lambda e: e.tensor_scalar(sm[:, 3:4], sm[:, 2:3], -1.0, None, op0=ALU.mult))
                S.op("act", lambda e: e.activation(gm[:, 0:4], lg[:, 0:4], AF.Exp, bias=sm[:, 3:4], scale=1.0, accum_out=sm[:, 4:5]), reads=R, writes=R)
                V(lambda e: e.reciprocal(sm[:, 5:6], sm[:, 4:5]))
                V(lambda e: e.tensor_scalar(gm[:, 4:8], lg[:, 0:4], sm[:, 2:3], None, op0=ALU.is_equal))
                V(lambda e: e.tensor_scalar(gm[:, 4:8], gm[:, 4:8], 1e30, -1e30, op0=ALU.mult, op1=ALU.add))
                for g in range(4):
                    V(lambda e: e.tensor_scalar(elm[:, g * 8:(g + 1) * 8], lg[:, 4 + g * 8:12 + g * 8], gm[:, 4 + g:5 + g], None, op0=ALU.add))
                V(lambda e: e.reduce_max(sm[:, 6:7], elm[:], axis=AX.X))
                V(lambda e: e.tensor_scalar(mk1[:], elm[:], sm[:, 6:7], None, op0=ALU.is_equal))
                V(lambda e: e.scalar_tensor_tensor(elm2[:], mk1[:], -1e30, elm[:], op0=ALU.mult, op1=ALU.add))
                V(lambda e: e.reduce_max(sm[:, 7:8], elm2[:], axis=AX.X))
                V(lambda e: e.tensor_scalar(mk2[:], elm2[:], sm[:, 7:8], None, op0=ALU.is_equal))
                V(lambda e: e.tensor_tensor(sm[:, 8:9], sm[:, 7:8], sm[:, 6:7], op=ALU.subtract))
                S.op("act", lambda e: e.activation(sm[:, 9:10], sm[:, 8:9], AF.Exp), reads=R, writes=R)
                V(lambda e: e.tensor_scalar(sm[:, 10:11], sm[:, 9:10], 1.0, None, op0=ALU.add))
                V(lambda e: e.reciprocal(sm[:, 11:12], sm[:, 10:11]))
                S.op("dve", lambda e: e.tensor_tensor(G12[:, t, 0:1], sm[:, 11:12], sm[:, 5:6], op=ALU.mult), reads=R, writes=R + [dG[t]])
                S.op("dve", lambda e: e.tensor_tensor(G12[:, t, 1:2], G12[:, t, 0:1], sm[:, 9:10], op=ALU.mult), reads=R, writes=R + [dG[t]])
                V(lambda e: e.tensor_tensor(mm_[:], mk1[:], mk2[:], op=ALU.add))
                S.op("pe", lambda e: e.matmul(pq[:, 0:32], ustr, mm_[:], start=True, stop=True), reads=R + [dcs2], writes=[dpq])
                S.op("pe", lambda e: e.matmul(pq[:, 32:64], onesf, mm_[:], start=True, stop=True), reads=R + [dcs2], writes=[dpq], pe_acc=True)
                RC = R + [dcnt]
                S.op("dve", lambda e: e.tensor_tensor(pos[:], pq[:, 0:32], cnt[:], op=ALU.add), reads=[dpq] + RC, writes=RC)
                S.op("dve", lambda e: e.tensor_tensor(cnt[:], pq[:, 32:64], cnt[:], op=ALU.add), reads=[dpq] + RC, writes=RC)
                for k_, mk in enumerate((mk1, mk2)):
                    V(lambda e: e.tensor_tensor(t32[:], mk[:], pos[:], op=ALU.mult))
                    S.op("dve", lambda e: e.reduce_sum(RK[:, 2 * t + k_:2 * t + k_ + 1], t32[:], axis=AX.X), reads=R, writes=R + [dRK[t]])
                    V(lambda e: e.tensor_tensor(t32[:], mk[:], iota, op=ALU.mult))
                    S.op("dve", lambda e: e.reduce_sum(EK[:, 2 * t + k_:2 * t + k_ + 1], t32[:], axis=AX.X), reads=R, writes=R + [dRK[t]])
            pa = C.sb([128, 32], F32, st1); pb = C.sb([128, 32], F32, st1); pc_ = C.sb([128, 32], F32, st1)
            ebk = C.sb([128, NBLK], F32, st1); fi = C.sb([128, 12], F32, st1)
            Z = [dcnt, dps]
            W = lambda f: S.op("dve", f, reads=Z, writes=Z)
            W(lambda e: e.memset(pc_[:], 0.0))
            for m_ in range(2 * NT * 128 // CAP + 1):
                W(lambda e: e.scalar_tensor_tensor(pc_[:], cnt[:], float(m_ * CAP), pc_[:], op0=ALU.is_gt, op1=ALU.add))
            W(lambda e: e.tensor_scalar(pc_[:], pc_[:], float(CAP), None, op0=ALU.mult))
            W(lambda e: e.tensor_copy(pa[:], pc_[:]))
            src, dst = pa, pb
            for sh in (1, 2, 4, 8, 16):
                W(lambda e: e.tensor_copy(dst[:, 0:sh], src[:, 0:sh]))
                W(lambda e: e.tensor_tensor(dst[:, sh:32], src[:, sh:32], src[:, 0:32 - sh], op=ALU.add))
                src, dst = dst, src
            pend = src
            W(lambda e: e.tensor_tensor(pstart[:], pend[:], pc_[:], op=ALU.subtract))
            for b_ in range(NBLK):
                W(lambda e: e.tensor_scalar(t32[:], pend[:], float(b_ * CAP), None, op0=ALU.is_le))
                W(lambda e: e.reduce_sum(ebk[:, b_:b_ + 1], t32[:], axis=AX.X))
            W(lambda e: e.tensor_scalar(ebk[:], ebk[:], float(NEXP - 1), None, op0=ALU.min))
            W(lambda e: e.tensor_scalar(ebk[:], ebk[:], 128.0, float(I.wl * NEXP * 128), op0=ALU.mult, op1=ALU.add))
            W(lambda e: e.tensor_tensor(ebk[:], ebk[:], wbase[:, 0:1].to_broadcast([128, NBLK]), op=ALU.add))
            S.op("dve", lambda e: e.tensor_copy(IDXG[:], ebk[:]), reads=Z, writes=Z + [dIDX])
            for t in range(NT):
                for k_ in range(2):
                    j_ = 2 * t + k_
                    W(lambda e: e.tensor_scalar(t32[:], iota, EK[:, j_:j_ + 1], None, op0=ALU.is_equal))
                    W(lambda e: e.tensor_tensor(t32[:], t32[:], pstart[:], op=ALU.mult))
                    W(lambda e: e.reduce_sum(fi[:, 0:1], t32[:], axis=AX.X))
                    W(lambda e: e.tensor_tensor(fi[:, 0:1], fi[:, 0:1], RK[:, j_:j_ + 1], op=ALU.add))
                    S.op("dve", lambda e: e.tensor_copy(SL[:, j_:j_ + 1], fi[:, 0:1]), reads=Z + [dRK[t]], writes=Z + [dSL[t]])
                    S.idma(I.xs, H2B[:, t, :], SL[:, j_:j_ + 1], False, NSLOT, reads=[dH2B[t], dSL[t]], writes=[dxs])
            S.barrier()

        with ExitStack() as st2:
            wgb = [C.sb([128, 8, 512], BF16, st2) for _ in range(2)]
            wub = [C.sb([128, 8, 512], BF16, st2) for _ in range(2)]
            wdb = [C.sb([128, 4, D], BF16, st2) for _ in range(2)]
            dwg = [Dep(), Dep()]; dwu = [Dep(), Dep()]; dwd = [Dep(), Dep()]
            xr = [C.sb([128, D], BF16, st2) for _ in range(2)]; dxr = [Dep(), Dep()]
            xT = [C.sb([128, 8, CAP], BF16, st2) for _ in range(2)]; dxT = [[Dep() for _ in range(CAP // 128)] for _ in range(2)]
            sgt = [C.sb([128, 512], F32, st2) for _ in range(2)]; dsgt = [Dep(), Dep()]
            hid = [C.sb([128, 4, CAP], BF16, st2) for _ in range(2)]; dhid = [Dep(), Dep()]
            yo = [C.sb([128, D], F32, st2) for _ in range(2)]; dyo = [Dep(), Dep()]
            ptb = C.ps([128, 8, 128], BF16, st2); dptb = Dep()
            pg = [C.ps([128, 512], F32, st2) for _ in range(2)]; dpg = [Dep(), Dep()]
            pu = [C.ps([128, 512], F32, st2) for _ in range(2)]; dpu = [Dep(), Dep()]
            py = [C.ps([128, 512], F32, st2) for _ in range(2)]; dpy = [Dep(), Dep()]

            wgf, wuf, wdf = I.wgf, I.wuf, I.wdf

            def load_w(e):
                b = e % 2
                ix = IDXG[:, e:e + 1]
                S.idma(wgb[b][:].rearrange("p c n -> p (c n)"), wgf, ix, True, 0, reads=[dIDX], writes=[dwg[b]])
                S.idma(wub[b][:].rearrange("p c n -> p (c n)"), wuf, ix, True, 0, reads=[dIDX], writes=[dwu[b]])
                S.idma(wdb[b][:].rearrange("p c n -> p (c n)"), wdf, ix, True, 0, reads=[dIDX], writes=[dwd[b]])

            def load_x(e):
                b = e % 2
                for j in range(CAP // 128):
                    r0 = e * CAP + j * 128
                    xb_ = xr[j % 2]; dxb_ = dxr[j % 2]
                    S.dma("sp", xb_[:], I.xs[r0:r0 + 128, :], reads=[dxs], writes=[dxb_])
                    for c in range(8):
                        S.op("pe", lambda e_: e_.transpose(ptb[:, c, :], xb_[:, c * 128:(c + 1) * 128], idb[:]), reads=[dxb_, didb], writes=[dptb], pe_acc=(c > 0))
                    if j % 2 == 0:
                        S.op("act", lambda e_: e_.copy(xT[b][:, :, j * 128:(j + 1) * 128], ptb[:]), reads=[dptb], writes=[dxT[b][j]])
                    else:
                        S.op("dve", lambda e_: e_.tensor_copy(xT[b][:, :, j * 128:(j + 1) * 128], ptb[:]), reads=[dptb], writes=[dxT[b][j]])

            load_w(0)
            load_x(0)
            k = 0
            ky = 0
            for e_ in range(NBLK):
                b = e_ % 2
                if e_ + 1 < NBLK:
                    load_w(e_ + 1)
                hb = e_ % 2
                for j in range(4):
                    kk = k % 2
                    k += 1
                    for c in range(8):
                        S.op("pe", lambda e: e.matmul(pg[kk][:, 0:CAP], wgb[b][:, c, j * 128:(j + 1) * 128], xT[b][:, c, :], start=(c == 0), stop=(c == 7)),
                             reads=[dwg[b]] + dxT[b], writes=[dpg[kk]], pe_acc=(c > 0))
                    for c in range(8):
                        S.op("pe", lambda e: e.matmul(pu[kk][:, 0:CAP], wub[b][:, c, j * 128:(j + 1) * 128], xT[b][:, c, :], start=(c == 0), stop=(c == 7)),
                             reads=[dwu[b]] + dxT[b], writes=[dpu[kk]], pe_acc=(c > 0))
                    S.op("act", lambda e: e.activation(sgt[kk][:, 0:CAP], pg[kk][:, 0:CAP], AF.Silu), reads=[dpg[kk]], writes=[dsgt[kk]])
                    S.op("dve", lambda e: e.tensor_tensor(hid[hb][:, j, :], pu[kk][:, 0:CAP], sgt[kk][:, 0:CAP], op=ALU.mult),
                         reads=[dpu[kk], dsgt[kk]], writes=[dhid[hb]])
                if e_ + 1 < NBLK:
                    load_x(e_ + 1)
                for ti in range(CAP // 128):
                    yb = yo[ti % 2]; dyb = dyo[ti % 2]
                    for hf in range(2):
                        q = ky % 2
                        ky += 1
                        for j in range(4):
                            S.op("pe", lambda e: e.matmul(py[q][:], hid[hb][:, j, ti * 128:(ti + 1) * 128], wdb[b][:, j, hf * 512:(hf + 1) * 512], start=(j == 0), stop=(j == 3)),
                                 reads=[dhid[hb], dwd[b]], writes=[dpy[q]], pe_acc=(j > 0))
                        if hf == 0:
                            S.op("act", lambda e: e.copy(yb[:, 0:512], py[q][:]), reads=[dpy[q]], writes=[dyb])
                        else:
                            S.op("dve", lambda e: e.tensor_copy(yb[:, 512:1024], py[q][:]), reads=[dpy[q]], writes=[dyb])
                    r0 = e_ * CAP + ti * 128
                    S.dma("sp", I.ys[r0:r0 + 128, :], yb[:], reads=[dyb], writes=[dys])
            S.barrier()

        with ExitStack() as st3:
            x1r = [C.sb([128, D], F32, st3) for _ in range(2)]; dx1r = [Dep(), Dep()]
            o1 = [C.sb([128, D], F32, st3) for _ in range(2)]; do1 = [Dep(), Dep()]
            o2 = [C.sb([128, D], F32, st3) for _ in range(2)]; do2 = [Dep(), Dep()]
            xo = [C.sb([128, D], F32, st3) for _ in range(2)]; dxo = [Dep(), Dep()]
            xf = [C.sb([128, D], F32, st3) for _ in range(2)]; dxf = [Dep(), Dep()]
            junk = C.sb([128, D], F32, st3); djunk = Dep()
            sm = C.sb([128, 4], F32, st3); dsm = Dep()
            for v in range(2):
                S.dma("sp", bc[v][:], I.modrow(v, 5).partition_broadcast(128), reads=dmods, writes=[dbc[v]])
            S.dma("sp", bc[2][:], I.fnw.partition_broadcast(128), writes=[dbc[2]])
            for t in range(NT):
                v = 1 if t >= 32 else 0
                b2 = t % 2
                S.dma("sp", x1r[b2][:], I.orow(t), reads=[dX[t]], writes=[dx1r[b2]])
                S.idma(o1[b2][:], I.ys, SL[:, 2 * t:2 * t + 1], True, NSLOT, reads=[dys, dSL[t]], writes=[do1[b2]])
                S.idma(o2[b2][:], I.ys, SL[:, 2 * t + 1:2 * t + 2], True, NSLOT, reads=[dys, dSL[t]], writes=[do2[b2]])
                S.op("dve", lambda e: e.tensor_scalar(o1[b2][:], o1[b2][:], G12[:, t, 0:1], None, op0=ALU.mult), reads=[dG[t]], writes=[do1[b2]])
                S.op("dve", lambda e: e.scalar_tensor_tensor(o1[b2][:], o2[b2][:], G12[:, t, 1:2], o1[b2][:], op0=ALU.mult, op1=ALU.add),
                     reads=[do2[b2], dG[t]], writes=[do1[b2]])
                S.op("dve", lambda e: e.tensor_tensor(o1[b2][:], o1[b2][:], bc[v][:], op=ALU.mult), reads=[dbc[v]], writes=[do1[b2]])
                S.op("dve", lambda e: e.tensor_tensor(xo[b2][:], o1[b2][:], x1r[b2][:], op=ALU.add), reads=[do1[b2], dx1r[b2]], writes=[dxo[b2]])
                S.dma("sp", I.orow(t), xo[b2][:], reads=[dxo[b2], dx1r[b2]], writes=[dX[t]])
                if not do_fin:
                    continue
                S.op("dve", lambda e: e.memset(sm[:], 0.0), writes=[dsm])
                S.op("act", lambda e: e.activation(junk[:], xo[b2][:], AF.Square, accum_out=sm[:, 0:1]), reads=[dxo[b2], dsm], writes=[djunk, dsm])
                rstd_from_ss(S, sm[:, 0:1], sm[:, 1:2], dsm, D, epst[:])
                S.op("dve", lambda e: e.scalar_tensor_tensor(xf[b2][:], xo[b2][:], sm[:, 1:2], bc[2][:], op0=ALU.mult, op1=ALU.mult),
                     reads=[dxo[b2], dsm, dbc[2]], writes=[dxf[b2]])
                S.dma("sp", I.frow(t), xf[b2][:], reads=[dxf[b2]], writes=[dF[t]])
            S.barrier()


def moe_relayout(w):
    sh = w.shape
    c = sh[-2] // 128
    w5 = w.reshape(-1, c, 128, sh[-1]).transpose(0, 2, 1, 3)
    return np.ascontiguousarray(w5).reshape(-1, c * sh[-1])


def _consts2():
    t_ = np.arange(128)[:, None]
    u_ = np.arange(128)[None, :]
    io = np.zeros((128, 128), np.float32)
    io[:, 0:32] = np.arange(32, dtype=np.float32)[None, :]
    io[:, 32:44] = (np.arange(12) % 8 * 128 + np.where(np.arange(12) < 8, 0, 0))[None, :] + np.arange(128)[:, None]
    io[:, 40:44] = (np.arange(4) * 128)[None, :] + np.arange(128)[:, None]
    return np.stack([np.eye(128, dtype=np.float32), (t_ < u_).astype(np.float32), np.ones((128, 128), np.float32), io], axis=1)


def build_k2s():
    nc = bass.Bass("TRN2", target_bir_lowering=False)
    di = lambda n, s, d=F32: nc.dram_tensor(n, list(s), d, kind="ExternalInput").ap()
    xin = di("xin", [NTOK1, D]); mixT = di("mixT", [D, NTOK1], BF16); wout = di("wout", [D, D]); modr = di("modr", [8, D])
    n2w = di("n2w", [1, D]); fnw = di("fnw", [1, D]); wr = di("wr", [D, 36]); ident_in = di("ident", [128, 128]); cst2 = di("cst2", [128, 4, 128])
    wg = di("wg", [NEXP * 128, 8 * 512]); wu = di("wu", [NEXP * 128, 8 * 512]); wd = di("wd", [NEXP * 128, 4 * D])
    xout = nc.dram_tensor("xout", [NTOK1, D], F32, kind="ExternalOutput").ap()
    xfin = nc.dram_tensor("xfin", [NTOK1, D], F32, kind="ExternalOutput").ap()
    xs = nc.dram_tensor("xs", [NSLOT, D], BF16).ap()
    ys = nc.dram_tensor("ys", [NSLOT, D], F32).ap()
    with ExitStack() as st:
        S = Sched(nc, st)
        dX = [Dep() for _ in range(NT1)]
        dF = [Dep() for _ in range(NT1)]
        for t in range(NT1):
            S.dma("sp", xout[t * 128:(t + 1) * 128, :], xin[t * 128:(t + 1) * 128, :], writes=[dX[t]])
        I = type("I", (), dict(
            xrow=staticmethod(lambda t: xout[t * 128:(t + 1) * 128, :]), orow=staticmethod(lambda t: xout[t * 128:(t + 1) * 128, :]),
            frow=staticmethod(lambda t: xfin[t * 128:(t + 1) * 128, :]),
            mtv=staticmethod(lambda t: mixT.rearrange("(c p) t -> p c t", p=128)[:, :, t * 128:(t + 1) * 128]),
            modrow=staticmethod(lambda v, k: modr[4 * v + k - 2:4 * v + k - 1, :]),
            wout=wout, n2w=n2w, fnw=fnw, wr=wr, ident=ident_in, cst2=cst2, wl=0, wgf=wg, wuf=wu, wdf=wd, xs=xs, ys=ys))
        emit_k2s(nc, S, I, dX, dF, [], [], Dep(), Dep())
        S.finish(dX + dF)
        print("K2s instrs", S.n_ins, "waits", S.n_wait, "dma sems", S.ndsem, "sems", len(S.sems))
    return nc


def build_k2():
    nc = bass.Bass("TRN2", target_bir_lowering=False)
    di = lambda n, s, d=F32: nc.dram_tensor(n, list(s), d, kind="ExternalInput").ap()
    xin = di("xin", [NTOK2, D])
    mixT = di("mixT", [D, NTOK2], BF16)
    wout = di("wout", [D, D])
    modr = di("modr", [8, D])
    n2w = di("n2w", [1, D])
    fnw = di("fnw", [1, D])
    wr = di("wr", [D, 36])
    ident_in = di("ident", [128, 128])
    wg = di("wg", [NEXP, D, 512])
    wu = di("wu", [NEXP, D, 512])
    wd = di("wd", [NEXP, 512, D])
    xout = nc.dram_tensor("xout", [NTOK2, D], F32, kind="ExternalOutput").ap()
    xfin = nc.dram_tensor("xfin", [NTOK2, D], F32, kind="ExternalOutput").ap()

    with ExitStack() as st:
        S = Sched(nc, st)
        I = type("I", (), dict(
            xrow=staticmethod(lambda t: xin[t * 128:(t + 1) * 128, :]), orow=staticmethod(lambda t: xout[t * 128:(t + 1) * 128, :]),
            frow=staticmethod(lambda t: xfin[t * 128:(t + 1) * 128, :]),
            mtv=staticmethod(lambda t: mixT.rearrange("(c p) t -> p c t", p=128)[:, :, t * 128:(t + 1) * 128]),
            modrow=staticmethod(lambda v, k: modr[4 * v + k - 2:4 * v + k - 1, :]),
            wout=wout, n2w=n2w, fnw=fnw, wr=wr, ident=ident_in, wg=wg, wu=wu, wd=wd))
        dX = [Dep() for _ in range(NT2)]
        dF = [Dep() for _ in range(NT2)]
        emit_k2(nc, S, I, dX, dF, [], [])
        S.finish(dX + dF)
        print("K2 instrs", S.n_ins, "waits", S.n_wait, "dma sems", S.ndsem, "sems", len(S.sems))
    return nc


NT1 = 34
NTOK1 = NT1 * 128
TPAD = 4360
FM = [("qA", 128), ("qAp", 128), ("kA", 128), ("kAp", 128),
      ("qC0", 64), ("qC0p", 64), ("qC1", 64), ("qC1p", 64),
      ("kC0", 64), ("kC0p", 64), ("kC1", 64), ("kC1p", 64),
      ("xs0", 128), ("xs1", 128), ("bm", 64), ("cm", 64)]
FMOFF = {}
_o = 0
for _n, _m in FM:
    FMOFF[_n] = (_o, _m)
    _o += _m
NFM = _o
NTM = 456
NCOL1 = NFM + NTM


def xcol(t):
    return t + 2 if t < 4096 else t + 6


def ucol(t):
    return t if t < 4096 else t + 4


def emit_k1(nc, S, I, dX, dmix, dmods, hc=None):
    hmode = hc[0] if hc else None
    xin_unused = None
    convw, convb, dtb, alog, d0b, d1b, nwb, mixo, zs = I.convw, I.convb, I.dtb, I.alog, I.d0b, I.d1b, I.nwb, I.mixo, I.zs
    with ExitStack() as st:
        C = Ctx(nc, st)
        QA = C.sb([128, NTOK1], BF16); KA = C.sb([128, NTOK1], BF16)
        QC = [C.sb([64, NTOK1], BF16) for _ in range(2)]
        KC = [C.sb([64, NTOK1], BF16) for _ in range(2)]
        dQA = [Dep() for _ in range(9)]; dKA = [Dep() for _ in range(9)]
        dQC = [[Dep() for _ in range(9)] for _ in range(2)]; dKC = [[Dep() for _ in range(9)] for _ in range(2)]
        VA = C.sb([128, NT1, 128], BF16); dVA = [Dep() for _ in range(NT1)]
        VC = C.sb([128, NT1, 256], BF16); dVC = [Dep() for _ in range(NT1)]
        DT = C.sb([128, NT1, 8], F32); dDT = Dep()
        DTS = C.sb([128, NT1, 8], F32); DTA = C.sb([128, NT1, 8], F32); AN = C.sb([128, NT1 * 8], F32)
        XP = C.sb([128, 4, TPAD], BF16); dXP = [Dep() for _ in range(4)]
        cs = C.sb([128, 6, 128], F32); dcs = Dep()
        csb = C.sb([128, 6, 128], BF16); dcsb = Dep()
        epst = C.sb([128, 1], F32); dmisc = Dep()
        es = C.sb([128, 2], F32)
        lam = C.sb([128, 8], F32)
        cl = C.sb([128, 128], F32)
        wsc = C.sb([64, 1], F32)
        dzs = [Dep() for _ in range(NT1)]
        identf = cs[:, 0, :]; Ud = [cs[:, 1, :], cs[:, 2, :]]; onesf = cs[:, 3, :]; NEGd = [cs[:, 4, :], cs[:, 5, :]]
        identb = csb[:, 0, :]
        negprev = csb[:, 5, :]
        negnext = csb[:, 4, :]

        S.dma("sp", cs[:], I.cst, writes=[dcs])
        S.dma("pool", csb[:], I.cst, writes=[dcsb])
        S.op("dve", lambda e: e.memset(epst[:], EPS), writes=[dmisc])
        S.dma("sp", es[:], I.sink, writes=[dmisc])
        S.op("act", lambda e: e.activation(es[:], es[:], AF.Exp), reads=[dmisc], writes=[dmisc])
        S.dma("sp", cl[:], I.clam, writes=[dmisc])
        S.dma("sp", lam[:, 0:2], I.lconst, writes=[dmisc])
        S.dma("sp", wsc[:], I.subw, writes=[dmisc])
        M_ = [dmisc]
        S.op("dve", lambda e: e.tensor_tensor(cl[:, 0:32], cl[:, 0:32], cl[:, 32:64], op=ALU.mult), reads=M_, writes=M_)
        S.op("dve", lambda e: e.tensor_tensor(cl[:, 64:96], cl[:, 64:96], cl[:, 96:128], op=ALU.mult), reads=M_, writes=M_)
        S.op("dve", lambda e: e.reduce_sum(lam[:, 2:3], cl[:, 0:32], axis=AX.X), reads=M_, writes=M_)
        S.op("dve", lambda e: e.reduce_sum(lam[:, 3:4], cl[:, 64:96], axis=AX.X), reads=M_, writes=M_)
        S.op("act", lambda e: e.activation(lam[:, 2:4], lam[:, 2:4], AF.Exp), reads=M_, writes=M_)
        S.op("dve", lambda e: e.tensor_tensor(lam[:, 4:5], lam[:, 3:4], lam[:, 2:3], op=ALU.subtract), reads=M_, writes=M_)
        S.op("dve", lambda e: e.tensor_tensor(lam[:, 5:6], lam[:, 4:5], lam[:, 0:1], op=ALU.subtract), reads=M_, writes=M_)
        S.op("dve", lambda e: e.tensor_tensor(wsc[:], wsc[:], lam[0:64, 1:2], op=ALU.mult), reads=M_, writes=M_)
        nlam = lam[0:64, 5:6]
        S.op("pool", lambda e: e.memset(VA[:, :, 64:128], 1.0), writes=dVA)
        S.op("pool", lambda e: e.memset(VC[:, :, 64:128], 1.0), writes=dVC)
        S.op("pool", lambda e: e.memset(VC[:, :, 192:256], 1.0), writes=dVC)
        for s_ in range(4):
            S.op("pool", lambda e: e.memset(XP[:, s_, :], 0.0), writes=[dXP[s_]])

        with ExitStack() as st1:
            w1b = C.sb([128, 8, NCOL1], BF16, st1); dw1 = Dep()
            tmpw = C.sb([128, D], F32, st1); dtmpw = Dep()
            Abc = C.sb([128, D], F32, st1); Bbc = C.sb([128, D], F32, st1); dAbc = Dep(); dBbc = Dep()
            xt = [C.sb([128, D], F32, st1) for _ in range(2)]; dxt = [Dep(), Dep()]
            hb = C.sb([128, D], BF16, st1); dhb = Dep()
            sm = C.sb([128, 4], F32, st1); dsm = Dep()
            hT = [C.sb([128, 8, 512], BF16, st1) for _ in range(2)]; dhT = [[Dep() for _ in range(4)] for _ in range(2)]
            tA = C.sb([128, 2, 512], F32, st1); dtA = Dep()
            tC = C.sb([64, 2, 512], F32, st1); dtC = Dep()
            r1 = [C.sb([128, 512], F32, st1)] * 2; r2 = [C.sb([128, 512], F32, st1)] * 2
            dr1 = [Dep()] * 2; dr2 = [Dep()] * 2
            zt = [C.sb([128, 256], BF16, st1) for _ in range(2)]; dzt = [Dep(), Dep()]
            ptb = C.ps([128, 8, 128], BF16, st1); dptb = Dep()
            pf = [C.ps([128, 512], F32, st1) for _ in range(4)]; dpf = [Dep() for _ in range(4)]
            ptm = [C.ps([128, 512], F32, st1) for _ in range(2)]; dptm = [Dep(), Dep()]

            S.dma("pool", w1b[:, 0:4, :], I.w1.rearrange("(c p) n -> p c n", p=128)[:, 0:4, :], writes=[dw1])
            S.dma("pool", w1b[:, 4:8, :], I.w1.rearrange("(c p) n -> p c n", p=128)[:, 4:8, :], writes=[dw1])
            S.dma("sp", tmpw[:], I.n1w.partition_broadcast(128), writes=[dtmpw])

            def load_mod(v):
                S.dma("sp", Abc[:], I.modrow(v, 1).partition_broadcast(128), reads=dmods, writes=[dAbc])
                S.dma("sp", Bbc[:], I.modrow(v, 0).partition_broadcast(128), reads=dmods, writes=[dBbc])
                S.op("dve", lambda e: e.scalar_tensor_tensor(Abc[:], Abc[:], 1.0, tmpw[:], op0=ALU.add, op1=ALU.mult), reads=[dtmpw], writes=[dAbc])

            load_mod(0)
            kf = 0
            import os
            STG = int(os.environ.get('K1_STG', '9'))
            for blk in range(9):
                if blk >= int(os.environ.get('K1_BLK', '9')):
                    break
                ctxb = blk == 8
                nt = 2 if ctxb else 4
                ntok = nt * 128
                t0 = blk * 4
                tok0 = t0 * 128
                hb_i = blk % 2
                if ctxb:
                    load_mod(1)
                else:
                    S.dma(os.environ.get("K1_RQ", "sp"), tA[:], I.ropeA[:, :, tok0:tok0 + 512].rearrange("a p t -> p a t"), writes=[dtA])
                    S.dma(os.environ.get("K1_RQ", "sp"), tC[:], I.ropeC[:, :, tok0:tok0 + 512].rearrange("a p t -> p a t"), writes=[dtC])
                for j in range(nt):
                    t = t0 + j
                    b2 = t % 2
                    if hmode == "read":
                        S.dma("sp", hT[hb_i][:, :, j * 128:(j + 1) * 128], hc[1][t].rearrange("p (c k) -> p c k", c=8), reads=[hc[2][t]], writes=[dhT[hb_i][j]])
                    else:
                      S.dma("sp", xt[b2][:], I.xrow(t), reads=[dX[t]], writes=[dxt[b2]])
                    if hmode == "read":
                        pass
                    elif True:
                        S.op("dve", lambda e: e.memset(sm[:], 0.0), writes=[dsm])
                        S.op("act", lambda e: e.activation(hb[:], xt[b2][:], AF.Square, accum_out=sm[:, 0:1]), reads=[dxt[b2], dsm], writes=[dhb, dsm])
                        rstd_from_ss(S, sm[:, 0:1], sm[:, 1:2], dsm, D, epst[:])
                        S.op("dve", lambda e: e.scalar_tensor_tensor(xt[b2][:], xt[b2][:], sm[:, 1:2], Abc[:], op0=ALU.mult, op1=ALU.mult),
                             reads=[dsm, dAbc], writes=[dxt[b2]])
                        S.op("dve", lambda e: e.tensor_tensor(hb[:], xt[b2][:], Bbc[:], op=ALU.add), reads=[dxt[b2], dBbc], writes=[dhb])
                        for c in range(8):
                            S.op("pe", lambda e: e.transpose(ptb[:, c, :], hb[:, c * 128:(c + 1) * 128], identb), reads=[dhb, dcsb], writes=[dptb], pe_acc=(c > 0))
                        S.op("act", lambda e: e.copy(hT[hb_i][:, :, j * 128:(j + 1) * 128], ptb[:]), reads=[dptb], writes=[dhT[hb_i][j]])

                        if hmode == "write":
                            S.dma("sp", hc[1][t].rearrange("p (c k) -> p c k", c=8), hT[hb_i][:, :, j * 128:(j + 1) * 128], reads=[dhT[hb_i][j]], writes=[hc[2][t]])
                    if STG < 2:
                        continue
                    pt_ = ptm[t % 2]; dpt_ = dptm[t % 2]
                    for c in range(8):
                        S.op("pe", lambda e: e.matmul(pt_[:, 0:NTM], hT[hb_i][:, c, j * 128:(j + 1) * 128], w1b[:, c, NFM:NCOL1], start=(c == 0), stop=(c == 7)),
                             reads=[dhT[hb_i][j], dw1], writes=[dpt_], pe_acc=(c > 0))
                    SB2 = int(os.environ.get('K1_SB2', '9'))
                    if SB2 >= 2:
                        S.op("act", lambda e: e.copy(VA[:, t, 0:64], pt_[:, 0:64]), reads=[dpt_], writes=[dVA[t]])
                    if SB2 >= 3:
                        for hh in range(2):
                            S.op("act", lambda e: e.copy(VC[:, t, hh * 128:hh * 128 + 64], pt_[:, 64 + hh * 64:128 + hh * 64]), reads=[dpt_], writes=[dVC[t]])
                    if SB2 >= 4:
                        S.op("act", lambda e: e.copy(zt[t % 2][:], pt_[:, 192:448]), reads=[dpt_], writes=[dzt[t % 2]])
                        S.dma("sp", zs[t * 128:(t + 1) * 128, :], zt[t % 2][:], reads=[dzt[t % 2]], writes=[dzs[t]])
                    if SB2 >= 5:
                        S.op("act", lambda e: e.copy(DT[:, t, :], pt_[:, 448:456]), reads=[dpt_], writes=[dDT])
                hdeps = dhT[hb_i][0:nt]

                def fm_group(name, pidx):
                    off, m = FMOFF[name]
                    p = pf[pidx]
                    for c in range(8):
                        S.op("pe", lambda e: e.matmul(p[0:m, 0:ntok], w1b[:, c, off:off + m], hT[hb_i][:, c, 0:ntok], start=(c == 0), stop=(c == 7)),
                             reads=hdeps + [dw1], writes=[dpf[pidx]], pe_acc=(c > 0))
                    return p

                def rope_pair(name, dst, ddst, tab, dtab, m):
                    nonlocal kf
                    i0 = (kf % 2) * 2
                    kf += 1
                    p = fm_group(name, i0)
                    if ctxb or os.environ.get('K1_NOROPE'):
                        S.op("act", lambda e: e.copy(dst[0:m, tok0:tok0 + ntok], p[0:m, 0:ntok]), reads=[dpf[i0]], writes=[ddst])
                        return
                    pp = fm_group(name + "p", i0 + 1)
                    rr = (kf % 2)
                    S.op("dve", lambda e: e.tensor_tensor(r1[rr][0:m, :], p[0:m, :], tab[0:m, 0, :], op=ALU.mult), reads=[dpf[i0], dtab], writes=[dr1[rr]])
                    S.op("dve", lambda e: e.tensor_tensor(r2[rr][0:m, :], pp[0:m, :], tab[0:m, 1, :], op=ALU.mult), reads=[dpf[i0 + 1], dtab], writes=[dr2[rr]])
                    S.op("dve", lambda e: e.tensor_tensor(dst[0:m, tok0:tok0 + ntok], r1[rr][0:m, :], r2[rr][0:m, :], op=ALU.add),
                         reads=[dr1[rr], dr2[rr]], writes=[ddst])

                if STG < 3:
                    continue
                pairs = [("qA", QA, dQA[blk], tA, dtA, 128), ("kA", KA, dKA[blk], tA, dtA, 128)]
                for hh in range(2):
                    pairs.append(("qC%d" % hh, QC[hh], dQC[hh][blk], tC, dtC, 64))
                    pairs.append(("kC%d" % hh, KC[hh], dKC[hh][blk], tC, dtC, 64))
                for pi_, pr_ in enumerate(pairs):
                    if pi_ < int(os.environ.get('K1_NP', '9')):
                        rope_pair(*pr_)
                if STG < 4:
                    continue
                c0 = xcol(tok0)
                for si, name in enumerate(["xs0", "xs1", "bm", "cm"]):
                    i0 = kf % 4
                    kf += 1
                    m = FMOFF[name][1]
                    p = fm_group(name, i0)
                    if si % 2 == 0:
                        S.op("act", lambda e: e.copy(XP[0:m, si, c0:c0 + ntok], p[0:m, 0:ntok]), reads=[dpf[i0]], writes=[dXP[si]])
                    else:
                        S.op("dve", lambda e: e.tensor_copy(XP[0:m, si, c0:c0 + ntok], p[0:m, 0:ntok]), reads=[dpf[i0]], writes=[dXP[si]])
            S.barrier()
        print("K1 after phase1: instrs", S.n_ins, "waits", S.n_wait)
        import os
        if int(os.environ.get("K1_UPTO", "9")) >= 2:
            _k1_rest(nc, S, C, locals())


def build_k1():
    nc = bass.Bass("TRN2", target_bir_lowering=False)
    di = lambda n, s, d=F32: nc.dram_tensor(n, list(s), d, kind="ExternalInput").ap()
    xin = di("xin", [NTOK1, D])
    modv = di("modv", [4, D])
    n1w = di("n1w", [1, D])
    w1 = di("w1", [D, NCOL1])
    convw = di("convw", [128, 20])
    convb = di("convb", [128, 4])
    dtb = di("dtb", [128, NT1 * 8])
    alog = di("alog", [128, NT1 * 8])
    d0b = di("d0b", [128, 256])
    d1b = di("d1b", [128, 256])
    nwb = di("nwb", [128, 256])
    sink = di("sink", [128, 2])
    clam = di("clam", [128, 128])
    lconst = di("lconst", [128, 2])
    subw = di("subw", [64, 1])
    cst = di("cst", [128, 6, 128])
    ropeA = di("ropeA", [2, 128, 4096])
    ropeC = di("ropeC", [2, 64, 4096])
    mixo = nc.dram_tensor("mixo", [512, NTOK1], BF16, kind="ExternalOutput").ap()
    zs = nc.dram_tensor("zs", [NTOK1, 256], BF16, kind="ExternalOutput").ap()

    with ExitStack() as st:
        S = Sched(nc, st)
        I = type("I", (), dict(
            xrow=staticmethod(lambda t: xin[t * 128:(t + 1) * 128, :]), modrow=staticmethod(lambda v, k: modv[2 * v + k:2 * v + k + 1, :]),
            n1w=n1w, w1=w1, convw=convw, convb=convb, dtb=dtb, alog=alog, d0b=d0b, d1b=d1b, nwb=nwb, sink=sink, clam=clam, lconst=lconst,
            subw=subw, cst=cst, ropeA=ropeA, ropeC=ropeC, mixo=mixo, zs=zs))
        dmix = Dep()
        emit_k1(nc, S, I, [Dep() for _ in range(NT1)], dmix, [])
        S.finish([dmix])
        print("K1 instrs", S.n_ins, "waits", S.n_wait, "dma sems", S.ndsem, "sems", len(S.sems))
    return nc


def _k1_rest(nc, S, C, L):
    V = type("V", (), L)
    QA, KA, QC, KC, VA, VC, XP = V.QA, V.KA, V.QC, V.KC, V.VA, V.VC, V.XP
    dQA, dKA, dQC, dKC, dVA, dVC, dXP = V.dQA, V.dKA, V.dQC, V.dKC, V.dVA, V.dVC, V.dXP
    DT, DTS, DTA, AN, dDT = V.DT, V.DTS, V.DTA, V.AN, V.dDT
    cs, dcs, csb, dcsb, dmisc = V.cs, V.dcs, V.csb, V.dcsb, V.dmisc
    identf, Ud, onesf, NEGd, identb, negprev, negnext = V.identf, V.Ud, V.onesf, V.NEGd, V.identb, V.negprev, V.negnext
    es, wsc, nlam, epst, mixo, zs, dzs, dmix = V.es, V.wsc, V.nlam, V.epst, V.mixo, V.zs, V.dzs, V.dmix
    allQA = list(dQA); allKA = list(dKA)
    blk_of = lambda tok: min(tok // 512, 8)

    with ExitStack() as sa:
        E = [C.sb([128, 5, 2, 128], BF16, sa) for _ in range(2)]; dE = [Dep(), Dep()]
        rd = [C.sb([64, 128], F32, sa) for _ in range(2)]; drd = [Dep(), Dep()]
        oa = [C.sb([64, 128], BF16, sa) for _ in range(4)]; doa = [Dep() for _ in range(4)]
        pss = [C.ps([128, 5, 128], F32, sa) for _ in range(2)]; dpss = [Dep(), Dep()]
        po = [C.ps([128, 2, 128], F32, sa) for _ in range(2)]; dpo = [Dep(), Dep()]
        ko = 0
        for qb in range(NT1):
            if qb < 32:
                kts = []
                if qb > 0:
                    kts.append((qb - 1, negprev))
                kts.append((qb, None))
                if qb < 31:
                    kts.append((qb + 1, negnext))
                kts += [(32, None), (33, None)]
            else:
                kts = [(32, None), (33, None)]
            nk = len(kts)
            qsl = slice(qb * 128, (qb + 1) * 128)
            eb = qb % 2
            for hh in range(2):
                hs = slice(hh * 64, (hh + 1) * 64)
                p = pss[hh]
                for i, (kt, msk) in enumerate(kts):
                    S.op("pe", lambda e: e.matmul(p[:, i, :], KA[hs, kt * 128:(kt + 1) * 128], QA[hs, qsl], start=True, stop=(msk is None)),
                         reads=[dKA[blk_of(kt * 128)], dQA[blk_of(qb * 128)]], writes=[dpss[hh]], pe_acc=(i > 0))
                    if msk is not None:
                        S.op("pe", lambda e: e.matmul(p[:, i, :], identb, msk, start=False, stop=True), reads=[dcsb], writes=[dpss[hh]], pe_acc=True)
                S.op("act", lambda e: e.activation(E[eb][:, 0:nk, hh, :], p[:, 0:nk, :], AF.Exp, scale=0.125), reads=[dpss[hh]], writes=[dE[eb]])
            pv = po[qb % 2]; dpv = dpo[qb % 2]
            for i, (kt, msk) in enumerate(kts):
                S.op("pe", lambda e: e.matmul(pv[:], VA[:, kt, :], E[eb][:, i, :, :], start=(i == 0), stop=(i == nk - 1)),
                     reads=[dVA[kt], dE[eb]], writes=[dpv], pe_acc=(i > 0))
            for hh in range(2):
                r_ = rd[hh]; o_ = oa[ko % 4]; do_ = doa[ko % 4]
                ko += 1
                S.op("dve", lambda e: e.tensor_scalar(r_[:], pv[64:128, hh, :], es[64:128, hh:hh + 1], None, op0=ALU.add), reads=[dpv, dmisc], writes=[drd[hh]])
                S.op("dve", lambda e: e.reciprocal(r_[:], r_[:]), reads=[drd[hh]], writes=[drd[hh]])
                S.op("dve", lambda e: e.tensor_tensor(o_[:], pv[0:64, hh, :], r_[:], op=ALU.mult), reads=[dpv, drd[hh]], writes=[do_])
                S.dma("sp", mixo[hh * 64:(hh + 1) * 64, qsl], o_[:], reads=[do_], writes=[dmix])
        S.barrier()
    print("K1 after A: instrs", S.n_ins, "waits", S.n_wait)
    import os
    if int(os.environ.get("K1_UPTO", "9")) < 3:
        return

    with ExitStack() as sc:
        Et = [C.sb([128, 512], BF16, sc) for _ in range(4)]; dEt = [Dep() for _ in range(4)]
        f32t = lambda: C.sb([64, 512], F32, sc)
        rd0, rd1, t0_, t1_, o_, sq_, rs_ = [f32t() for _ in range(7)]
        dfin = Dep()
        oc = [C.sb([64, 512], BF16, sc) for _ in range(2)]; doc = [Dep(), Dep()]
        ones64 = cs[0:64, 3, 0:64]
        acc = [[C.ps([128, 512], F32, sc) for _ in range(2)] for _ in range(2)]
        dacc = [[Dep(), Dep()] for _ in range(2)]
        psc = [C.ps([128, 512], F32, sc) for _ in range(4)]; dpsc = [Dep() for _ in range(4)]
        kk = 0
        qblocks = [(i * 512, 512, list(range(NT1))) for i in range(8)] + [(4096, 256, [32, 33])]
        def finalize(q0, qn, hh):
            nonlocal kk
            F = [dfin]
            a0 = acc[hh][0]; a1 = acc[hh][1]
            S.op("dve", lambda e: e.reciprocal(rd0[:, 0:qn], a0[64:128, 0:qn]), reads=[dacc[hh][0]] + F, writes=F)
            S.op("dve", lambda e: e.reciprocal(rd1[:, 0:qn], a1[64:128, 0:qn]), reads=[dacc[hh][1]] + F, writes=F)
            S.op("dve", lambda e: e.tensor_tensor(t0_[:, 0:qn], a0[0:64, 0:qn], rd0[:, 0:qn], op=ALU.mult), reads=[dacc[hh][0]] + F, writes=F)
            S.op("dve", lambda e: e.tensor_tensor(t1_[:, 0:qn], a1[0:64, 0:qn], rd1[:, 0:qn], op=ALU.mult), reads=[dacc[hh][1]] + F, writes=F)
            S.op("dve", lambda e: e.scalar_tensor_tensor(o_[:, 0:qn], t1_[:, 0:qn], nlam, t0_[:, 0:qn], op0=ALU.mult, op1=ALU.add), reads=F + [dmisc], writes=F)
            S.op("pool", lambda e: e.tensor_tensor(sq_[:, 0:qn], o_[:, 0:qn], o_[:, 0:qn], op=ALU.mult), reads=F, writes=F)
            p = psc[kk % 4]; dp = dpsc[kk % 4]
            S.op("pe", lambda e: e.matmul(p[0:64, 0:qn], ones64, sq_[:, 0:qn], start=True, stop=True), reads=F + [dcs], writes=[dp])
            S.op("act", lambda e: e.activation(rs_[:, 0:qn], p[0:64, 0:qn], AF.Ln, bias=epst[0:64, :], scale=1.0 / 64), reads=[dp, dmisc] + F, writes=F)
            S.op("act", lambda e: e.activation(rs_[:, 0:qn], rs_[:, 0:qn], AF.Exp, scale=-0.5), reads=F, writes=F)
            ob = oc[hh]
            S.op("dve", lambda e: e.scalar_tensor_tensor(ob[:, 0:qn], o_[:, 0:qn], wsc[:, 0:1], rs_[:, 0:qn], op0=ALU.mult, op1=ALU.mult),
                 reads=F + [dmisc], writes=[doc[hh]])
            S.dma("sp", mixo[384 + hh * 64:384 + (hh + 1) * 64, q0:q0 + qn], ob[:, 0:qn], reads=[doc[hh]], writes=[dmix])

        LOOK = 3
        pending = None
        for (q0, qn, kts) in qblocks:
            qblk = blk_of(q0)
            for hh_u in range(2):
                items = [(ki, kt, hh_u, m) for ki, kt in enumerate(kts) for m in range(2)]
                bufs = {}

                def qk_exp(i):
                    nonlocal kk
                    ki, kt, hh, m = items[i]
                    ms = slice(m * 32, (m + 1) * 32)
                    bi = kk % 4
                    kk += 1
                    bufs[i] = bi
                    p = psc[bi]; dp = dpsc[bi]; e_ = Et[bi]; de_ = dEt[bi]
                    S.op("pe", lambda e: e.matmul(p[:, 0:qn], KC[hh][ms, kt * 128:(kt + 1) * 128], QC[hh][ms, q0:q0 + qn], start=True, stop=True),
                         reads=[dKC[hh][blk_of(kt * 128)], dQC[hh][qblk]], writes=[dp])
                    S.op("act", lambda e: e.activation(e_[:, 0:qn], p[:, 0:qn], AF.Exp, scale=32 ** -0.5), reads=[dp], writes=[de_])

                def pv(i):
                    ki, kt, hh, m = items[i]
                    bi = bufs[i]
                    e_ = Et[bi]; de_ = dEt[bi]
                    S.op("pe", lambda e: e.matmul(acc[hh][m][:, 0:qn], VC[:, kt, hh * 128:(hh + 1) * 128], e_[:, 0:qn], start=(ki == 0), stop=(ki == len(kts) - 1)),
                         reads=[dVC[kt], de_], writes=[dacc[hh][m]], pe_acc=(ki > 0))

                for i in range(len(items) + LOOK):
                    if i < len(items):
                        qk_exp(i)
                    if i - LOOK >= 0:
                        pv(i - LOOK)
                    if i == 8 and pending is not None:
                        finalize(*pending)
                        pending = None
                if pending is not None:
                    finalize(*pending)
                pending = (q0, qn, hh_u)
        if pending is not None:
            finalize(*pending)
        S.barrier()
    print("K1 after C: instrs", S.n_ins, "waits", S.n_wait)
    if int(os.environ.get("K1_UPTO", "9")) < 4:
        return

    with ExitStack() as sb_:
        cw = C.sb([128, 20], F32, sb_); cb = C.sb([128, 4], F32, sb_); dcw = Dep()
        ctmp = [C.sb([128, 1024], F32, sb_)] * 2; dctmp = [Dep()] * 2
        XTM = C.sb([128, NT1, 320], BF16, sb_); dXTM = [Dep() for _ in range(NT1)]
        Y = C.sb([128, NT1, 256], F32, sb_); dY = [Dep() for _ in range(NT1)]
        dtbt = C.sb([128, NT1 * 8], F32, sb_)
        dsum = C.sb([128, 256], F32, sb_); d1t = C.sb([128, 256], F32, sb_); nwt = C.sb([128, 256], F32, sb_); dpar = Dep()
        H = C.sb([64, 256], F32, sb_); Hb = C.sb([64, 256], BF16, sb_); dH = Dep(); dHb = Dep()
        sm = [C.sb([128, 24], F32, sb_) for _ in range(2)]; dsmm = [Dep(), Dep()]
        xdt = [C.sb([128, 4, 64], BF16, sb_) for _ in range(2)]; dxdt = [Dep(), Dep()]
        xdw = [C.sb([128, 4, 64], BF16, sb_) for _ in range(2)]; dxdw = [Dep(), Dep()]
        GT = [C.sb([128, 128], F32, sb_) for _ in range(2)]; dGT = [Dep(), Dep()]
        lrep4 = C.sb([128, 4, 128], F32, sb_); dlrep4 = Dep()
        X4 = C.sb([128, 4, 128], F32, sb_); dX4 = Dep()
        dec4 = C.sb([128, 4, 128], F32, sb_); ddec4 = Dep()
        MT4 = C.sb([128, 4, 128], BF16, sb_); dMT4 = Dep()
        tmpy = C.sb([128, 4, 64], F32, sb_); dtmpy = Dep()
        NDTA = C.sb([128, NT1, 8], F32, sb_)
        zr = [C.sb([128, 256], BF16, sb_) for _ in range(2)]; dzr = [Dep(), Dep()]
        f1 = C.sb([128, 256], F32, sb_); f2 = C.sb([128, 256], F32, sb_); f3 = C.sb([128, 256], F32, sb_); dff = Dep()
        fjunk = C.sb([128, 256], F32, sb_)
        fs = C.sb([128, 4], F32, sb_)
        obt = C.sb([128, 256], BF16, sb_); dobt = Dep()
        obT = [C.sb([128, 2, 128], BF16, sb_) for _ in range(2)]; dobT = [Dep(), Dep()]
        pc = C.ps([128, 8], F32, sb_); dpc = Dep()
        pgt = C.ps([128, 128], F32, sb_); dpgt = Dep()
        pd4 = C.ps([128, 4, 128], F32, sb_); dpd4 = Dep()
        pyd = C.ps([128, 4, 64], F32, sb_); dpyd = Dep()
        pyo = C.ps([128, 256], F32, sb_); dpyo = Dep()
        pst = C.ps([64, 256], F32, sb_); dpst = Dep()
        ptr = C.ps([128, 512], BF16, sb_); dptr = Dep()

        S.dma("sp", cw[:], V.convw, writes=[dcw])
        S.dma("sp", cb[:], V.convb, writes=[dcw])
        S.dma("sp", dtbt[:], V.dtb, writes=[dpar])
        S.dma("sp", AN[:], V.alog, writes=[dpar])
        S.dma("sp", dsum[:], V.d0b, writes=[dpar])
        S.dma("sp", d1t[:], V.d1b, writes=[dpar])
        S.dma("sp", nwt[:], V.nwb, writes=[dpar])
        P_ = [dpar, dDT]
        DTf = DT[:].rearrange("p a b -> p (a b)"); DTSf = DTS[:].rearrange("p a b -> p (a b)"); DTAf = DTA[:].rearrange("p a b -> p (a b)")
        S.op("dve", lambda e: e.tensor_tensor(dsum[:], dsum[:], d1t[:], op=ALU.add), reads=P_, writes=P_)
        S.op("dve", lambda e: e.tensor_tensor(DTf, DTf, dtbt[:], op=ALU.add), reads=P_, writes=P_)
        S.op("act", lambda e: e.activation(DTSf, DTf, AF.Exp), reads=P_, writes=P_)
        S.op("act", lambda e: e.activation(DTSf, DTSf, AF.Ln, bias=1.0), reads=P_, writes=P_)
        S.op("act", lambda e: e.activation(AN[:], AN[:], AF.Exp), reads=P_, writes=P_)
        S.op("dve", lambda e: e.scalar_tensor_tensor(DTAf, DTSf, -1.0, AN[:], op0=ALU.mult, op1=ALU.mult), reads=P_, writes=P_)
        S.op("dve", lambda e: e.tensor_scalar(NDTA[:].rearrange("p a b -> p (a b)"), DTAf, -1.0, None, op0=ALU.mult), reads=P_, writes=P_)

        segs = [(i * 1024, 1024) for i in range(4)] + [(4096, 256)]
        kc = 0
        for si in range(4):
            m = 128 if si < 2 else 64
            for (tk0, n) in segs:
                eng = "dve"
                tb = ctmp[kc % 2]; dtb_ = dctmp[kc % 2]
                kc += 1
                c0 = xcol(tk0) - 2
                S.op(eng, lambda e: e.tensor_scalar(tb[0:m, 0:n], XP[0:m, si, c0:c0 + n], cw[0:m, si * 5:si * 5 + 1], None, op0=ALU.mult),
                     reads=[dXP[si], dcw], writes=[dtb_])
                for k in range(1, 5):
                    S.op(eng, lambda e: e.scalar_tensor_tensor(tb[0:m, 0:n], XP[0:m, si, c0 + k:c0 + k + n], cw[0:m, si * 5 + k:si * 5 + k + 1], tb[0:m, 0:n],
                                                               op0=ALU.mult, op1=ALU.add), reads=[dXP[si], dcw], writes=[dtb_])
                u0 = ucol(tk0)
                S.op("act", lambda e: e.activation(XP[0:m, si, u0:u0 + n], tb[0:m, 0:n], AF.Silu, bias=cb[0:m, si:si + 1], scale=1.0),
                     reads=[dtb_, dcw], writes=[dXP[si]])
        for t in range(NT1):
            u0 = ucol(t * 128)
            S.op("pe", lambda e: e.transpose(ptr[:, 0:128], XP[:, 0, u0:u0 + 128], identb), reads=[dXP[0], dcsb], writes=[dptr])
            S.op("pe", lambda e: e.transpose(ptr[:, 128:256], XP[:, 1, u0:u0 + 128], identb), reads=[dXP[1], dcsb], writes=[dptr], pe_acc=True)
            S.op("pe", lambda e: e.transpose(ptr[:, 256:320], XP[0:64, 2, u0:u0 + 128], csb[0:64, 0, 0:64]), reads=[dXP[2], dcsb], writes=[dptr], pe_acc=True)
            if t % 2 == 0:
                S.op("act", lambda e: e.copy(XTM[:, t, :], ptr[:, 0:320]), reads=[dptr], writes=[dXTM[t]])
            else:
                S.op("dve", lambda e: e.tensor_copy(XTM[:, t, :], ptr[:, 0:320]), reads=[dptr], writes=[dXTM[t]])

        order = {0: [32, 33] + list(range(32)), 1: [33, 32] + list(range(31, -1, -1))}
        it = 0
        for d in range(2):
            S.op("dve", lambda e: e.memset(H[:], 0.0), writes=[dH])
            S.op("dve", lambda e: e.memset(Hb[:], 0.0), writes=[dHb])
            for c in order[d]:
                u0 = ucol(c * 128)
                dta = DTA[:, c, d * 4:(d + 1) * 4]
                dts = DTS[:, c, d * 4:(d + 1) * 4]
                s_ = sm[it % 2]; ds_ = dsmm[it % 2]
                xd = xdt[it % 2]; dxd = dxdt[it % 2]; xw = xdw[it % 2]; dxw = dxdw[it % 2]
                g_ = GT[it % 2]; dg_ = dGT[it % 2]
                it += 1
                S.op("pe", lambda e: e.matmul(pc[:, 0:4], Ud[d], dta, start=True, stop=True), reads=[dcs, dpar], writes=[dpc])
                S.op("pe", lambda e: e.matmul(pc[:, 4:8], onesf, dta, start=True, stop=True), reads=[dcs, dpar], writes=[dpc], pe_acc=True)
                Q_ = [ds_]
                S.op("act", lambda e: e.copy(s_[:, 0:4], pc[:, 0:4]), reads=[dpc], writes=Q_)
                S.op("act", lambda e: e.copy(s_[:, 16:20], pc[:, 4:8]), reads=[dpc] + Q_, writes=Q_)
                S.op("dve", lambda e: e.tensor_scalar(s_[:, 4:8], s_[:, 0:4], -1.0, None, op0=ALU.mult), reads=Q_, writes=Q_)
                S.op("act", lambda e: e.activation(s_[:, 8:12], s_[:, 0:4], AF.Exp), reads=Q_, writes=Q_)
                S.op("dve", lambda e: e.tensor_tensor(s_[:, 12:16], s_[:, 16:20], s_[:, 0:4], op=ALU.subtract), reads=Q_, writes=Q_)
                S.op("act", lambda e: e.activation(s_[:, 12:16], s_[:, 12:16], AF.Exp), reads=Q_, writes=Q_)
                S.op("act", lambda e: e.activation(s_[:, 16:20], s_[:, 16:20], AF.Exp), reads=Q_, writes=Q_)
                S.op("dve", lambda e: e.tensor_tensor(s_[:, 20:24], dts, s_[:, 12:16], op=ALU.mult), reads=Q_ + [dpar], writes=Q_)
                xv = XTM[:, c, 0:256].rearrange("p (a b) -> p a b", a=4)
                S.op("pool", lambda e: e.tensor_tensor(xd[:], xv, dts.unsqueeze(2).to_broadcast([128, 4, 64]), op=ALU.mult), reads=[dXTM[c], dpar], writes=[dxd])
                S.op("pool", lambda e: e.tensor_tensor(xw[:], xv, s_[:, 20:24].unsqueeze(2).to_broadcast([128, 4, 64]), op=ALU.mult), reads=[dXTM[c]] + Q_, writes=[dxw])
                S.op("pe", lambda e: e.matmul(pgt[:], XP[0:64, 2, u0:u0 + 128], XP[0:64, 3, u0:u0 + 128], start=True, stop=True),
                     reads=[dXP[2], dXP[3]], writes=[dpgt])
                S.op("act", lambda e: e.copy(g_[:], pgt[:]), reads=[dpgt], writes=[dg_])
                S.op("dve", lambda e: e.tensor_copy(lrep4[:], dta.unsqueeze(2).to_broadcast([128, 4, 128])), reads=[dpar], writes=[dlrep4])
                S.op("dve", lambda e: e.tensor_tensor(X4[:], Ud[d].unsqueeze(1).to_broadcast([128, 4, 128]), NDTA[:, c, d * 4:(d + 1) * 4].unsqueeze(2).to_broadcast([128, 4, 128]), op=ALU.mult),
                     reads=[dcs, dpar], writes=[dX4])
                for h in range(4):
                    S.op("pe", lambda e: e.matmul(pd4[:, h, :], lrep4[:, h, :], Ud[d], start=True, stop=False), reads=[dlrep4, dcs], writes=[dpd4], pe_acc=(h > 0))
                    S.op("pe", lambda e: e.matmul(pd4[:, h, :], X4[:, h, :], onesf, start=False, stop=False), reads=[dX4, dcs], writes=[dpd4], pe_acc=True)
                    S.op("pe", lambda e: e.matmul(pd4[:, h, :], identf, NEGd[d], start=False, stop=True), reads=[dcs], writes=[dpd4], pe_acc=True)
                S.op("act", lambda e: e.activation(dec4[:], pd4[:], AF.Exp), reads=[dpd4], writes=[ddec4])
                S.op("dve", lambda e: e.tensor_tensor(MT4[:], dec4[:], g_[:].unsqueeze(1).to_broadcast([128, 4, 128]), op=ALU.mult), reads=[ddec4, dg_], writes=[dMT4])
                for h in range(4):
                    S.op("pe", lambda e: e.matmul(pyd[:, h, :], MT4[:, h, :], xd[:, h, :], start=True, stop=True), reads=[dMT4, dxd], writes=[dpyd], pe_acc=(h > 0))
                S.op("pe", lambda e: e.matmul(pyo[:], XP[0:64, 3, u0:u0 + 128], Hb[:], start=True, stop=True), reads=[dXP[3], dHb], writes=[dpyo])
                S.op("pe", lambda e: e.matmul(pst[:], XTM[:, c, 256:320], xw[:].rearrange("p a b -> p (a b)"), start=True, stop=True),
                     reads=[dXTM[c], dxw], writes=[dpst])
                Yc = Y[:, c, :]
                if d == 0:
                    S.op("act", lambda e: e.copy(Yc, pyd[:].rearrange("p a b -> p (a b)")), reads=[dpyd], writes=[dY[c]])
                else:
                    S.op("dve", lambda e: e.tensor_tensor(Yc, pyd[:].rearrange("p a b -> p (a b)"), Yc, op=ALU.add), reads=[dpyd], writes=[dY[c]])
                S.op("dve", lambda e: e.tensor_tensor(tmpy[:], pyo[:].rearrange("p (a b) -> p a b", a=4), s_[:, 8:12].unsqueeze(2).to_broadcast([128, 4, 64]), op=ALU.mult),
                     reads=[dpyo] + Q_, writes=[dtmpy])
                S.op("pool", lambda e: e.tensor_tensor(Yc, Yc, tmpy[:].rearrange("p a b -> p (a b)"), op=ALU.add), reads=[dtmpy], writes=[dY[c]])
                Hv = H[:].rearrange("p (a b) -> p a b", a=4)
                S.op("dve", lambda e: e.tensor_tensor(Hv, Hv, s_[0:64, 16:20].unsqueeze(2).to_broadcast([64, 4, 64]), op=ALU.mult), reads=Q_, writes=[dH])
                S.op("dve", lambda e: e.tensor_tensor(H[:], H[:], pst[:], op=ALU.add), reads=[dpst], writes=[dH])
                S.op("act", lambda e: e.copy(Hb[:], H[:]), reads=[dH], writes=[dHb])
                if d == 1:
                    zb = zr[c % 2]; dzb = dzr[c % 2]
                    S.dma("sp", zb[:], zs[c * 128:(c + 1) * 128, :], reads=[dzs[c]], writes=[dzb])
                    Fd = [dff]
                    S.op("dve", lambda e: e.tensor_tensor(f1[:], XTM[:, c, 0:256], dsum[:], op=ALU.mult), reads=[dXTM[c], dpar] + Fd, writes=Fd)
                    S.op("pool", lambda e: e.tensor_tensor(f1[:], f1[:], Yc, op=ALU.add), reads=[dY[c]] + Fd, writes=Fd)
                    S.op("act", lambda e: e.activation(f2[:], zb[:], AF.Silu), reads=[dzb] + Fd, writes=Fd)
                    S.op("pool", lambda e: e.tensor_tensor(f3[:], f1[:], f2[:], op=ALU.mult), reads=Fd, writes=Fd)
                    S.op("dve", lambda e: e.memset(fs[:], 0.0), reads=Fd, writes=Fd)
                    S.op("act", lambda e: e.activation(fjunk[:], f3[:], AF.Square, accum_out=fs[:, 0:1]), reads=Fd, writes=Fd)
                    rstd_from_ss(S, fs[:, 0:1], fs[:, 1:2], dff, 256, epst[:])
                    S.op("dve", lambda e: e.scalar_tensor_tensor(obt[:], f3[:], fs[:, 1:2], nwt[:], op0=ALU.mult, op1=ALU.mult), reads=Fd + [dpar], writes=[dobt])
                    S.op("pe", lambda e: e.transpose(ptr[:, 0:128], obt[:, 0:128], identb), reads=[dobt, dcsb], writes=[dptr])
                    S.op("pe", lambda e: e.transpose(ptr[:, 128:256], obt[:, 128:256], identb), reads=[dobt, dcsb], writes=[dptr], pe_acc=True)
                    ot = obT[c % 2]; dot_ = dobT[c % 2]
                    S.op("act", lambda e: e.copy(ot[:], ptr[:, 0:256].rearrange("p (a b) -> p a b", a=2)), reads=[dptr], writes=[dot_])
                    S.dma("sp", mixo[128:384, c * 128:(c + 1) * 128].rearrange("(a p) t -> p a t", p=128), ot[:], reads=[dot_], writes=[dmix])
        S.barrier()


OFF_AK, OFF_AV, OFF_BZ, OFF_BX, OFF_BDT, OFF_CQ, OFF_CK, OFF_CV = 256, 384, 512, 1024, 1792, 1808, 2064, 2320


def _rope_perm(dim):
    q = dim // 4
    idx = np.arange(dim)
    return np.where((idx // q) % 2 == 0, idx + q, idx - q)


def k1_cols(g):
    pa, pc = _rope_perm(64), _rope_perm(32)
    a64 = np.arange(64)
    cols = []
    cols.append(np.concatenate([(2 * g + hh) * 64 + a64 for hh in range(2)]))
    cols.append(np.concatenate([(2 * g + hh) * 64 + pa for hh in range(2)]))
    cols.append(np.concatenate([OFF_AK + g * 64 + a64] * 2))
    cols.append(np.concatenate([OFF_AK + g * 64 + pa] * 2))
    pc2 = np.concatenate([pc, 32 + pc])
    for off in (OFF_CQ, OFF_CK):
        for hh in range(2):
            cols.append(off + (2 * g + hh) * 64 + a64)
            cols.append(off + (2 * g + hh) * 64 + pc2)
    cols.append(OFF_BX + g * 256 + np.arange(128))
    cols.append(OFF_BX + g * 256 + 128 + np.arange(128))
    cols.append(OFF_BX + 512 + g * 64 + a64)
    cols.append(OFF_BX + 640 + g * 64 + a64)
    cols.append(OFF_AV + g * 64 + a64)
    cols.append(OFF_CV + 2 * g * 64 + np.arange(128))
    cols.append(OFF_BZ + g * 256 + np.arange(256))
    cols.append(OFF_BDT + 4 * g + np.arange(4))
    cols.append(OFF_BDT + 8 + 4 * g + np.arange(4))
    c = np.concatenate(cols)
    assert c.shape[0] == NCOL1
    return c


def _rope_tables():
    t = np.arange(4096)
    row, col = (t // 64).astype(np.float64), (t % 64).astype(np.float64)

    def tab(dim, reps):
        q = dim // 4
        cosr = np.zeros((dim, 4096)); sinr = np.zeros((dim, 4096))
        for j in range(dim):
            blk, i = j // q, j % q
            f = np.float32(10000.0) ** (-np.float32(i) / np.float32(q))
            pos = row if blk < 2 else col
            ang = (pos.astype(np.float32) * np.float32(f)).astype(np.float32)
            cosr[j] = np.cos(ang)
            sinr[j] = np.sin(ang) * (-1.0 if blk % 2 == 0 else 1.0)
        return np.stack([np.tile(cosr, (reps, 1)), np.tile(sinr, (reps, 1))]).astype(np.float32)

    return tab(64, 2), tab(32, 2)


def _consts():
    s = np.arange(128)[:, None]
    l = np.arange(128)[None, :]
    z = np.zeros((128, 128), np.float32)
    return np.stack([np.eye(128, dtype=np.float32), (s <= l).astype(np.float32), (s >= l).astype(np.float32),
                     np.ones((128, 128), np.float32), np.where(s > l, NEG, z).astype(np.float32),
                     np.where(s < l, NEG, z).astype(np.float32)], axis=1)


def k1_inputs(p, l, b, g, xfull, modrow, modctx, ropeA, ropeC, cst):
    f = np.float32
    rep = lambda v, n=128: np.ascontiguousarray(np.broadcast_to(np.asarray(v, f).reshape(1, -1), (n, np.asarray(v).size)))
    sh1, sc1 = modrow[0:D], modrow[D:2 * D]
    sh1c, sc1c = modctx[0:D], modctx[D:2 * D]
    cw = np.zeros((128, 4, 5), f)
    cb = np.zeros((128, 4), f)
    chs = [g * 256 + np.arange(128), g * 256 + 128 + np.arange(128), 512 + g * 64 + np.arange(64), 640 + g * 64 + np.arange(64)]
    for si, ch in enumerate(chs):
        cw[:len(ch), si, :] = p['b_conv_w'][l][:, ch].T
        cb[:len(ch), si] = p['b_conv_b'][l][ch]
    hsel = 4 * g + np.arange(4)
    dtb = np.tile(np.concatenate([p['b_dt_bias'][l][0, hsel], p['b_dt_bias'][l][1, hsel]]), NT1)
    alog = np.tile(np.concatenate([p['b_a_log'][l][0, hsel], p['b_a_log'][l][1, hsel]]), NT1)
    lam0 = 0.8 - 0.6 * math.exp(-0.3 * l)
    return dict(
        xin=xfull, modv=np.stack([sh1, sc1, sh1c, sc1c]).astype(f), n1w=p['norm1_w'][l][None].astype(f),
        w1=np.ascontiguousarray(p['w_in'][l][:, k1_cols(g)]),
        convw=cw.reshape(128, 20), convb=cb, dtb=rep(dtb), alog=rep(alog),
        d0b=rep(np.repeat(p['b_d'][l][0, hsel], 64)), d1b=rep(np.repeat(p['b_d'][l][1, hsel], 64)),
        nwb=rep(p['b_norm_w'][l][g * 256:(g + 1) * 256]), sink=rep(p['a_sink'][l][2 * g:2 * g + 2]),
        clam=rep(p['c_lambda'][l].reshape(-1)), lconst=rep(np.array([lam0, 1.0 - lam0], f)),
        subw=np.ascontiguousarray(p['c_subln_w'][l].reshape(64, 1).astype(f)), cst=cst, ropeA=ropeA, ropeC=ropeC)


def mix_from_k1(o0, o1):
    return np.concatenate([o0[0:128], o1[0:128], o0[128:384], o1[128:384], o0[384:512], o1[384:512]], axis=0)


WOUT_ORDER = [0, 2, 3, 6, 1, 4, 5, 7]


def emit_k0f(nc, S, ccT, wmod, bmod, mods, dmods):
    with ExitStack() as st:
        C = Ctx(nc, st)
        cs = C.sb([128, 8, 2], F32); dcs = Dep()
        sg = C.sb([128, 8, 2], F32)
        ws = [C.sb([128, 8, 512], F32) for _ in range(2)]; dws = [Dep(), Dep()]
        bs = C.sb([2, 6144], F32); dbs = Dep()
        os_ = C.sb([2, 6144], F32); dos = Dep()
        pp = [C.ps([2, 512], F32) for _ in range(2)]; dpp = [Dep(), Dep()]
        S.dma("sp", cs[:], ccT.rearrange("(c p) m -> p c m", p=128), writes=[dcs])
        S.op("act", lambda e: e.activation(sg[:], cs[:], AF.Sigmoid), reads=[dcs], writes=[dcs])
        S.op("dve", lambda e: e.tensor_tensor(cs[:], cs[:], sg[:], op=ALU.mult), reads=[dcs], writes=[dcs])
        k = 0
        for l in range(DEPTH):
            S.dma("sp", bs[:], bmod[l].partition_broadcast(2), writes=[dbs])
            for n in range(12):
                w_ = ws[k % 2]; dw_ = dws[k % 2]; p = pp[k % 2]; dp = dpp[k % 2]
                k += 1
                S.dma("sp", w_[:], wmod[l].rearrange("(c p) n -> p c n", p=128)[:, :, n * 512:(n + 1) * 512], writes=[dw_])
                for c in range(8):
                    S.op("pe", lambda e: e.matmul(p[:], cs[:, c, :], w_[:, c, :], start=(c == 0), stop=(c == 7)), reads=[dcs, dw_], writes=[dp], pe_acc=(c > 0))
                S.op("dve", lambda e: e.tensor_tensor(os_[:, n * 512:(n + 1) * 512], p[:], bs[:, n * 512:(n + 1) * 512], op=ALU.add), reads=[dp, dbs], writes=[dos])
            S.dma("sp", mods[l], os_[:], reads=[dos], writes=[dmods])
        S.barrier()


def emit_zero_rows(nc, S, dram, dep, rows):
    with ExitStack() as st:
        C = Ctx(nc, st)
        n = rows // 128
        g = 11 if n % 11 == 0 else (8 if n % 8 == 0 else 1)
        z = C.sb([128, g, D], BF16); dz = Dep()
        S.op("pool", lambda e: e.memset(z[:], 0.0), writes=[dz])
        v = dram.rearrange("(a p) d -> p a d", p=128)
        for i in range(n // g):
            S.dma("sp", v[:, i * g:(i + 1) * g, :], z[:], reads=[dz], writes=[dep])
        S.barrier()


def build_fused():
    nc = bass.Bass("TRN2", target_bir_lowering=False)
    di = lambda n, s, d=F32: nc.dram_tensor(n, list(s), d, kind="ExternalInput").ap()
    do = lambda n, s, d=F32: nc.dram_tensor(n, list(s), d, kind="ExternalOutput").ap()
    x0 = di("x0", [NTOK1, D]); ccT = di("ccT", [D, 2]); wmod = di("wmod", [DEPTH, D, 6 * D]); bmod = di("bmod", [DEPTH, 1, 6 * D])
    n1w = di("n1w", [DEPTH, 1, D]); w1 = di("w1", [DEPTH, 2, D, NCOL1])
    convw = di("convw", [DEPTH, 2, 128, 20]); convb = di("convb", [DEPTH, 2, 128, 4])
    dtb = di("dtb", [DEPTH, 2, 128, NT1 * 8]); alog = di("alog", [DEPTH, 2, 128, NT1 * 8])
    d0b = di("d0b", [DEPTH, 2, 128, 256]); d1b = di("d1b", [DEPTH, 2, 128, 256]); nwb = di("nwb", [DEPTH, 2, 128, 256])
    sink = di("sink", [DEPTH, 2, 128, 2]); clam = di("clam", [DEPTH, 128, 128]); lconst = di("lconst", [DEPTH, 128, 2]); subw = di("subw", [DEPTH, 64, 1])
    cst = di("cst", [128, 6, 128]); ropeA = di("ropeA", [2, 128, 4096]); ropeC = di("ropeC", [2, 64, 4096])
    wout = di("wout", [DEPTH, D, D]); n2w = di("n2w", [DEPTH, 1, D]); fnw = di("fnw", [1, D]); wr = di("wr", [DEPTH, D, 36]); ident = di("ident", [128, 128])
    wg = di("wg", [DEPTH * NEXP * 128, 8 * 512]); wu = di("wu", [DEPTH * NEXP * 128, 8 * 512]); wd = di("wd", [DEPTH * NEXP * 128, 4 * D])
    out = do("out", [4096, D])
    xcur = do("xcur", [NTOK1, D]); mixo = do("mixo", [2, 512, NTOK1], BF16); zs = do("zs", [NTOK1, 256], BF16)
    mods = do("mods", [DEPTH, 2, 6 * D]); fscr = do("fscr", [128, D])
    cst2 = di("cst2", [128, 4, 128])
    xs = nc.dram_tensor("xs", [NSLOT, D], BF16).ap()
    ys = nc.dram_tensor("ys", [NSLOT, D], F32).ap()
    hcache = nc.dram_tensor("hcache", [NT1, 128, D], BF16).ap()
    with ExitStack() as st:
        S = Sched(nc, st)
        dX = [Dep() for _ in range(NT1)]
        dF = [Dep() for _ in range(NT1)]
        dmods = Dep()
        dmixg = [Dep(), Dep()]
        dxs = Dep(); dys = Dep()
        dhc = [Dep() for _ in range(NT1)]
        for t in range(NT1):
            S.dma("sp", xcur[t * 128:(t + 1) * 128, :], x0[t * 128:(t + 1) * 128, :], writes=[dX[t]])
        emit_zero_rows(nc, S, xs, dxs, NSLOT)
        emit_k0f(nc, S, ccT, wmod, bmod, mods, dmods)
        for l in range(DEPTH):
            for g in range(2):
                I1 = type("I1", (), dict(
                    xrow=staticmethod(lambda t: xcur[t * 128:(t + 1) * 128, :]),
                    modrow=staticmethod(lambda v, k, l=l: mods[l, v:v + 1, k * D:(k + 1) * D]),
                    n1w=n1w[l], w1=w1[l, g], convw=convw[l, g], convb=convb[l, g], dtb=dtb[l, g], alog=alog[l, g], d0b=d0b[l, g], d1b=d1b[l, g],
                    nwb=nwb[l, g], sink=sink[l, g], clam=clam[l], lconst=lconst[l], subw=subw[l], cst=cst, ropeA=ropeA, ropeC=ropeC,
                    mixo=mixo[g], zs=zs))
                emit_k1(nc, S, I1, dX, dmixg[g], [dmods], hc=("write" if g == 0 else "read", hcache, dhc))
            last = l == DEPTH - 1
            I2 = type("I2", (), dict(
                xrow=staticmethod(lambda t: xcur[t * 128:(t + 1) * 128, :]),
                orow=staticmethod(lambda t: xcur[t * 128:(t + 1) * 128, :]),
                frow=staticmethod(lambda t: out[t * 128:(t + 1) * 128, :] if t < 32 else fscr),
                mtv=staticmethod(lambda t: mixo.rearrange("g (k p) t -> p (g k) t", p=128)[:, :, t * 128:(t + 1) * 128]),
                modrow=staticmethod(lambda v, k, l=l: mods[l, v:v + 1, k * D:(k + 1) * D]),
                wout=wout[l], n2w=n2w[l], fnw=fnw, wr=wr[l], ident=ident, cst2=cst2, wl=l, wgf=wg, wuf=wu, wdf=wd, xs=xs, ys=ys))
            emit_k2s(nc, S, I2, dX, dF, dmixg, [dmods], dxs, dys, do_fin=last)
            print("fused: layer", l, "instrs", S.n_ins, "waits", S.n_wait, "sems", len(S.sems))
        S.finish(dF + dX + dmixg + [dmods])
    return nc


def fused_inputs(p, b, ropeA, ropeC, cst):
    f = np.float32
    K1 = [[k1_inputs(p, l, b, g, None, np.zeros(2 * D, f), np.zeros(2 * D, f), ropeA, ropeC, cst) for g in range(2)] for l in range(DEPTH)]
    stk = lambda key: np.ascontiguousarray(np.stack([np.stack([K1[l][g][key] for g in range(2)]) for l in range(DEPTH)]))
    stl = lambda key: np.ascontiguousarray(np.stack([K1[l][0][key] for l in range(DEPTH)]))
    im = dict(
        x0=np.ascontiguousarray(np.concatenate([p['x'][b], p['ctx'][b]], 0).astype(f)),
        ccT=np.ascontiguousarray(np.stack([p['c'][b], p['c_ctx']], 0).T.astype(f)),
        wmod=p['w_mod'], bmod=np.ascontiguousarray(p['b_mod'][:, None, :]),
        n1w=np.ascontiguousarray(p['norm1_w'][:, None, :]), w1=stk('w1'), convw=stk('convw'), convb=stk('convb'), dtb=stk('dtb'), alog=stk('alog'),
        d0b=stk('d0b'), d1b=stk('d1b'), nwb=stk('nwb'), sink=stk('sink'), clam=stl('clam'), lconst=stl('lconst'), subw=stl('subw'),
        cst=cst, ropeA=ropeA, ropeC=ropeC,
        wout=np.ascontiguousarray(np.stack([np.concatenate([p['w_out'][l][c * 128:(c + 1) * 128] for c in WOUT_ORDER], 0) for l in range(DEPTH)])),
        n2w=np.ascontiguousarray(p['norm2_w'][:, None, :]), fnw=np.ascontiguousarray(p['final_norm_w'][None]),
        wr=np.ascontiguousarray(np.concatenate([p['moe_group_router'], p['moe_router']], axis=2).astype(f)),
        ident=np.eye(128, dtype=f), cst2=_consts2(), wg=p['_wg_r'], wu=p['_wu_r'], wd=p['_wd_r'])
    return im


_NC = {}


def _get(name, fn):
    if name not in _NC:
        _NC[name] = fn()
    return _NC[name]


def kernel_unfused(**inp):
    p = {k: np.asarray(v) for k, v in inp.items()}
    f = np.float32
    B, L = 4, 4096
    cores = list(range(8))
    ccT = np.ascontiguousarray(np.concatenate([p['c'], p['c_ctx'][None]], 0).T.astype(f))
    im0 = []
    for j in cores:
        l, hf = j // 2, j % 2
        im0.append(dict(ccT=ccT, w=np.ascontiguousarray(p['w_mod'][l][:, hf * 3072:(hf + 1) * 3072]),
                        b=np.ascontiguousarray(p['b_mod'][l][None, hf * 3072:(hf + 1) * 3072])))
    r0 = run_bass_kernel_spmd(_get('k0', build_k0), im0, core_ids=cores).results
    mods = [np.concatenate([r0[2 * l]['out'], r0[2 * l + 1]['out']], axis=1) for l in range(DEPTH)]

    ropeA, ropeC = _rope_tables()
    cst = _consts()
    ident = np.eye(128, dtype=f)
    x = p['x'].astype(f)
    xc = p['ctx'].astype(f)
    out = None
    for l in range(DEPTH):
        m = mods[l]
        im1 = []
        for j in cores:
            b, g = j // 2, j % 2
            xfull = np.ascontiguousarray(np.concatenate([x[b], xc[b]], 0))
            im1.append(k1_inputs(p, l, b, g, xfull, m[b], m[4], ropeA, ropeC, cst))
        r1 = run_bass_kernel_spmd(_get('k1', build_k1), im1, core_ids=cores).results
        wr = np.ascontiguousarray(np.concatenate([p['moe_group_router'][l], p['moe_router'][l]], axis=1).astype(f))
        im2 = []
        for j in cores:
            b, s = j // 2, j % 2
            mix = mix_from_k1(r1[2 * b]['mixo'], r1[2 * b + 1]['mixo'])
            mixT = np.ascontiguousarray(np.concatenate([mix[:, s * 2048:(s + 1) * 2048], mix[:, 4096 + s * 128:4096 + (s + 1) * 128]], 1))
            xin = np.ascontiguousarray(np.concatenate([x[b, s * 2048:(s + 1) * 2048], xc[b, s * 128:(s + 1) * 128]], 0))
            sp6 = lambda v: [v[i * D:(i + 1) * D] for i in range(6)]
            ml, mc = sp6(m[b]), sp6(m[4])
            modr = np.stack([ml[2], ml[3], ml[4], ml[5], mc[2], mc[3], mc[4], mc[5]]).astype(f)
            im2.append(dict(xin=xin, mixT=mixT, wout=p['w_out'][l], modr=modr, n2w=p['norm2_w'][l][None], fnw=p['final_norm_w'][None],
                            wr=wr, ident=ident, wg=p['moe_w_gate'][l], wu=p['moe_w_up'][l], wd=p['moe_w_down'][l]))
        r2 = run_bass_kernel_spmd(_get('k2', build_k2), im2, core_ids=cores).results
        key = 'xfin' if l == DEPTH - 1 else 'xout'
        xn = np.empty_like(x)
        xcn = np.empty_like(xc)
        for j in cores:
            b, s = j // 2, j % 2
            o = r2[j][key]
            xn[b, s * 2048:(s + 1) * 2048] = o[0:2048]
            xcn[b, s * 128:(s + 1) * 128] = o[2048:2176]
        x, xc = xn, xcn
    return x


def kernel(**inp):
    p = {k: np.asarray(v) for k, v in inp.items()}
    ropeA, ropeC = _rope_tables()
    cst = _consts()
    p['_wg_r'] = moe_relayout(p['moe_w_gate']); p['_wu_r'] = moe_relayout(p['moe_w_up']); p['_wd_r'] = moe_relayout(p['moe_w_down'])
    per_b = [fused_inputs(p, b, ropeA, ropeC, cst) for b in range(4)]
    in_maps = [per_b[j // 2] for j in range(8)]
    res = run_bass_kernel_spmd(_get('fused', build_fused), in_maps, core_ids=list(range(8))).results
    return np.stack([res[2 * b]['out'] for b in range(4)]).astype(np.float32)
```
